# Optimizing a Trainium2 kernel written in Bass

```python
import math
import jax, jax.numpy as jnp
from jax import lax
import numpy as np

D_MODEL = 1024
BATCH = 16
SEQ = 2048
DEPTH = 4

N_EVEN = (DEPTH + 1) // 2
N_ODD = DEPTH // 2
DN_ALPHA = (2.0 * DEPTH) ** 0.25
DN_BETA = (8.0 * DEPTH) ** -0.25
LN_EPS = 1e-5
RMS_EPS = 1e-6
Q_BLOCK = 128

A_HEADS = 8
A_HEAD_DIM = 64
A_KV_LATENT = 128
IDX_HEADS = 8
IDX_DIM = 64
TOPK_MAX = 256

B_HEADS = 4
B_DK = 64
B_DV = 128
B_GATE_RANK = 16
B_GATE_TEMP = 16.0
B_CHUNK = 64

EVEN_SPLITS = (A_HEADS * A_HEAD_DIM, A_KV_LATENT, IDX_HEADS * IDX_DIM, IDX_DIM, IDX_HEADS,
               B_HEADS * B_DK, B_HEADS * B_DK, B_HEADS * B_DV, B_GATE_RANK, B_HEADS * B_DV)
EVEN_IN = sum(EVEN_SPLITS)
EVEN_MIX = A_HEADS * A_HEAD_DIM + B_HEADS * B_DV

C_HEADS = 8
C_QK_DIM = 64
C_V_DIM = 2 * C_QK_DIM
C_MIX = C_HEADS * C_V_DIM

D_FF = 2816
N_EXPERTS = 8
TOP_K = 2
D_FF_EXPERT = 2816

kernel_name = 'hybrid_dsa_gla_diffattn_moe_deepnorm'


def layer_norm(x, g, b):
    xf = x.astype(jnp.float32)
    mu = jnp.mean(xf, axis=-1, keepdims=True)
    var = jnp.mean(jnp.square(xf - mu), axis=-1, keepdims=True)
    return ((xf - mu) * lax.rsqrt(var + LN_EPS) * g + b).astype(x.dtype)


def rms_norm(x, g):
    xf = x.astype(jnp.float32)
    return (xf * lax.rsqrt(jnp.mean(jnp.square(xf), axis=-1, keepdims=True) + RMS_EPS) * g).astype(x.dtype)


def split_cols(t, sizes):
    out, off = [], 0
    for s in sizes:
        out.append(t[..., off:off + s])
        off += s
    return out


def query_blocks(t):
    bsz, seq = t.shape[0], t.shape[1]
    return t.reshape((bsz, seq // Q_BLOCK, Q_BLOCK) + t.shape[2:]).swapaxes(0, 1)


def dsa_attention(q, c_kv, q_idx, k_idx, w_idx, kv_norm_g, w_uk, w_uv, kidx_ln_g, kidx_ln_b):
    bsz, seq = q.shape[0], q.shape[1]
    n_sel = min(TOPK_MAX, seq // 4)
    c = rms_norm(c_kv, kv_norm_g)
    k = c @ w_uk
    v = c @ w_uv
    k_i = layer_norm(k_idx, kidx_ln_g, kidx_ln_b).astype(jnp.float32)
    key_pos = jnp.arange(seq)
    gather = jax.vmap(lambda t, idx: t[idx])

    def block(args):
        qb, qib, wb, start = args
        qpos = start + jnp.arange(Q_BLOCK)
        rel = jax.nn.relu(jnp.einsum('bqhd,bsd->bqhs', qib.astype(jnp.float32), k_i) * IDX_DIM ** -0.5)
        score = jnp.einsum('bqh,bqhs->bqs', wb.astype(jnp.float32) * IDX_HEADS ** -0.5, rel)
        causal = key_pos[None, :] <= qpos[:, None]
        score = jnp.where(causal[None], score, -jnp.inf)
        _, idx = lax.top_k(score, n_sel)
        k_sel = gather(k, idx)
        v_sel = gather(v, idx)
        logits = jnp.einsum('bqhd,bqkd->bqhk', qb, k_sel).astype(jnp.float32) * A_HEAD_DIM ** -0.5
        valid = (idx <= qpos[None, :, None])[:, :, None, :]
        p = jax.nn.softmax(jnp.where(valid, logits, -jnp.inf), axis=-1).astype(v.dtype)
        return jnp.einsum('bqhk,bqkd->bqhd', p, v_sel)

    starts = jnp.arange(seq // Q_BLOCK) * Q_BLOCK
    out = lax.map(block, (query_blocks(q), query_blocks(q_idx), query_blocks(w_idx), starts))
    return out.swapaxes(0, 1).reshape(bsz, seq, A_HEADS * A_HEAD_DIM)


def gla_chunked(q, k, v, log_a):
    bsz, seq, nh, dk = q.shape
    dv = v.shape[-1]
    n = seq // B_CHUNK

    def chunks(t):
        return t.reshape(bsz, n, B_CHUNK, nh, t.shape[-1]).transpose(1, 0, 3, 2, 4).astype(jnp.float32)

    causal = jnp.tril(jnp.ones((B_CHUNK, B_CHUNK), dtype=bool))[None, None, :, :, None]

    def step(state, inp):
        qb, kb, vb, gb = inp
        b = jnp.cumsum(gb, axis=2)
        diff = b[:, :, :, None, :] - b[:, :, None, :, :]
        decay = jnp.exp(jnp.where(causal, diff, -jnp.inf))
        scores = jnp.einsum('bhid,bhjd,bhijd->bhij', qb, kb, decay)
        o = (jnp.einsum('bhij,bhje->bhie', scores, vb)
             + jnp.einsum('bhid,bhde->bhie', qb * jnp.exp(b), state))
        b_last = b[:, :, -1, :]
        state = (jnp.exp(b_last)[..., None] * state
                 + jnp.einsum('bhjd,bhje->bhde', kb * jnp.exp(b_last[:, :, None, :] - b), vb))
        return state, o

    state0 = jnp.zeros((bsz, nh, dk, dv), jnp.float32)
    _, o = lax.scan(step, state0, (chunks(q * dk ** -0.5), chunks(k), chunks(v), chunks(log_a)))
    return o.transpose(1, 0, 3, 2, 4).reshape(bsz, seq, nh, dv).astype(v.dtype)


def even_mixer(x, w_in, a_kv_norm_g, a_w_uk, a_w_uv, a_kidx_ln_g, a_kidx_ln_b, b_w_g2, b_b_g, b_norm_g, w_out):
    bsz, seq, _ = x.shape
    aq, ackv, aqi, aki, awi, bq, bk, bv, bg, br = split_cols(x @ w_in, EVEN_SPLITS)
    a_out = dsa_attention(aq.reshape(bsz, seq, A_HEADS, A_HEAD_DIM), ackv,
                          aqi.reshape(bsz, seq, IDX_HEADS, IDX_DIM), aki, awi,
                          a_kv_norm_g, a_w_uk, a_w_uv, a_kidx_ln_g, a_kidx_ln_b)
    log_a = jax.nn.log_sigmoid((bg @ b_w_g2 + b_b_g).astype(jnp.float32)) / B_GATE_TEMP
    heads = lambda t, d: t.reshape(bsz, seq, B_HEADS, d)
    o = gla_chunked(heads(bq, B_DK), heads(bk, B_DK), heads(bv, B_DV), heads(log_a, B_DK))
    b_out = (rms_norm(o, b_norm_g) * jax.nn.silu(heads(br, B_DV))).reshape(bsz, seq, B_HEADS * B_DV)
    return jnp.concatenate([a_out, b_out], axis=-1) @ w_out


def diff_attention(x, w_qkv, lam_q1, lam_k1, lam_q2, lam_k2, subln_g, w_out, lambda_init):
    bsz, seq, _ = x.shape
    f32 = jnp.float32
    q, k, v = split_cols(x @ w_qkv, (C_MIX, C_MIX, C_MIX))
    q = q.reshape(bsz, seq, C_HEADS, 2, C_QK_DIM)
    k = k.reshape(bsz, seq, C_HEADS, 2, C_QK_DIM)
    v = v.reshape(bsz, seq, C_HEADS, C_V_DIM)
    lam = (jnp.exp(jnp.sum(lam_q1.astype(f32) * lam_k1.astype(f32)))
           - jnp.exp(jnp.sum(lam_q2.astype(f32) * lam_k2.astype(f32))) + lambda_init)
    key_pos = jnp.arange(seq)

    def block(args):
        qb, start = args
        qpos = start + jnp.arange(Q_BLOCK)
        s = jnp.einsum('bqhmd,bshmd->bhmqs', qb, k).astype(f32) * C_QK_DIM ** -0.5
        s = jnp.where(key_pos[None, :] <= qpos[:, None], s, -jnp.inf)
        p = jax.nn.softmax(s, axis=-1)
        w = (p[:, :, 0] - lam * p[:, :, 1]).astype(v.dtype)
        return jnp.einsum('bhqs,bshe->bqhe', w, v)

    starts = jnp.arange(seq // Q_BLOCK) * Q_BLOCK
    o = lax.map(block, (query_blocks(q), starts)).swapaxes(0, 1).reshape(bsz, seq, C_HEADS, C_V_DIM)
    o = rms_norm(o, subln_g) * (1.0 - lambda_init)
    return o.reshape(bsz, seq, C_MIX) @ w_out


def swiglu(x, w_gate, w_up, w_down):
    return (jax.nn.silu(x @ w_gate) * (x @ w_up)) @ w_down


def moe_swiglu(x, w_router, b_router, w_gate, w_up, w_down):
    logits = (x @ w_router).astype(jnp.float32) + b_router
    top_vals, top_idx = lax.top_k(logits, TOP_K)
    gates = jax.nn.softmax(top_vals, axis=-1)
    combine = jnp.sum(jax.nn.one_hot(top_idx, N_EXPERTS, dtype=jnp.float32) * gates[..., None], axis=-2)
    y = jnp.zeros_like(x)
    for e in range(N_EXPERTS):
        y = y + combine[..., e:e + 1].astype(x.dtype) * swiglu(x, w_gate[e], w_up[e], w_down[e])
    return y


def setup_inputs(seed: int = 0) -> dict:
    key = jax.random.key(seed)
    keys = list(jax.random.split(key, 48))
    D = D_MODEL

    def nrm(shape, scale):
        return scale * jax.random.normal(keys.pop(), shape, jnp.float32)

    def gain(shape):
        return 1.0 + nrm(shape, 0.02)

    return {
        'x': nrm((BATCH, SEQ, D), 1.0),
        'ev_w_in': nrm((N_EVEN, D, EVEN_IN), D ** -0.5),
        'ev_a_kv_norm_g': gain((N_EVEN, A_KV_LATENT)),
        'ev_a_w_uk': nrm((N_EVEN, A_KV_LATENT, A_HEAD_DIM), A_KV_LATENT ** -0.5),
        'ev_a_w_uv': nrm((N_EVEN, A_KV_LATENT, A_HEAD_DIM), A_KV_LATENT ** -0.5),
        'ev_a_kidx_ln_g': gain((N_EVEN, IDX_DIM)),
        'ev_a_kidx_ln_b': nrm((N_EVEN, IDX_DIM), 0.02),
        'ev_b_w_g2': nrm((N_EVEN, B_GATE_RANK, B_HEADS * B_DK), B_GATE_RANK ** -0.5),
        'ev_b_b_g': nrm((N_EVEN, B_HEADS * B_DK), 0.1),
        'ev_b_norm_g': gain((N_EVEN, B_HEADS, B_DV)),
        'ev_w_out': nrm((N_EVEN, EVEN_MIX, D), DN_BETA * EVEN_MIX ** -0.5),
        'ev_ln1_g': gain((N_EVEN, D)),
        'ev_ln1_b': nrm((N_EVEN, D), 0.02),
        'ev_ffn_w_gate': nrm((N_EVEN, D, D_FF), D ** -0.5),
        'ev_ffn_w_up': nrm((N_EVEN, D, D_FF), D ** -0.5),
        'ev_ffn_w_down': nrm((N_EVEN, D_FF, D), DN_BETA * D_FF ** -0.5),
        'ev_ln2_g': gain((N_EVEN, D)),
        'ev_ln2_b': nrm((N_EVEN, D), 0.02),
        'od_w_qkv': nrm((N_ODD, D, 3 * C_MIX), D ** -0.5),
        'od_lam_q1': nrm((N_ODD, C_QK_DIM), 0.1),
        'od_lam_k1': nrm((N_ODD, C_QK_DIM), 0.1),
        'od_lam_q2': nrm((N_ODD, C_QK_DIM), 0.1),
        'od_lam_k2': nrm((N_ODD, C_QK_DIM), 0.1),
        'od_subln_g': gain((N_ODD, C_V_DIM)),
        'od_w_out': nrm((N_ODD, C_MIX, D), DN_BETA * C_MIX ** -0.5),
        'od_ln1_g': gain((N_ODD, D)),
        'od_ln1_b': nrm((N_ODD, D), 0.02),
        'od_router_w': nrm((N_ODD, D, N_EXPERTS), D ** -0.5),
        'od_router_b': nrm((N_ODD, N_EXPERTS), 0.01),
        'od_moe_w_gate': nrm((N_ODD, N_EXPERTS, D, D_FF_EXPERT), D ** -0.5),
        'od_moe_w_up': nrm((N_ODD, N_EXPERTS, D, D_FF_EXPERT), D ** -0.5),
        'od_moe_w_down': nrm((N_ODD, N_EXPERTS, D_FF_EXPERT, D), DN_BETA * D_FF_EXPERT ** -0.5),
        'od_ln2_g': gain((N_ODD, D)),
        'od_ln2_b': nrm((N_ODD, D), 0.02),
    }


def reference(x, ev_w_in, ev_a_kv_norm_g, ev_a_w_uk, ev_a_w_uv, ev_a_kidx_ln_g, ev_a_kidx_ln_b,
              ev_b_w_g2, ev_b_b_g, ev_b_norm_g, ev_w_out, ev_ln1_g, ev_ln1_b,
              ev_ffn_w_gate, ev_ffn_w_up, ev_ffn_w_down, ev_ln2_g, ev_ln2_b,
              od_w_qkv, od_lam_q1, od_lam_k1, od_lam_q2, od_lam_k2, od_subln_g, od_w_out,
              od_ln1_g, od_ln1_b, od_router_w, od_router_b,
              od_moe_w_gate, od_moe_w_up, od_moe_w_down, od_ln2_g, od_ln2_b):
    for i in range(DEPTH):
        j = i // 2
        if i % 2 == 0:
            h = even_mixer(x, ev_w_in[j], ev_a_kv_norm_g[j], ev_a_w_uk[j], ev_a_w_uv[j],
                           ev_a_kidx_ln_g[j], ev_a_kidx_ln_b[j], ev_b_w_g2[j], ev_b_b_g[j],
                           ev_b_norm_g[j], ev_w_out[j])
            x = layer_norm(DN_ALPHA * x + h, ev_ln1_g[j], ev_ln1_b[j])
            h = swiglu(x, ev_ffn_w_gate[j], ev_ffn_w_up[j], ev_ffn_w_down[j])
            x = layer_norm(DN_ALPHA * x + h, ev_ln2_g[j], ev_ln2_b[j])
        else:
            lambda_init = 0.8 - 0.6 * math.exp(-0.3 * i)
            h = diff_attention(x, od_w_qkv[j], od_lam_q1[j], od_lam_k1[j], od_lam_q2[j], od_lam_k2[j],
                               od_subln_g[j], od_w_out[j], lambda_init)
            x = layer_norm(DN_ALPHA * x + h, od_ln1_g[j], od_ln1_b[j])
            h = moe_swiglu(x, od_router_w[j], od_router_b[j], od_moe_w_gate[j], od_moe_w_up[j], od_moe_w_down[j])
            x = layer_norm(DN_ALPHA * x + h, od_ln2_g[j], od_ln2_b[j])
    return x
```

```python
import math
import numpy as np
import ml_dtypes
from contextlib import ExitStack
import concourse.bass as bass
import concourse.mybir as mybir
from concourse.bass_utils import run_bass_kernel_spmd

F32 = mybir.dt.float32
BF16 = mybir.dt.bfloat16
U8 = mybir.dt.uint8
ALU = mybir.AluOpType
AF = mybir.ActivationFunctionType
AX = mybir.AxisListType

D = 1024
DC = 8
DEPTH = 4
ALPHA = (2.0 * DEPTH) ** 0.25
LN_EPS = 1e-5
RMS_EPS = 1e-6
DFF = 2816
FC = 22
NEXP = 8
EVEN_IN = 2776


class Res:
    __slots__ = ("ws", "rs", "name")

    def __init__(self, name=""):
        self.ws = {}
        self.rs = {}
        self.name = name


class T:
    def __init__(self, ap, name="", res=None):
        self.ap = ap
        self.name = name
        self.res = res if res is not None else Res(name)

    def __getitem__(self, idx):
        return self.ap[idx]


def _res(x):
    return x.res if isinstance(x, T) else x


class KB:
    ENG = ("pe", "act", "dve", "pool", "sp")
    NDMA = 8

    def __init__(self, nc):
        self.nc = nc
        self.stack = ExitStack()
        self.prog = {e: [] for e in self.ENG}
        self.cnt = {e: 0 for e in self.ENG}
        self.seen = {e: {} for e in self.ENG}
        self.sems = {e: self.stack.enter_context(nc.semaphore("s_" + e)) for e in self.ENG}
        self.dsem, self.dcnt, self.dnext = {}, {}, {}
        for q in ("sp", "pool", "act", "bulk"):
            self.dsem[q] = [self.stack.enter_context(nc.semaphore("d_%s%d" % (q, i))) for i in range(self.NDMA)]
            self.dcnt[q] = [0] * self.NDMA
            self.dnext[q] = 0
        self.nalloc = 0
        self.nins = 0

    def sb(self, shape, dtype, name=None):
        self.nalloc += 1
        name = name or "t%d" % self.nalloc
        h = self.stack.enter_context(self.nc.sbuf_tensor(name, list(shape), dtype))
        return T(h[tuple(slice(None) for _ in shape)], name)

    def ps(self, shape, dtype, name=None):
        self.nalloc += 1
        name = name or "p%d" % self.nalloc
        h = self.stack.enter_context(self.nc.psum_tensor(name, list(shape), dtype))
        return T(h[tuple(slice(None) for _ in shape)], name)

    def dram(self, name, shape, dtype, kind="Internal"):
        h = self.nc.dram_tensor(name, list(shape), dtype, kind=kind)
        return T(h[tuple(slice(None) for _ in shape)], name)

    def _waits(self, eng, reads, writes):
        need = {}
        seen = self.seen[eng]

        def add(d):
            for key, (val, src) in d.items():
                if src == "pe" and eng == "pe":
                    continue
                if seen.get(key, 0) >= val:
                    continue
                if need.get(key, 0) < val:
                    need[key] = val

        for r in reads:
            add(_res(r).ws)
        for w in writes:
            w = _res(w)
            add(w.ws)
            add(w.rs)
        out = []
        for key, val in need.items():
            seen[key] = val
            out.append((key, val))
        return out

    def _commit(self, ev, reads, writes):
        key, val, src = ev
        for r in reads:
            _res(r).rs[key] = (val, src)
        for w in writes:
            w = _res(w)
            w.ws[key] = (val, src)
            w.rs = {}

    def _semof(self, key):
        if isinstance(key, str):
            return self.sems[key]
        return self.dsem[key[0]][key[1]]

    def op(self, eng, fn, reads=(), writes=()):
        waits = self._waits(eng, reads, writes)
        self.cnt[eng] += 1
        ev = (eng, self.cnt[eng], eng)
        self.prog[eng].append((waits, fn, (eng, 1)))
        self._commit(ev, reads, writes)
        self.nins += 1
        return ev

    def dma(self, q, fn, reads=(), writes=(), ring=None):
        ring = ring or q
        i = self.dnext[ring]
        self.dnext[ring] = (i + 1) % self.NDMA
        key = (ring, i)
        waits = self._waits(q, reads, writes)
        prev = self.dcnt[ring][i]
        if prev and self.seen[q].get(key, 0) < prev:
            self.seen[q][key] = prev
            waits.append((key, prev))
        self.dcnt[ring][i] = prev + 16
        ev = (key, prev + 16, "dma")
        self.prog[q].append((waits, fn, (key, 16)))
        self._commit(ev, reads, writes)
        self.nins += 1
        return ev

    def barrier(self, full=False):
        for e in self.ENG:
            waits = []
            for f in self.ENG:
                if f != e and self.cnt[f] and self.seen[e].get(f, 0) < self.cnt[f]:
                    self.seen[e][f] = self.cnt[f]
                    waits.append((f, self.cnt[f]))
            if self.cnt[e] and e != "pe" and self.seen[e].get(e, 0) < self.cnt[e]:
                self.seen[e][e] = self.cnt[e]
                waits.append((e, self.cnt[e]))
            for q in self.dsem:
                if q == "bulk" and not full:
                    continue
                for i in range(self.NDMA):
                    v = self.dcnt[q][i]
                    if v and self.seen[e].get((q, i), 0) < v:
                        self.seen[e][(q, i)] = v
                        waits.append(((q, i), v))
            if waits:
                self.prog[e].append((waits, None, None))

    def emit(self):
        nc = self.nc
        with nc.Block() as block:
            def run(e, name):
                for waits, fn, inc in self.prog[name]:
                    for key, val in waits:
                        e.wait_ge(self._semof(key), val)
                    if fn is not None:
                        fn(e).then_inc(self._semof(inc[0]), inc[1])

            @block.tensor
            def _(e):
                run(e, "pe")

            @block.scalar
            def _(e):
                run(e, "act")

            @block.vector
            def _(e):
                run(e, "dve")

            @block.gpsimd
            def _(e):
                run(e, "pool")

            @block.sync
            def _(e):
                run(e, "sp")
        self.stack.close()


class Arena:
    def __init__(self, kb, nbytes):
        self.kb = kb
        self.t = kb.sb([128, nbytes], U8, name="arena")
        self.n = nbytes
        self.off = 0

    def reset(self):
        self.kb.barrier()
        self.off = 0

    def alloc(self, shape, dtype, name=""):
        nb = 4 if dtype == F32 else 2
        n = int(np.prod(shape[1:])) * nb
        n_al = (n + 63) // 64 * 64
        assert self.off + n_al <= self.n, "arena overflow %s %d+%d>%d" % (name, self.off, n_al, self.n)
        ap = self.t.ap[:, self.off:self.off + n].bitcast(dtype)
        self.off += n_al
        if len(shape) == 3:
            ap = ap.rearrange("p (a b) -> p a b", b=shape[2])
        elif len(shape) == 4:
            ap = ap.rearrange("p (a b c) -> p a b c", b=shape[2], c=shape[3])
        if shape[0] < 128:
            ap = ap[0:shape[0]]
        return T(ap, name)


class Prog:
    def __init__(self, S, NSEQ, NL, dbg=None):
        self.S, self.NSEQ, self.NL = S, NSEQ, NL
        self.N = S * NSEQ
        self.NT = S // 128
        self.NB = S // 512
        self.n_sel = min(256, S // 4)
        self.dbg = dbg
        nc = self.nc = bass.Bass("TRN2", target_bir_lowering=False)
        kb = self.kb = KB(nc)
        N = self.N
        self.inp = {}

        def ext(name, shape, dt=F32):
            self.inp[name] = kb.dram(name, shape, dt, kind="ExternalInput")
            return self.inp[name]

        ext("x", [N, D])
        ext("cst", [128, 1024])
        ext("ev_w_in", [2, D, EVEN_IN]); ext("ev_a_kv_norm_g", [2, 128]); ext("ev_a_w_uk", [2, 128, 64]); ext("ev_a_w_uv", [2, 128, 64])
        ext("ev_a_kidx_ln_g", [2, 64]); ext("ev_a_kidx_ln_b", [2, 64]); ext("ev_b_w_g2", [2, 16, 256]); ext("ev_b_b_g", [2, 256])
        ext("ev_b_norm_g", [2, 512]); ext("ev_w_out", [2, D, D]); ext("ev_ln1_g", [2, D]); ext("ev_ln1_b", [2, D])
        ext("ev_ffn_w_gate", [2, D, DFF]); ext("ev_ffn_w_up", [2, D, DFF]); ext("ev_ffn_w_down", [2, DFF, D])
        ext("ev_ln2_g", [2, D]); ext("ev_ln2_b", [2, D])
        ext("od_w_qkv", [2, D, 3072]); ext("od_lam", [2, 4, 64]); ext("od_subln_g", [2, 128]); ext("od_w_out", [2, D, D])
        ext("od_ln1_g", [2, D]); ext("od_ln1_b", [2, D]); ext("od_router_w", [2, D, 8]); ext("od_router_b", [2, 8])
        if NL > 1:
            ext("od_moe_w_gate", [2, NEXP, D, DFF]); ext("od_moe_w_up", [2, NEXP, D, DFF]); ext("od_moe_w_down", [2, NEXP, DFF, D])
        ext("od_ln2_g", [2, D]); ext("od_ln2_b", [2, D])
        self.out = kb.dram("out", [N, D], F32, kind="ExternalOutput")
        self.XA = kb.dram("XA", [N, D], F32)
        self.XB = kb.dram("XB", [N, D], F32)
        self.XT = [kb.dram("XT0", [D, N], BF16), kb.dram("XT1", [D, N], BF16)]
        self.MIXD = kb.dram("MIXD", [N, D], BF16)
        NG = self.NG = (2 * N) // 512 + NEXP
        def table(name):
            full = [kb.dram("%s%d" % (name, j), [(NEXP + 1) * 1408, 2048], BF16) for j in range(2)]
            parts = [[T(full[j].ap[sl * 1408:(sl + 1) * 1408, :], "%s%d_%d" % (name, j, sl)) for sl in range(NEXP + 1)] for j in range(2)]
            return full, parts
        self.TgF, self.Tg = table("Tg")
        self.TuF, self.Tu = table("Tu")
        self.TdF, self.Td = table("Td")
        self.XS = kb.dram("XS", [NG * 512, D], BF16)
        self.YS = kb.dram("YS", [NG * 512, D], F32)
        self.PB = [kb.ps([128, 512], F32, name="bank%d" % i) for i in range(8)]
        self.cstf = kb.sb([128, 1024], F32, name="cstf")
        self.identb = kb.sb([128, 128], BF16, name="identb")
        self.trib = kb.sb([128, 128], BF16, name="trib")
        self.trigb = kb.sb([128, 64], BF16, name="trigb")
        self.zb = kb.sb([128, 512], BF16, name="zb")
        NTT = self.NTT = NSEQ * self.NT
        self.comb = kb.sb([128, NTT, 8], F32, name="comb")
        self.sel12 = kb.sb([128, NTT, 16], F32, name="sel12")
        self.gates = kb.sb([128, NTT, 2], F32, name="gates")
        self.pidx = kb.sb([128, NTT, 2], mybir.dt.int32, name="pidx")
        self.idxq = kb.sb([128, NG, 11], mybir.dt.int32, name="idxq")
        self.onesb = kb.sb([128, 128], BF16, name="onesb")
        self.onesf = kb.sb([128, 8], F32, name="onesf")
        self.ustb = kb.sb([128, 128], BF16, name="ustb")
        self.ar = Arena(kb, 192 * 1024)
        self.identf = T(self.cstf[:, 0:128], "identf", self.cstf.res)
        self.cbias = T(self.cstf[:, 128:256], "cbias", self.cstf.res)
        self.rst = T(self.cstf[:, 384:896], "rst", self.cstf.res)
        kb.dma("sp", lambda e: e.dma_start(out=self.cstf[:, :], in_=self.inp["cst"][:, :]), reads=[self.inp["cst"]], writes=[self.cstf])
        kb.op("dve", lambda e: e.tensor_copy(out=self.identb[:, :], in_=self.cstf[:, 0:128]), reads=[self.cstf], writes=[self.identb])
        kb.op("dve", lambda e: e.tensor_copy(out=self.trib[:, :], in_=self.cstf[:, 256:384]), reads=[self.cstf], writes=[self.trib])
        kb.op("dve", lambda e: e.tensor_copy(out=self.trigb[:, :], in_=self.cstf[:, 896:960]), reads=[self.cstf], writes=[self.trigb])
        kb.op("pool", lambda e: e.memset(self.zb[:, :], 0.0), writes=[self.zb])
        kb.op("pool", lambda e: e.memset(self.onesb[:, :], 1.0), writes=[self.onesb])
        kb.op("pool", lambda e: e.memset(self.onesf[:, :], 1.0), writes=[self.onesf])
        kb.op("dve", lambda e: e.tensor_tensor(out=self.ustb[:, :], in0=self.cstf[:, 256:384], in1=self.cstf[:, 0:128], op=ALU.subtract),
              reads=[self.cstf], writes=[self.ustb])
        self.conv_q = []

    def mm(self, out, lhsT, rhs, start, stop, reads, writes, skip=False):
        self.kb.op("pe", lambda e: e.matmul(out, lhsT=lhsT, rhs=rhs, start=start, stop=stop, skip_group_check=skip), reads=reads, writes=writes)

    def tp(self, out, in_, ident, reads, writes):
        self.kb.op("pe", lambda e: e.transpose(out, in_, ident), reads=reads, writes=writes)

    def act(self, out, in_, func, reads, writes, **kw):
        self.kb.op("act", lambda e: e.activation(out=out, in_=in_, func=func, **kw), reads=reads, writes=writes)

    def tt(self, eng, out, in0, in1, op, reads, writes):
        self.kb.op(eng, lambda e: e.tensor_tensor(out=out, in0=in0, in1=in1, op=op), reads=reads, writes=writes)

    def ts(self, eng, out, in0, s1, s2, op0, op1, reads, writes, accum=None):
        if accum is None:
            self.kb.op(eng, lambda e: e.tensor_scalar(out=out, in0=in0, scalar1=s1, scalar2=s2, op0=op0, op1=op1), reads=reads, writes=writes)
        else:
            self.kb.op(eng, lambda e: e.tensor_scalar(out=out, in0=in0, scalar1=s1, scalar2=s2, op0=op0, op1=op1, accum_out=accum), reads=reads, writes=writes)

    def stt(self, out, in0, scalar, in1, op0, op1, reads, writes):
        self.kb.op("dve", lambda e: e.scalar_tensor_tensor(out=out, in0=in0, scalar=scalar, in1=in1, op0=op0, op1=op1), reads=reads, writes=writes)

    def cp(self, eng, out, in_, reads, writes):
        if eng == "act":
            self.kb.op("act", lambda e: e.copy(out=out, in_=in_), reads=reads, writes=writes)
        else:
            self.kb.op(eng, lambda e: e.tensor_copy(out=out, in_=in_), reads=reads, writes=writes)

    def ld(self, q, out, in_, reads, writes):
        return self.kb.dma(q, lambda e: e.dma_start(out=out, in_=in_), reads=reads, writes=writes)

    def bcast_load(self, dst, src_row, reads):
        self.ld("sp", dst[:, :], src_row.partition_broadcast(128), reads, [dst])

    def load_w_cast(self, dst, src2d, ncols, reads):
        step = 1024
        for c0 in range(0, ncols, step):
            c1 = min(ncols, c0 + step)
            self.ld("pool", dst[:, :, c0:c1], src2d[:, c0:c1].rearrange("(c p) n -> p c n", p=128), reads, [dst])

    def queue_conversions(self):
        inp = self.inp
        for j in range(2):
            srcs = []
            if 2 * j < self.NL:
                srcs.append((NEXP, inp["ev_ffn_w_gate"][j], inp["ev_ffn_w_up"][j], inp["ev_ffn_w_down"][j]))
            if 2 * j + 1 < self.NL:
                for e in range(NEXP):
                    srcs.append((e, inp["od_moe_w_gate"][j, e], inp["od_moe_w_up"][j, e], inp["od_moe_w_down"][j, e]))
            for (slot, g, u, d) in srcs:
                for (tab, src) in ((self.Tg, g), (self.Tu, u)):
                    for c0 in (0, 4):
                        self.conv_q.append((j, slot, tab[j][slot],
                                            tab[j][slot][:, :].rearrange("(p q) (c f) -> p q c f", q=11, f=256)[:, :, c0:c0 + 4, :],
                                            src.rearrange("(c p) (q f) -> p q c f", p=128, f=256)[:, :, c0:c0 + 4, :]))
                self.conv_q.append((j, slot, self.Td[j][slot],
                                    self.Td[j][slot][:, :].rearrange("(p q) (a n) -> p q a n", q=11, n=1024),
                                    d.rearrange("(q a p) n -> p q a n", a=2, p=128)))

    def pump(self, n=1, upto=None):
        while self.conv_q and (n > 0 or (upto is not None and (self.conv_q[0][0], self.conv_q[0][1] != NEXP) <= upto)):
            j, slot, t, dst, src = self.conv_q.pop(0)
            self.kb.dma("pool", lambda e, dst=dst, src=src: e.dma_start(out=dst, in_=src), reads=[], writes=[t], ring="bulk")
            n -= 1

    def ln_tile(self, hsrc, hres, Xold, Xnew, XTnew, n0, gbc, bbc, bufs, router=None):
        kb = self.kb
        xo, z, st, xtb = bufs["xo"], bufs["z"], bufs["st"], bufs["xtb"]
        self.ld("sp", xo[:, :], Xold[n0:n0 + 128, :], [Xold], [xo])
        for h in range(2):
            self.stt(z[:, h * 512:(h + 1) * 512], xo[:, h * 512:(h + 1) * 512], ALPHA, hsrc[h], ALU.mult, ALU.add, [xo] + hres, [z])
        for h in range(2):
            kb.op("dve", lambda e, h=h: e.bn_stats(out=st[:, h * 6:(h + 1) * 6], in_=z[:, h * 512:(h + 1) * 512]), reads=[z], writes=[st])
        kb.op("dve", lambda e: e.bn_aggr(out=st[:, 12:14], in_=st[:, 0:12]), reads=[st], writes=[st])
        self.act(st[:, 14:15], st[:, 13:14], AF.Sqrt, [st], [st], bias=bufs["eps"][:, 0:1], scale=1.0)
        kb.op("dve", lambda e: e.reciprocal(out=st[:, 15:16], in_=st[:, 14:15]), reads=[st], writes=[st])
        self.ts("dve", st[:, 16:17], st[:, 12:13], -1.0, st[:, 15:16], ALU.mult, ALU.mult, [st], [st])
        self.act(xo[:, :], z[:, :], AF.Identity, [z, st], [xo], scale=st[:, 15:16], bias=st[:, 16:17])
        self.tt("dve", xo[:, :], xo[:, :], gbc[:, :], ALU.mult, [xo, gbc], [xo])
        self.tt("pool", xo[:, :], xo[:, :], bbc[:, :], ALU.add, [xo, bbc], [xo])
        self.ld("sp", Xnew[n0:n0 + 128, :], xo[:, :], [xo], [Xnew])
        if XTnew is not None:
            pb = [self.PB[6], self.PB[7]]
            for c in range(DC):
                self.tp(pb[c // 4][:, (c % 4) * 128:(c % 4 + 1) * 128], xo[:, c * 128:(c + 1) * 128], self.identf[:, :], [xo, self.identf], [pb[c // 4]])
            for h in range(2):
                self.cp("act", xtb[:, h * 4:(h + 1) * 4, :], pb[h][:, :].rearrange("p (c t) -> p c t", t=128), [pb[h]], [xtb])
            self.ld("sp", XTnew[:, n0:n0 + 128].rearrange("(c p) t -> p c t", p=128), xtb[:, :, :], [xtb], [XTnew])
            if router is not None:
                self.router_tile(pb, router, n0, bufs)

    def router_tile(self, pb, router, n0, bufs):
        kb = self.kb
        wrh, wrl, brb = router
        xtl, st, xtb = bufs["xtl"], bufs["st"], bufs["xtb"]
        for h in range(2):
            self.tt("dve", xtl[:, h * 4:(h + 1) * 4, :], pb[h][:, :].rearrange("p (c t) -> p c t", t=128), xtb[:, h * 4:(h + 1) * 4, :],
                    ALU.subtract, [pb[h], xtb], [xtl])
        lg = self.PB[5]
        k = 0
        for c in range(DC):
            for (xa, wa) in ((xtb, wrh), (xtb, wrl), (xtl, wrh)):
                self.mm(lg[:, 0:8], xa[:, c, :], wa[:, c, :], k == 0, k == 3 * DC - 1, [xa, wa], [lg])
                k += 1
        tix = n0 // 128
        L = st[:, 24:32]
        self.tt("dve", L, lg[:, 0:8], brb[:, :], ALU.add, [lg, brb], [st])
        kb.op("dve", lambda e: e.max(out=st[:, 32:40], in_=L), reads=[st], writes=[st])
        self.tt("dve", st[:, 40:41], st[:, 33:34], st[:, 32:33], ALU.subtract, [st], [st])
        self.act(st[:, 41:42], st[:, 40:41], AF.Exp, [st], [st])
        self.ts("dve", st[:, 42:43], st[:, 41:42], 1.0, None, ALU.add, ALU.bypass, [st], [st])
        kb.op("dve", lambda e: e.reciprocal(out=st[:, 43:44], in_=st[:, 42:43]), reads=[st], writes=[st])
        self.tt("dve", st[:, 44:45], st[:, 41:42], st[:, 43:44], ALU.mult, [st], [st])
        self.ts("dve", self.sel12[:, tix, 0:8], L, st[:, 32:33], None, ALU.is_equal, ALU.bypass, [st], [self.sel12])
        self.ts("dve", self.sel12[:, tix, 8:16], L, st[:, 33:34], None, ALU.is_equal, ALU.bypass, [st], [self.sel12])
        self.cp("dve", self.gates[:, tix, :], st[:, 43:45], [st], [self.gates])

    def ln_bufs(self):
        ar = self.ar
        b = {"xo": ar.alloc([128, D], F32, "xo"), "z": ar.alloc([128, D], F32, "z"), "st": ar.alloc([128, 64], F32, "st"),
             "xtb": ar.alloc([128, DC, 128], BF16, "xtb"), "eps": ar.alloc([128, 1], F32, "eps"),
             "gbc": ar.alloc([128, D], F32, "gbc"), "bbc": ar.alloc([128, D], F32, "bbc")}
        self.kb.op("pool", lambda e: e.memset(b["eps"][:, :], LN_EPS), writes=[b["eps"]])
        return b

    def ffn_setup(self):
        ar = self.ar
        f = {"xT": [ar.alloc([128, DC, 512], BF16, "xT%d" % i) for i in range(2)],
             "hT": ar.alloc([128, FC, 512], BF16, "hT"),
             "ring": [ar.alloc([128, 2048], BF16, "ring%d" % i) for i in range(12)],
             "sg": [ar.alloc([128, 512], BF16, "sg%d" % i) for i in range(2)],
             "ri": 0}
        return f

    def ffn_block(self, f, xT, load):
        PB = self.PB
        hT = f["hT"]
        k = 0
        for q in range(11):
            gs = f["ring"][f["ri"] % 12]; f["ri"] += 1
            us = f["ring"][f["ri"] % 12]; f["ri"] += 1
            load("g", q, gs)
            load("u", q, us)
            gv = gs[:, :].rearrange("p (c n) -> p c n", n=256)
            uv = us[:, :].rearrange("p (c n) -> p c n", n=256)
            for fl in range(2):
                fi = 2 * q + fl
                pg, pu = PB[(k % 2) * 2], PB[(k % 2) * 2 + 1]
                for c in range(DC):
                    self.mm(pg[:, :], gv[:, c, fl * 128:(fl + 1) * 128], xT[:, c, :], c == 0, c == DC - 1, [gs, xT], [pg])
                for c in range(DC):
                    self.mm(pu[:, :], uv[:, c, fl * 128:(fl + 1) * 128], xT[:, c, :], c == 0, c == DC - 1, [us, xT], [pu])
                sg = f["sg"][k % 2]
                self.act(sg[:, :], pg[:, :], AF.Silu, [pg], [sg])
                self.tt("dve", hT[:, fi, :], sg[:, :], pu[:, :], ALU.mult, [sg, pu], [hT])
                k += 1
        for q in range(11):
            ds = f["ring"][f["ri"] % 12]; f["ri"] += 1
            load("d", q, ds)
            dv = ds[:, :].rearrange("p (a n) -> p a n", n=D)
            for a_ in range(2):
                fi = 2 * q + a_
                for u in range(4):
                    for h in range(2):
                        self.mm(PB[2 * u + h][:, :], hT[:, fi, u * 128:(u + 1) * 128], dv[:, a_, h * 512:(h + 1) * 512],
                                fi == 0, fi == FC - 1, [hT, ds], [PB[2 * u + h]])

    def static_loader(self, j, slot):
        tabs = {"g": self.Tg[j][slot], "u": self.Tu[j][slot], "d": self.Td[j][slot]}

        def load(kind, q, dst):
            t = tabs[kind]
            self.ld("sp", dst[:, :], t[:, :].rearrange("(p q) n -> p q n", q=11)[:, q, :], [t], [dst])
        return load

    def ffn_phase(self, j, Xold, Xnew, XTold, XTnew, lng, lnb):
        kb, ar = self.kb, self.ar
        self.pump(0, upto=(j, 0))
        ar.reset()
        f = self.ffn_setup()
        lb = self.ln_bufs()
        self.bcast_load(lb["gbc"], lng, [])
        self.bcast_load(lb["bbc"], lnb, [])
        acc = ar.alloc([128, 4, D], F32, "acc")
        load = self.static_loader(j, NEXP)
        for blk in range(self.N // 512):
            n0 = blk * 512
            xT = f["xT"][blk % 2]
            self.ld("sp", xT[:, :, :], XTold[:, n0:n0 + 512].rearrange("(c p) t -> p c t", p=128), [XTold], [xT])
            self.ffn_block(f, xT, load)
            self.pump(3)
            for u in range(4):
                for h in range(2):
                    self.cp("act" if h == 0 else "dve", acc[:, u, h * 512:(h + 1) * 512], self.PB[2 * u + h][:, :], [self.PB[2 * u + h]], [acc])
            for u in range(4):
                hs = [acc[:, u, 0:512], acc[:, u, 512:1024]]
                self.ln_tile(hs, [acc], Xold, Xnew, XTnew, n0 + u * 128, lb["gbc"], lb["bbc"], lb)

    def route(self, Xcur):
        kb, ar, PB = self.kb, self.ar, self.PB
        NTT, NG = self.NTT, self.NG
        I32 = mybir.dt.int32
        ar.reset()
        SEL = ar.alloc([128, NTT, 8], BF16, "SEL")
        RANK = ar.alloc([128, NTT, 8], F32, "RANK")
        carry = ar.alloc([128, 8], F32, "carry")
        w = ar.alloc([128, 256], F32, "rw")
        big = ar.alloc([128, NG, 11], F32, "rbig")
        t3 = ar.alloc([128, NTT, 8], F32, "rt3")
        pf = ar.alloc([128, NTT, 2], F32, "rpf")
        self.tt("dve", SEL[:, :, :], self.sel12[:, :, 0:8], self.sel12[:, :, 8:16], ALU.add, [self.sel12], [SEL])
        kb.op("pool", lambda e: e.memset(carry[:, :], 0.0), writes=[carry])
        for i in range(NTT):
            p = PB[i % 2]
            self.mm(p[:, 0:8], self.ustb[:, :], SEL[:, i, :], True, True, [self.ustb, SEL], [p])
            self.mm(p[:, 8:16], self.onesb[:, :], SEL[:, i, :], True, True, [self.onesb, SEL], [p])
            self.tt("dve", RANK[:, i, :], p[:, 0:8], carry[:, :], ALU.add, [p, carry], [RANK])
            self.tt("dve", carry[:, :], p[:, 8:16], carry[:, :], ALU.add, [p, carry], [carry])
        cst = self.cstf
        THR, GT, QC, P11 = cst[:, 960:968], cst[:, 968:968 + NG], cst[:, 992:1003], cst[:, 1003:1004]
        ge, pn, incl, off = w[:, 0:8], w[:, 8:16], w[:, 16:24], w[:, 24:32]
        cmp_ = w[:, 64:128].rearrange("p (e k) -> p e k", k=8)
        self.tt("dve", cmp_, carry[:, :].unsqueeze(2).to_broadcast([128, 8, 8]), THR.unsqueeze(1).to_broadcast([128, 8, 8]), ALU.is_gt, [carry, cst], [w])
        kb.op("dve", lambda e: e.tensor_reduce(out=ge, in_=cmp_, axis=AX.X, op=ALU.add), reads=[w], writes=[w])
        self.ts("dve", pn, ge, 512.0, None, ALU.mult, ALU.bypass, [w], [w])
        kb.op("dve", lambda e: e.tensor_tensor_scan(out=incl, data0=self.onesf[:, 0:8], data1=pn, initial=0.0, op0=ALU.mult, op1=ALU.add), reads=[w, self.onesf], writes=[w])
        self.tt("dve", off, incl, pn, ALU.subtract, [w], [w])
        self.tt("dve", t3[:, :, :], RANK[:, :, :], off.unsqueeze(1).to_broadcast([128, NTT, 8]), ALU.add, [RANK, w], [t3])
        for k in range(2):
            self.tt("dve", RANK[:, :, :], t3[:, :, :], self.sel12[:, :, k * 8:(k + 1) * 8], ALU.mult, [t3, self.sel12], [RANK])
            kb.op("dve", lambda e, k=k: e.tensor_reduce(out=pf[:, :, k], in_=RANK[:, :, :], axis=AX.X, op=ALU.add), reads=[RANK], writes=[pf])
        self.cp("dve", self.pidx[:, :, :], pf[:, :, :], [pf], [self.pidx])
        cmp2 = big[:, :, 0:8]
        self.tt("dve", cmp2, GT.unsqueeze(2).to_broadcast([128, NG, 8]), incl.unsqueeze(1).to_broadcast([128, NG, 8]), ALU.is_ge, [cst, w], [big])
        eg = w[:, 32:32 + NG]
        kb.op("dve", lambda e: e.tensor_reduce(out=eg, in_=cmp2, axis=AX.X, op=ALU.add), reads=[big], writes=[w])
        self.ts("dve", eg, eg, 7.0, 1408.0, ALU.min, ALU.mult, [w], [w])
        self.ts("dve", eg, eg, P11, None, ALU.add, ALU.bypass, [w, cst], [w])
        self.tt("dve", big[:, :, :], eg.unsqueeze(2).to_broadcast([128, NG, 11]), QC.unsqueeze(1).to_broadcast([128, NG, 11]), ALU.add, [w, cst], [big])
        self.cp("dve", self.idxq[:, :, :], big[:, :, :], [big], [self.idxq])
        xos = [ar.alloc([128, D], F32, "sxo%d" % i) for i in range(2)]
        xbs = [ar.alloc([128, D], BF16, "sxb%d" % i) for i in range(2)]
        for i in range(NTT):
            xo, xb = xos[i % 2], xbs[i % 2]
            self.ld("sp", xo[:, :], Xcur[i * 128:(i + 1) * 128, :], [Xcur], [xo])
            self.cp("act", xb[:, :], xo[:, :], [xo], [xb])
            for k in range(2):
                self.kb.dma("pool", lambda e, xb=xb, i=i, k=k: e.indirect_dma_start(
                    out=self.XS[:, :], out_offset=bass.IndirectOffsetOnAxis(self.pidx[:, i, k:k + 1], 0), in_=xb[:, :], in_offset=None),
                    reads=[xb, self.pidx], writes=[self.XS])

    def moe_sparse(self, j, Xold, Xnew, XTnew, lng, lnb):
        kb, ar, PB = self.kb, self.ar, self.PB
        NG = self.NG
        self.pump(0, upto=(j, 1))
        ar.reset()
        f = self.ffn_setup()
        xss = [ar.alloc([128, 4, D], BF16, "xs%d" % i) for i in range(2)]
        ys = ar.alloc([128, 4, D], F32, "ys")
        tabs = {"g": self.Tg[j], "u": self.Tu[j], "d": self.Td[j]}
        fulls = {"g": self.TgF[j], "u": self.TuF[j], "d": self.TdF[j]}
        tres = [t for k in tabs for t in tabs[k][0:NEXP]]
        for g in range(NG):
            xs = xss[g % 2]
            xT = f["xT"][g % 2]
            self.ld("sp", xs[:, :, :], self.XS[g * 512:(g + 1) * 512, :].rearrange("(u p) d -> p u d", p=128), [self.XS], [xs])
            for u in range(4):
                pt = PB[4 + u]
                ptb = pt[:, :].bitcast(BF16)
                for c in range(DC):
                    self.tp(ptb[:, c * 128:(c + 1) * 128], xs[:, u, c * 128:(c + 1) * 128], self.identb[:, :], [xs, self.identb], [pt])
                self.cp("act" if u % 2 == 0 else "dve", xT[:, :, u * 128:(u + 1) * 128], ptb.rearrange("p (c t) -> p c t", t=128), [pt], [xT])

            def load(kind, q, dst, g=g):
                src = fulls[kind].ap[:, :]
                self.kb.dma("pool", lambda e, src=src, dst=dst, q=q: e.indirect_dma_start(
                    out=dst[:, :], out_offset=None, in_=src, in_offset=bass.IndirectOffsetOnAxis(self.idxq[:, g, q:q + 1], 0)),
                    reads=tres + [self.idxq], writes=[dst])
            self.ffn_block(f, xT, load)
            for u in range(4):
                for h in range(2):
                    self.cp("act" if h == 0 else "dve", ys[:, u, h * 512:(h + 1) * 512], PB[2 * u + h][:, :], [PB[2 * u + h]], [ys])
            self.ld("sp", self.YS[g * 512:(g + 1) * 512, :].rearrange("(u p) d -> p u d", p=128), ys[:, :, :], [ys], [self.YS])
        ar.reset()
        lb = self.ln_bufs()
        self.bcast_load(lb["gbc"], lng, [])
        self.bcast_load(lb["bbc"], lnb, [])
        y1s = [ar.alloc([128, D], F32, "y1_%d" % i) for i in range(2)]
        y2s = [ar.alloc([128, D], F32, "y2_%d" % i) for i in range(2)]
        for i in range(self.NTT):
            y1, y2 = y1s[i % 2], y2s[i % 2]
            for k, y in ((0, y1), (1, y2)):
                self.kb.dma("pool", lambda e, y=y, i=i, k=k: e.indirect_dma_start(
                    out=y[:, :], out_offset=None, in_=self.YS[:, :], in_offset=bass.IndirectOffsetOnAxis(self.pidx[:, i, k:k + 1], 0)),
                    reads=[self.YS, self.pidx], writes=[y])
            self.ts("dve", y1[:, :], y1[:, :], self.gates[:, i, 0:1], None, ALU.mult, ALU.bypass, [y1, self.gates], [y1])
            self.stt(y1[:, :], y2[:, :], self.gates[:, i, 1:2], y1[:, :], ALU.mult, ALU.add, [y2, self.gates, y1], [y1])
            self.ln_tile([y1[:, 0:512], y1[:, 512:1024]], [y1], Xold, Xnew, XTnew, i * 128, lb["gbc"], lb["bbc"], lb)

    def outproj_phase(self, w_out, Xold, Xnew, XTnew, lng, lnb, router=None):
        kb, ar = self.kb, self.ar
        ar.reset()
        lb = self.ln_bufs()
        self.bcast_load(lb["gbc"], lng, [])
        self.bcast_load(lb["bbc"], lnb, [])
        W = ar.alloc([128, DC, D], BF16, "wout")
        self.load_w_cast(W, w_out, D, [])
        rt = None
        if router is not None:
            wr = ar.alloc([128, DC, 8], F32, "wr")
            wrh = ar.alloc([128, DC, 8], BF16, "wrh")
            wrl = ar.alloc([128, DC, 8], BF16, "wrl")
            brb = ar.alloc([128, 8], F32, "brb")
            lb["xtl"] = ar.alloc([128, DC, 128], BF16, "xtl")
            self.ld("sp", wr[:, :, :], router[0].rearrange("(c p) n -> p c n", p=128), [], [wr])
            self.cp("dve", wrh[:, :, :], wr[:, :, :], [wr], [wrh])
            self.tt("dve", wrl[:, :, :], wr[:, :, :], wrh[:, :, :], ALU.subtract, [wr, wrh], [wrl])
            self.bcast_load(brb, router[1], [])
            rt = (wrh, wrl, brb)
        mixs = [ar.alloc([128, D], BF16, "mix%d" % i) for i in range(2)]
        mixT = [ar.alloc([128, DC, 128], BF16, "mixT%d" % i) for i in range(2)]
        PB = self.PB
        for t in range(self.N // 128):
            n0 = t * 128
            mx, mt = mixs[t % 2], mixT[t % 2]
            self.ld("sp", mx[:, :], self.MIXD[n0:n0 + 128, :], [self.MIXD], [mx])
            pt = PB[4 + (t % 2)]
            ptb = pt[:, :].bitcast(BF16)
            for c in range(DC):
                self.tp(ptb[:, c * 128:(c + 1) * 128], mx[:, c * 128:(c + 1) * 128], self.identb[:, :], [mx, self.identb], [pt])
            self.cp("act", mt[:, :, :], ptb.rearrange("p (c t) -> p c t", t=128), [pt], [mt])
            hb = [PB[(t % 2) * 2], PB[(t % 2) * 2 + 1]]
            for h in range(2):
                for c in range(DC):
                    self.mm(hb[h][:, :], mt[:, c, :], W[:, c, h * 512:(h + 1) * 512], c == 0, c == DC - 1, [mt, W], [hb[h]])
            self.ln_tile([hb[0][:, :], hb[1][:, :]], hb, Xold, Xnew, XTnew, n0, lb["gbc"], lb["bbc"], lb, router=rt)


    def init_xt(self, Xin, XTnew):
        ar = self.ar
        ar.reset()
        xos = [ar.alloc([128, D], F32, "ixo%d" % i) for i in range(2)]
        xtbs = [ar.alloc([128, DC, 128], BF16, "ixt%d" % i) for i in range(2)]
        for t in range(self.N // 128):
            n0 = t * 128
            xo, xtb = xos[t % 2], xtbs[t % 2]
            self.ld("sp", xo[:, :], Xin[n0:n0 + 128, :], [Xin], [xo])
            pb = [self.PB[(t % 2) * 2], self.PB[(t % 2) * 2 + 1]]
            for c in range(DC):
                self.tp(pb[c // 4][:, (c % 4) * 128:(c % 4 + 1) * 128], xo[:, c * 128:(c + 1) * 128], self.identf[:, :], [xo, self.identf], [pb[c // 4]])
            for h in range(2):
                self.cp("act" if h == 0 else "dve", xtb[:, h * 4:(h + 1) * 4, :], pb[h][:, :].rearrange("p (c t) -> p c t", t=128), [pb[h]], [xtb])
            self.ld("sp", XTnew[:, n0:n0 + 128].rearrange("(c p) t -> p c t", p=128), xtb[:, :, :], [xtb], [XTnew])

    def even_scratch(self):
        kb, N = self.kb, self.N
        if hasattr(self, "PQ"):
            return
        self.PQ = kb.dram("PQ", [2048, N], BF16)
        self.PBQK = kb.dram("PBQK", [512, N], F32)
        self.CNTd = kb.dram("CNTd", [128, N], BF16)
        self.KITd = kb.dram("KITd", [128, N], BF16)
        self.WId = kb.dram("WId", [N, 8], F32)
        self.BGTd = kb.dram("BGTd", [16, N], BF16)
        self.BVd = kb.dram("BVd", [N, 1024], BF16)

    def even_proj(self, j, XTold):
        kb, ar, PB = self.kb, self.ar, self.PB
        inp = self.inp
        self.even_scratch()
        ar.reset()
        WIN = ar.alloc([128, DC, EVEN_IN], BF16, "WIN")
        self.load_w_cast(WIN, inp["ev_w_in"][j], EVEN_IN, [])
        kvg = ar.alloc([128, 128], F32, "kvg"); self.bcast_load(kvg, inp["ev_a_kv_norm_g"][j:j + 1, :], [])
        klg = ar.alloc([128, 64], F32, "klg"); self.bcast_load(klg, inp["ev_a_kidx_ln_g"][j:j + 1, :], [])
        klb = ar.alloc([128, 64], F32, "klb"); self.bcast_load(klb, inp["ev_a_kidx_ln_b"][j:j + 1, :], [])
        epsr = ar.alloc([128, 2], F32, "epsr")
        kb.op("pool", lambda e: e.memset(epsr[:, 0:1], RMS_EPS), writes=[epsr])
        kb.op("pool", lambda e: e.memset(epsr[:, 1:2], LN_EPS), writes=[epsr])
        xTs = [ar.alloc([128, DC, 512], BF16, "xT%d" % i) for i in range(2)]
        pqs = [ar.alloc([128, 8, 512], BF16, "pqs%d" % i) for i in range(2)]
        pbs = [ar.alloc([128, 4, 512], F32, "pbs%d" % i) for i in range(2)]
        bgs = [ar.alloc([16, 512], BF16, "bgs%d" % i) for i in range(2)]
        cns = [ar.alloc([128, 512], BF16, "cns%d" % i) for i in range(2)]
        kis = [ar.alloc([128, 512], BF16, "kis%d" % i) for i in range(2)]
        wis = [ar.alloc([128, 4, 8], F32, "wis%d" % i) for i in range(2)]
        bvs = [ar.alloc([128, 4, 1024], BF16, "bvs%d" % i) for i in range(2)]
        st = ar.alloc([128, 32], F32, "st")
        junk = ar.alloc([128, 128], F32, "junk")
        cn = ar.alloc([128, 128], BF16, "cn")
        kin = ar.alloc([128, 64], F32, "kin")
        kin2 = ar.alloc([128, 128], BF16, "kin2")
        WSC = (8.0 ** -0.5) * (64.0 ** -0.5)
        for blk in range(self.N // 512):
            n0 = blk * 512
            b2 = blk % 2
            xT = xTs[b2]
            self.pump(2)
            self.ld("sp", xT[:, :, :], XTold[:, n0:n0 + 512].rearrange("(c p) t -> p c t", p=128), [XTold], [xT])
            fm = [(g * 128, 128, pqs[b2], g, 1.0) for g in range(4)] + [(640 + g * 128, 128, pqs[b2], 4 + g, 1.0) for g in range(4)] + \
                 [(1224 + g * 128, 128, pbs[b2], g, 0.125) for g in range(2)] + [(1480 + g * 128, 128, pbs[b2], 2 + g, 1.0) for g in range(2)] + \
                 [(2248, 16, bgs[b2], None, 1.0)]
            for gi, (c0, M, dst, slot, sc) in enumerate(fm):
                pb = PB[gi % 2]
                for c in range(DC):
                    self.mm(pb[0:M, :], WIN[:, c, c0:c0 + M], xT[:, c, :], c == 0, c == DC - 1, [WIN, xT], [pb])
                o = dst[0:M, :] if slot is None else dst[:, slot, :]
                if gi % 2 == 0:
                    self.act(o, pb[0:M, :], AF.Copy, [pb], [dst], scale=sc)
                else:
                    self.ts("dve", o, pb[0:M, :], sc, None, ALU.mult, ALU.bypass, [pb], [dst])
            self.ld("sp", self.PQ[0:1024, n0:n0 + 512].rearrange("(g p) t -> p g t", p=128), pqs[b2][:, :, :], [pqs[b2]], [self.PQ])
            self.ld("sp", self.PBQK[:, n0:n0 + 512].rearrange("(g p) t -> p g t", p=128), pbs[b2][:, :, :], [pbs[b2]], [self.PBQK])
            self.ld("sp", self.BGTd[:, n0:n0 + 512], bgs[b2][:, :], [bgs[b2]], [self.BGTd])
            for u in range(4):
                xs = lambda c: xT[:, c, u * 128:(u + 1) * 128]
                p2 = PB[2]
                for c in range(DC):
                    self.mm(p2[:, 0:128], xs(c), WIN[:, c, 512:640], c == 0, c == DC - 1, [xT, WIN], [p2])
                for c in range(DC):
                    self.mm(p2[:, 128:200], xs(c), WIN[:, c, 1152:1224], c == 0, c == DC - 1, [xT, WIN], [p2])
                self.act(junk[:, :], p2[:, 0:128], AF.Square, [p2], [junk, st], accum_out=st[:, 0:1])
                self.act(st[:, 1:2], st[:, 0:1], AF.Sqrt, [st, epsr], [st], scale=1.0 / 128, bias=epsr[:, 0:1])
                kb.op("dve", lambda e: e.reciprocal(out=st[:, 2:3], in_=st[:, 1:2]), reads=[st], writes=[st])
                self.stt(cn[:, :], p2[:, 0:128], st[:, 2:3], kvg[:, :], ALU.mult, ALU.mult, [p2, st, kvg], [cn])
                p3 = PB[3]
                p3b = p3[:, :].bitcast(BF16)
                self.tp(p3b[:, 0:128], cn[:, :], self.identb[:, :], [cn, self.identb], [p3])
                kb.op("dve", lambda e: e.bn_stats(out=st[:, 4:10], in_=p2[:, 128:192]), reads=[p2], writes=[st])
                kb.op("dve", lambda e: e.bn_aggr(out=st[:, 10:12], in_=st[:, 4:10]), reads=[st], writes=[st])
                self.act(st[:, 12:13], st[:, 11:12], AF.Sqrt, [st, epsr], [st], scale=1.0, bias=epsr[:, 1:2])
                kb.op("dve", lambda e: e.reciprocal(out=st[:, 13:14], in_=st[:, 12:13]), reads=[st], writes=[st])
                self.ts("dve", st[:, 14:15], st[:, 10:11], -1.0, st[:, 13:14], ALU.mult, ALU.mult, [st], [st])
                self.act(kin[:, :], p2[:, 128:192], AF.Identity, [p2, st], [kin], scale=st[:, 13:14], bias=st[:, 14:15])
                self.tt("dve", kin[:, :], kin[:, :], klg[:, :], ALU.mult, [kin, klg], [kin])
                self.tt("dve", kin2[:, 0:64], kin[:, :], klb[:, :], ALU.add, [kin, klb], [kin2])
                self.tt("dve", kin2[:, 64:128], kin[:, :], klb[:, :], ALU.add, [kin, klb], [kin2])
                self.tp(p3b[:, 128:256], kin2[:, :], self.identb[:, :], [kin2, self.identb], [p3])
                self.cp("act", cns[b2][:, u * 128:(u + 1) * 128], p3b[:, 0:128], [p3], [cns[b2]])
                self.cp("act", kis[b2][:, u * 128:(u + 1) * 128], p3b[:, 128:256], [p3], [kis[b2]])
                self.ts("dve", wis[b2][:, u, :], p2[:, 192:200], WSC, None, ALU.mult, ALU.bypass, [p2], [wis[b2]])
                p4, p5 = PB[4], PB[5]
                for c in range(DC):
                    self.mm(p4[:, :], xs(c), WIN[:, c, 1736:2248], c == 0, c == DC - 1, [xT, WIN], [p4])
                for c in range(DC):
                    self.mm(p5[:, :], xs(c), WIN[:, c, 2264:2776], c == 0, c == DC - 1, [xT, WIN], [p5])
                self.cp("dve", bvs[b2][:, u, 0:512], p4[:, :], [p4], [bvs[b2]])
                self.act(bvs[b2][:, u, 512:1024], p5[:, :], AF.Silu, [p5], [bvs[b2]])
            self.ld("sp", self.CNTd[:, n0:n0 + 512], cns[b2][:, :], [cns[b2]], [self.CNTd])
            self.ld("sp", self.KITd[:, n0:n0 + 512], kis[b2][:, :], [kis[b2]], [self.KITd])
            self.ld("sp", self.WId[n0:n0 + 512, :].rearrange("(u p) e -> p u e", p=128), wis[b2][:, :, :], [wis[b2]], [self.WId])
            self.ld("sp", self.BVd[n0:n0 + 512, :].rearrange("(u p) f -> p u f", p=128), bvs[b2][:, :, :], [bvs[b2]], [self.BVd])

    def dsa(self, j, sq):
        kb, ar, PB = self.kb, self.ar, self.PB
        inp = self.inp
        S, NT = self.S, self.NT
        nb = sq * S
        ar.reset()
        QT = ar.alloc([128, 4, S], BF16, "QT")
        QIT = ar.alloc([128, 4, S], BF16, "QIT")
        CNT = ar.alloc([128, S], BF16, "CNT")
        KIT = ar.alloc([128, S], BF16, "KIT")
        KT2 = ar.alloc([128, S], BF16, "KT2")
        V1 = ar.alloc([128, NT, 65], BF16, "V1")
        WI = ar.alloc([128, NT, 8], F32, "WI")
        WUK2 = ar.alloc([128, 128], BF16, "WUK2")
        WUV = ar.alloc([128, 64], BF16, "WUV")
        self.ld("sp", QT[:, :, :], self.PQ[0:512, nb:nb + S].rearrange("(g p) t -> p g t", p=128), [self.PQ], [QT])
        self.ld("sp", QIT[:, :, :], self.PQ[512:1024, nb:nb + S].rearrange("(g p) t -> p g t", p=128), [self.PQ], [QIT])
        self.ld("sp", CNT[:, :], self.CNTd[:, nb:nb + S], [self.CNTd], [CNT])
        self.ld("sp", KIT[:, :], self.KITd[:, nb:nb + S], [self.KITd], [KIT])
        self.ld("sp", WI[:, :, :], self.WId[nb:nb + S, :].rearrange("(t p) e -> p t e", p=128), [self.WId], [WI])
        self.ld("pool", WUK2[:, 0:64], inp["ev_a_w_uk"][j], [], [WUK2])
        self.ld("pool", WUK2[:, 64:128], inp["ev_a_w_uk"][j], [], [WUK2])
        self.ld("pool", WUV[:, :], inp["ev_a_w_uv"][j], [], [WUV])
        kb.op("pool", lambda e: e.memset(V1[:, :, 64:65], 1.0), writes=[V1])
        for blk in range(S // 512):
            pb = PB[blk % 2]
            self.mm(pb[:, :], WUK2[:, :], CNT[:, blk * 512:(blk + 1) * 512], True, True, [WUK2, CNT], [pb])
            self.cp("act", KT2[:, blk * 512:(blk + 1) * 512], pb[:, :], [pb], [KT2])
        for t in range(NT):
            pb = PB[2 + t % 2]
            self.mm(pb[:, 0:64], CNT[:, t * 128:(t + 1) * 128], WUV[:, :], True, True, [CNT, WUV], [pb])
            self.cp("dve", V1[:, t, 0:64], pb[:, 0:64], [pb], [V1])
        SC = ar.alloc([128, S], F32, "SC")
        JK = ar.alloc([128, S], BF16, "JK")
        MK = ar.alloc([128, S], BF16, "MK")
        MKT = ar.alloc([128, NT, 128], BF16, "MKT")
        Rs = [ar.alloc([128, 512], F32, "R%d" % i) for i in range(2)]
        PTs = [ar.alloc([128, 512], BF16, "PT%d" % i) for i in range(4)]
        bs = ar.alloc([128, 16], F32, "bs")
        mixs = [ar.alloc([128, 512], BF16, "amix%d" % i) for i in range(2)]
        NIT = 16
        K = float(self.n_sel)
        rk = 0
        pk = 0
        for jt in range(NT):
            n = (jt + 1) * 128
            self.pump(1)
            for h in range(8):
                g, hf = h // 2, h % 2
                for k0 in range(0, n, 512):
                    w = min(512, n - k0)
                    pb = PB[rk % 2]
                    R = Rs[rk % 2]
                    rk += 1
                    self.mm(pb[:, 0:w], QIT[hf * 64:(hf + 1) * 64, g, jt * 128:(jt + 1) * 128], KIT[hf * 64:(hf + 1) * 64, k0:k0 + w], True, True, [QIT, KIT], [pb])
                    self.act(R[:, 0:w], pb[:, 0:w], AF.Relu, [pb], [R])
                    if h == 0:
                        self.ts("dve", SC[:, k0:k0 + w], R[:, 0:w], WI[:, jt, 0:1], None, ALU.mult, ALU.bypass, [R, WI], [SC])
                    else:
                        self.stt(SC[:, k0:k0 + w], R[:, 0:w], WI[:, jt, h:h + 1], SC[:, k0:k0 + w], ALU.mult, ALU.add, [R, WI, SC], [SC])
            thr = jt * 128 >= self.n_sel
            if thr:
                kb.op("dve", lambda e, n=n: e.tensor_reduce(out=bs[:, 0:1], in_=SC[:, 0:n], axis=AX.X, op=ALU.max), reads=[SC], writes=[bs])
                kb.op("dve", lambda e, n=n: e.tensor_reduce(out=bs[:, 1:2], in_=SC[:, 0:n], axis=AX.X, op=ALU.min), reads=[SC], writes=[bs])
                self.tt("dve", bs[:, 2:3], bs[:, 0:1], bs[:, 1:2], ALU.subtract, [bs], [bs])
            self.tt("dve", SC[:, jt * 128:n], SC[:, jt * 128:n], self.cbias[:, :], ALU.add, [SC, self.cbias], [SC])
            if thr:
                lo = bs[:, 1:2]
                for it in range(NIT):
                    f = 2.0 ** -(it + 1)
                    self.stt(bs[:, 3:4], bs[:, 2:3], f, lo, ALU.mult, ALU.add, [bs], [bs])
                    self.ts("dve", JK[:, 0:n], SC[:, 0:n], bs[:, 3:4], None, ALU.is_ge, ALU.add, [SC, bs], [JK, bs], accum=bs[:, 4:5])
                    self.ts("dve", bs[:, 5:6], bs[:, 4:5], K - 0.5, f, ALU.is_ge, ALU.mult, [bs], [bs])
                    self.stt(lo, bs[:, 5:6], bs[:, 2:3], lo, ALU.mult, ALU.add, [bs], [bs])
                self.ts("dve", MK[:, 0:n], SC[:, 0:n], lo, None, ALU.is_ge, ALU.bypass, [SC, bs], [MK])
            else:
                self.ts("dve", MK[:, 0:n], SC[:, 0:n], -1e29, None, ALU.is_ge, ALU.bypass, [SC], [MK])
            for i0 in range(0, jt + 1, 8):
                i1 = min(jt + 1, i0 + 8)
                p2 = PB[2]
                p2b = p2[:, :].bitcast(BF16)
                for i in range(i0, i1):
                    self.tp(p2b[:, (i - i0) * 128:(i - i0 + 1) * 128], MK[:, i * 128:(i + 1) * 128], self.identb[:, :], [MK, self.identb], [p2])
                self.cp("act", MKT[:, i0:i1, :], p2b[:, 0:(i1 - i0) * 128].rearrange("p (i t) -> p i t", t=128), [p2], [MKT])
            for b in range(2):
                self.mm(PB[6 + b][:, 0:260], self.zb[:, 0:128], self.zb[:, 0:260], True, False, [self.zb], [PB[6 + b]], skip=True)
            for i in range(jt + 1):
                for hf in range(2):
                    ps = PB[2 + pk % 4]
                    PT = PTs[pk % 4]
                    pk += 1
                    self.mm(ps[:, :], KT2[hf * 64:(hf + 1) * 64, i * 128:(i + 1) * 128], QT[hf * 64:(hf + 1) * 64, :, jt * 128:(jt + 1) * 128], True, True, [KT2, QT], [ps])
                    self.act(PT[:, :], ps[:, :], AF.Exp, [ps], [PT], scale=0.125)
                    pv = PT[:, :].rearrange("p (g t) -> p g t", t=128)
                    self.tt("dve", pv, pv, MKT[:, i, :].unsqueeze(1).to_broadcast([128, 4, 128]), ALU.mult, [PT, MKT], [PT])
                    for g in range(4):
                        h = 2 * g + hf
                        ob = PB[6 + h // 4]
                        self.mm(ob[:, (h % 4) * 65:(h % 4) * 65 + 65], PT[:, g * 128:(g + 1) * 128], V1[:, i, :], False, i == jt, [PT, V1], [ob], skip=True)
            mx = mixs[jt % 2]
            for b in range(2):
                ov = PB[6 + b][:, 0:260].rearrange("p (h d) -> p h d", d=65)
                kb.op("dve", lambda e, ov=ov, b=b: e.reciprocal(out=bs[:, 8 + b * 4:12 + b * 4].unsqueeze(2), in_=ov[:, :, 64:65]), reads=[PB[6 + b]], writes=[bs])
                self.tt("dve", mx[:, b * 256:(b + 1) * 256].rearrange("p (h d) -> p h d", d=64), ov[:, :, 0:64],
                        bs[:, 8 + b * 4:12 + b * 4].unsqueeze(2).to_broadcast([128, 4, 64]), ALU.mult, [PB[6 + b], bs], [mx])
            self.ld("sp", self.MIXD[nb + jt * 128:nb + (jt + 1) * 128, 0:512], mx[:, :], [mx], [self.MIXD])

    def gla(self, j, sq):
        kb, ar, PB = self.kb, self.ar, self.PB
        inp = self.inp
        S = self.S
        NCH = S // 64
        nb = sq * S
        ar.reset()
        BQs = [ar.alloc([64, S], F32, "BQ%d" % i) for i in range(2)]
        BKs = [ar.alloc([64, S], F32, "BK%d" % i) for i in range(2)]
        BG = ar.alloc([16, S], BF16, "BG")
        BV = ar.alloc([64, NCH, 1024], BF16, "BV")
        WG2 = ar.alloc([16, 256], BF16, "WG2")
        nbias = ar.alloc([64, 4], F32, "nbias")
        GNB = ar.alloc([64, 512], F32, "GNB")
        self.ld("sp", BG[:, :], self.BGTd[:, nb:nb + S], [self.BGTd], [BG])
        self.ld("sp", BV[:, :, :], self.BVd[nb:nb + S, :].rearrange("(c p) f -> p c f", p=64), [self.BVd], [BV])
        self.ld("pool", WG2[:, :], inp["ev_b_w_g2"][j], [], [WG2])
        for h in range(4):
            self.ld("sp", nbias[:, h:h + 1], inp["ev_b_b_g"][j, h * 64:(h + 1) * 64].rearrange("(p o) -> p o", o=1), [], [nbias])
        kb.op("dve", lambda e: e.tensor_scalar(out=nbias[:, :], in0=nbias[:, :], scalar1=-1.0, scalar2=None, op0=ALU.mult), reads=[nbias], writes=[nbias])
        self.ld("sp", GNB[:, :], inp["ev_b_norm_g"][j:j + 1, :].partition_broadcast(64), [], [GNB])
        LA = ar.alloc([64, 512], F32, "LA")
        BP = ar.alloc([64, 512], F32, "BP")
        EQ = ar.alloc([64, 512], F32, "EQ")
        EBL = ar.alloc([64, 4, NCH], F32, "EBL")
        QG = ar.alloc([64, 4, S], BF16, "QG")
        KG = ar.alloc([64, 4, S], BF16, "KG")
        KTM = ar.alloc([64, NCH, 256], BF16, "KTM")
        epsr = ar.alloc([64, 1], F32, "epsr")
        kb.op("pool", lambda e: e.memset(epsr[:, :], RMS_EPS), writes=[epsr])
        rst = self.rst[0:64, :]
        for h in range(4):
            BQ, BK = BQs[h % 2], BKs[h % 2]
            self.ld("sp", BQ[:, :], self.PBQK[h * 64:(h + 1) * 64, nb:nb + S], [self.PBQK], [BQ])
            self.ld("sp", BK[:, :], self.PBQK[256 + h * 64:256 + (h + 1) * 64, nb:nb + S], [self.PBQK], [BK])
            for blk in range(S // 512):
                sl = slice(blk * 512, (blk + 1) * 512)
                pb = PB[blk % 2]
                self.mm(pb[0:64, :], WG2[0:16, h * 64:(h + 1) * 64], BG[0:16, sl], True, True, [WG2, BG], [pb])
                self.act(LA[:, :], pb[0:64, :], AF.Exp, [pb, nbias], [LA], scale=-1.0, bias=nbias[:, h:h + 1])
                self.act(LA[:, :], LA[:, :], AF.Ln, [LA], [LA], bias=1.0)
                kb.op("dve", lambda e: e.tensor_tensor_scan(out=BP[:, :], data0=rst, data1=LA[:, :], initial=0.0, op0=ALU.mult, op1=ALU.add),
                      reads=[self.rst, LA], writes=[BP])
                self.act(EQ[:, :], BP[:, :], AF.Exp, [BP], [EQ], scale=-1.0 / 16)
                self.tt("dve", QG[:, h, sl], BQ[:, sl], EQ[:, :], ALU.mult, [BQ, EQ], [QG])
                self.cp("dve", EBL[:, h, blk * 8:(blk + 1) * 8], EQ[:, :].rearrange("p (c j) -> p c j", j=64)[:, :, 63], [EQ], [EBL])
                self.act(EQ[:, :], BP[:, :], AF.Exp, [BP], [EQ], scale=1.0 / 16)
                self.tt("dve", KG[:, h, sl], BK[:, sl], EQ[:, :], ALU.mult, [BK, EQ], [KG])
        for c in range(NCH):
            pb = PB[2 + c % 2]
            pbb = pb[:, :].bitcast(BF16)
            for h in range(4):
                self.tp(pbb[0:64, h * 64:(h + 1) * 64], KG[:, h, c * 64:(c + 1) * 64], self.identb[0:64, 0:64], [KG, self.identb], [pb])
            self.cp("act", KTM[:, c, :], pbb[0:64, 0:256], [pb], [KTM])
            self.tt("pool", BV[:, c, 512:1024], BV[:, c, 512:1024], GNB[:, :], ALU.mult, [BV, GNB], [BV])
        ST32 = ar.alloc([64, 4, 128], F32, "ST32")
        STB = ar.alloc([64, 4, 128], BF16, "STB")
        TMP = ar.alloc([64, 4, 128], F32, "TMP")
        SCMs = [ar.alloc([64, 256], BF16, "SCM%d" % i) for i in range(2)]
        ss = ar.alloc([64, 16], F32, "ss")
        junk = ar.alloc([64, 128], F32, "gjunk")
        mixs = [ar.alloc([64, 512], BF16, "bmix%d" % i) for i in range(2)]
        kb.op("pool", lambda e: e.memset(ST32[:, :, :], 0.0), writes=[ST32])
        kb.op("pool", lambda e: e.memset(STB[:, :, :], 0.0), writes=[STB])
        trig = self.trigb[0:64, :]
        for c in range(NCH):
            cs = slice(c * 64, (c + 1) * 64)
            psc = PB[c % 2]
            for h in range(4):
                self.mm(psc[0:64, h * 64:(h + 1) * 64], KG[:, h, cs], QG[:, h, cs], True, True, [KG, QG], [psc])
            SCM = SCMs[c % 2]
            self.tt("dve", SCM[:, :].rearrange("p (h i) -> p h i", i=64), psc[0:64, 0:256].rearrange("p (h i) -> p h i", i=64),
                    trig.unsqueeze(1).to_broadcast([64, 4, 64]), ALU.mult, [psc, self.trigb], [SCM])
            og = PB[2 + c % 2]
            for h in range(4):
                self.mm(og[0:64, h * 128:(h + 1) * 128], SCM[:, h * 64:(h + 1) * 64], BV[:, c, h * 128:(h + 1) * 128], True, c == 0, [SCM, BV], [og])
                if c > 0:
                    self.mm(og[0:64, h * 128:(h + 1) * 128], QG[:, h, cs], STB[:, h, :], False, True, [QG, STB], [og])
            if c < NCH - 1:
                pst = PB[4 + c % 2]
                for h in range(4):
                    self.mm(pst[0:64, h * 128:(h + 1) * 128], KTM[:, c, h * 64:(h + 1) * 64], BV[:, c, h * 128:(h + 1) * 128], True, True, [KTM, BV], [pst])
                for h in range(4):
                    self.act(TMP[:, h, :], pst[0:64, h * 128:(h + 1) * 128], AF.Identity, [pst, EBL], [TMP], scale=EBL[:, h, c:c + 1])
                    self.stt(ST32[:, h, :], ST32[:, h, :], EBL[:, h, c:c + 1], TMP[:, h, :], ALU.mult, ALU.add, [ST32, EBL, TMP], [ST32])
                self.cp("pool", STB[:, :, :], ST32[:, :, :], [ST32], [STB])
            mx = mixs[c % 2]
            for h in range(4):
                self.act(junk[:, :], og[0:64, h * 128:(h + 1) * 128], AF.Square, [og], [junk, ss], accum_out=ss[:, h:h + 1])
            self.act(ss[:, 4:8], ss[:, 0:4], AF.Sqrt, [ss, epsr], [ss], scale=1.0 / 128, bias=epsr[:, 0:1])
            kb.op("dve", lambda e: e.reciprocal(out=ss[:, 8:12], in_=ss[:, 4:8]), reads=[ss], writes=[ss])
            for h in range(4):
                self.stt(mx[:, h * 128:(h + 1) * 128], og[0:64, h * 128:(h + 1) * 128], ss[:, 8 + h:9 + h], BV[:, c, 512 + h * 128:512 + (h + 1) * 128],
                         ALU.mult, ALU.mult, [og, ss, BV], [mx])
            self.ld("sp", self.MIXD[nb + c * 64:nb + (c + 1) * 64, 512:1024], mx[:, :], [mx], [self.MIXD])

    def odd_proj(self, j, XTold):
        kb, ar, PB = self.kb, self.ar, self.PB
        self.even_scratch()
        ar.reset()
        W = ar.alloc([128, DC, 3072], BF16, "WQKV")
        self.load_w_cast(W, self.inp["od_w_qkv"][j], 3072, [])
        xTs = [ar.alloc([128, DC, 512], BF16, "xT%d" % i) for i in range(2)]
        pqs = [ar.alloc([128, 16, 512], BF16, "pqs%d" % i) for i in range(2)]
        vs = [ar.alloc([128, 4, 1024], BF16, "vs%d" % i) for i in range(2)]
        for blk in range(self.N // 512):
            n0 = blk * 512
            b2 = blk % 2
            xT = xTs[b2]
            self.ld("sp", xT[:, :, :], XTold[:, n0:n0 + 512].rearrange("(c p) t -> p c t", p=128), [XTold], [xT])
            for g in range(16):
                pb = PB[g % 2]
                for c in range(DC):
                    self.mm(pb[:, :], W[:, c, g * 128:(g + 1) * 128], xT[:, c, :], c == 0, c == DC - 1, [W, xT], [pb])
                self.cp("act" if g % 2 == 0 else "dve", pqs[b2][:, g, :], pb[:, :], [pb], [pqs[b2]])
            self.ld("sp", self.PQ[:, n0:n0 + 512].rearrange("(g p) t -> p g t", p=128), pqs[b2][:, :, :], [pqs[b2]], [self.PQ])
            for u in range(4):
                for h in range(2):
                    pb = PB[2 + (2 * u + h) % 4]
                    for c in range(DC):
                        self.mm(pb[:, :], xT[:, c, u * 128:(u + 1) * 128], W[:, c, 2048 + h * 512:2048 + (h + 1) * 512], c == 0, c == DC - 1, [xT, W], [pb])
                    self.cp("act" if h == 0 else "dve", vs[b2][:, u, h * 512:(h + 1) * 512], pb[:, :], [pb], [vs[b2]])
            self.ld("sp", self.BVd[n0:n0 + 512, :].rearrange("(u p) f -> p u f", p=128), vs[b2][:, :, :], [vs[b2]], [self.BVd])

    def diffattn(self, j, sq, lambda_init):
        kb, ar, PB = self.kb, self.ar, self.PB
        inp = self.inp
        S, NT, NB = self.S, self.NT, self.NB
        nb = sq * S
        ar.reset()
        QT = ar.alloc([128, 8, S], BF16, "QT")
        KT = ar.alloc([128, 8, S], BF16, "KT")
        V1 = ar.alloc([128, NT, 8, 129], BF16, "V1")
        self.ld("sp", QT[:, :, :], self.PQ[0:1024, nb:nb + S].rearrange("(g p) t -> p g t", p=128), [self.PQ], [QT])
        self.ld("sp", KT[:, :, :], self.PQ[1024:2048, nb:nb + S].rearrange("(g p) t -> p g t", p=128), [self.PQ], [KT])
        kb.op("pool", lambda e: e.memset(V1[:, :, :, 128:129], 1.0), writes=[V1])
        for t in range(NT):
            self.ld("sp", V1[:, t, :, 0:128], self.BVd[nb + t * 128:nb + (t + 1) * 128, :].rearrange("p (h e) -> p h e", e=128), [self.BVd], [V1])
        LM = ar.alloc([128, 4, 64], F32, "LM")
        lm = ar.alloc([128, 16], F32, "lm")
        SG = ar.alloc([128, 128], F32, "SG")
        epsr = ar.alloc([128, 1], F32, "epsr")
        kb.op("pool", lambda e: e.memset(epsr[:, :], RMS_EPS), writes=[epsr])
        self.ld("sp", LM[:, :, :].rearrange("p a b -> p (a b)"), inp["od_lam"][j:j + 1].rearrange("o a b -> o (a b)").partition_broadcast(128), [], [LM])
        self.bcast_load(SG, inp["od_subln_g"][j:j + 1, :], [])
        kb.op("dve", lambda e: e.tensor_scalar(out=SG[:, :], in0=SG[:, :], scalar1=1.0 - lambda_init, scalar2=None, op0=ALU.mult), reads=[SG], writes=[SG])
        junk = ar.alloc([128, 128], F32, "junk")
        for q in range(2):
            kb.op("dve", lambda e, q=q: e.tensor_tensor(out=junk[:, 0:64], in0=LM[:, 2 * q, :], in1=LM[:, 2 * q + 1, :], op=ALU.mult), reads=[LM], writes=[junk])
            kb.op("dve", lambda e, q=q: e.tensor_reduce(out=lm[:, q:q + 1], in_=junk[:, 0:64], axis=AX.X, op=ALU.add), reads=[junk], writes=[lm])
        self.act(lm[:, 2:4], lm[:, 0:2], AF.Exp, [lm], [lm])
        self.tt("dve", lm[:, 4:5], lm[:, 3:4], lm[:, 2:3], ALU.subtract, [lm], [lm])
        self.ts("dve", lm[:, 5:6], lm[:, 4:5], -lambda_init, None, ALU.add, ALU.bypass, [lm], [lm])
        PTs = [ar.alloc([128, 512], BF16, "PT%d" % i) for i in range(4)]
        dt_ = ar.alloc([128, 128], F32, "dt")
        st = ar.alloc([128, 16], F32, "st")
        mos = [ar.alloc([128, 4, 128], BF16, "mo%d" % i) for i in range(2)]
        pk = 0
        it = 0
        for h in range(8):
            for tb in range(NB):
                self.pump(1)

                def bank(m, u):
                    return PB[4 + 2 * m + u // 2], (u % 2) * 129
                for b in range(4):
                    self.mm(PB[4 + b][:, 0:258], self.zb[:, 0:128], self.zb[:, 0:258], True, False, [self.zb], [PB[4 + b]], skip=True)
                for m in range(2):
                    ms = slice(m * 64, (m + 1) * 64)
                    for i in range(4 * tb + 4):
                        u0 = max(0, i - 4 * tb)
                        w = 512 - u0 * 128
                        ps = PB[pk % 4]
                        PT = PTs[pk % 4]
                        pk += 1
                        self.mm(ps[:, 0:w], KT[ms, h, i * 128:(i + 1) * 128], QT[ms, h, tb * 512 + u0 * 128:(tb + 1) * 512], True, True, [KT, QT], [ps])
                        self.act(PT[:, 0:w], ps[:, 0:w], AF.Exp, [ps], [PT], scale=0.125)
                        if i >= 4 * tb:
                            self.tt("pool", PT[:, 0:128], PT[:, 0:128], self.trib[:, :], ALU.mult, [PT, self.trib], [PT])
                        for u in range(u0, 4):
                            bk, off = bank(m, u)
                            self.mm(bk[:, off:off + 129], PT[:, (u - u0) * 128:(u - u0 + 1) * 128], V1[:, i, h, :], False, i == 4 * tb + u, [PT, V1], [bk], skip=True)
                mo = mos[it % 2]
                it += 1
                for u in range(4):
                    b0, o0 = bank(0, u)
                    b1, o1 = bank(1, u)
                    kb.op("dve", lambda e, b0=b0, o0=o0: e.reciprocal(out=st[:, 0:1], in_=b0[:, o0 + 128:o0 + 129]), reads=[b0], writes=[st])
                    kb.op("dve", lambda e, b1=b1, o1=o1: e.reciprocal(out=st[:, 1:2], in_=b1[:, o1 + 128:o1 + 129]), reads=[b1], writes=[st])
                    self.tt("dve", st[:, 2:3], st[:, 1:2], lm[:, 5:6], ALU.mult, [st, lm], [st])
                    self.ts("dve", dt_[:, :], b0[:, o0:o0 + 128], st[:, 0:1], None, ALU.mult, ALU.bypass, [b0, st], [dt_])
                    self.stt(dt_[:, :], b1[:, o1:o1 + 128], st[:, 2:3], dt_[:, :], ALU.mult, ALU.add, [b1, st, dt_], [dt_])
                    self.act(junk[:, :], dt_[:, :], AF.Square, [dt_], [junk, st], accum_out=st[:, 3:4])
                    self.act(st[:, 4:5], st[:, 3:4], AF.Sqrt, [st, epsr], [st], scale=1.0 / 128, bias=epsr[:, 0:1])
                    kb.op("dve", lambda e: e.reciprocal(out=st[:, 5:6], in_=st[:, 4:5]), reads=[st], writes=[st])
                    self.stt(mo[:, u, :], dt_[:, :], st[:, 5:6], SG[:, :], ALU.mult, ALU.mult, [dt_, st, SG], [mo])
                self.ld("sp", self.MIXD[nb + tb * 512:nb + (tb + 1) * 512, h * 128:(h + 1) * 128].rearrange("(u p) e -> p u e", p=128), mo[:, :, :], [mo], [self.MIXD])

    def build(self):
        inp = self.inp
        NL = self.NL
        nsub = 2 * NL
        xs = [inp["x"]] + [self.XA if k % 2 == 0 else self.XB for k in range(nsub - 1)] + [self.out]
        ph = self.dbg
        on = lambda p: ph is None or p in ph
        self.queue_conversions()
        self.pump(0, upto=(0, 0))
        if on("init"):
            self.init_xt(inp["x"], self.XT[0])
        k = 0
        for li in range(NL):
            j = li // 2
            if li % 2 == 0:
                if on("eproj"):
                    self.even_proj(j, self.XT[k % 2])
                for sq in range(self.NSEQ):
                    if on("dsa"):
                        self.dsa(j, sq)
                    if on("gla"):
                        self.gla(j, sq)
                if on("oproj"):
                    self.outproj_phase(inp["ev_w_out"][j], xs[k], xs[k + 1], self.XT[(k + 1) % 2], inp["ev_ln1_g"][j:j + 1, :], inp["ev_ln1_b"][j:j + 1, :])
                k += 1
                if on("ffn"):
                    self.ffn_phase(j, xs[k], xs[k + 1], self.XT[k % 2], self.XT[(k + 1) % 2], inp["ev_ln2_g"][j:j + 1, :], inp["ev_ln2_b"][j:j + 1, :])
                k += 1
            else:
                lambda_init = 0.8 - 0.6 * math.exp(-0.3 * li)
                if on("qkv"):
                    self.odd_proj(j, self.XT[k % 2])
                for sq in range(self.NSEQ):
                    if on("dattn"):
                        self.diffattn(j, sq, lambda_init)
                if on("oproj2"):
                    self.outproj_phase(inp["od_w_out"][j], xs[k], xs[k + 1], self.XT[(k + 1) % 2], inp["od_ln1_g"][j:j + 1, :], inp["od_ln1_b"][j:j + 1, :],
                                       router=(inp["od_router_w"][j], inp["od_router_b"][j:j + 1, :]))
                k += 1
                if on("moe"):
                    self.route(xs[k])
                    self.moe_sparse(j, xs[k], xs[k + 1], self.XT[(k + 1) % 2], inp["od_ln2_g"][j:j + 1, :], inp["od_ln2_b"][j:j + 1, :])
                k += 1
        self.kb.barrier()
        self.kb.emit()
        return self.nc


def make_consts():
    c = np.zeros((128, 1024), np.float32)
    p = np.arange(128)
    c[:, 0:128] = np.eye(128, dtype=np.float32)
    c[:, 128:256] = np.where(p[None, :] <= p[:, None], 0.0, -1e30)
    c[:, 256:384] = (p[:, None] <= p[None, :]).astype(np.float32)
    c[:, 384:896] = (np.arange(512)[None, :] % 64 != 0).astype(np.float32)
    c[:, 896:960] = ((p[:, None] % 64) <= np.arange(64)[None, :]).astype(np.float32)
    c[:, 960:968] = 512.0 * np.arange(8)[None, :]
    c[:, 968:992] = 512.0 * np.arange(24)[None, :]
    c[:, 992:1003] = np.arange(11)[None, :]
    c[:, 1003] = 11.0 * p
    return c


_CACHE = {}


def run_prog(inputs, S, NSEQ, NL, ncores, trace=False):
    key = (S, NSEQ, NL)
    if key not in _CACHE:
        p_ = Prog(S, NSEQ, NL)
        _CACHE[key] = (p_.build(), set(p_.inp.keys()))
    nc, names = _CACHE[key]
    x = np.ascontiguousarray(inputs["x"], dtype=np.float32)
    shared = {"cst": make_consts()}
    for name, v in inputs.items():
        if name == "x" or name.startswith("od_lam_") or name not in names:
            continue
        v = np.ascontiguousarray(v, dtype=np.float32)
        if name == "ev_b_norm_g":
            v = v.reshape(2, 512)
        shared[name] = v
    shared["od_lam"] = np.ascontiguousarray(np.stack([inputs["od_lam_q1"], inputs["od_lam_k1"], inputs["od_lam_q2"], inputs["od_lam_k2"]], axis=1), dtype=np.float32)
    in_maps = []
    for c in range(ncores):
        m = dict(shared)
        m["x"] = np.ascontiguousarray(x[c * NSEQ:(c + 1) * NSEQ].reshape(NSEQ * S, D))
        in_maps.append(m)
    res = run_bass_kernel_spmd(nc, in_maps, core_ids=list(range(ncores)), trace=trace)
    out = np.concatenate([r["out"].reshape(NSEQ, S, D) for r in res.results], axis=0)
    return out, res


def kernel(**inputs):
    out, _ = run_prog(inputs, 2048, 2, 4, 8)
    return out.astype(np.float32)
```

```python
import math
import numpy as np
import ml_dtypes
from contextlib import ExitStack
import concourse.bass as bass
import concourse.mybir as mybir
from concourse.bass_utils import run_bass_kernel_spmd

F32 = mybir.dt.float32
BF16 = mybir.dt.bfloat16
U8 = mybir.dt.uint8
ALU = mybir.AluOpType
AF = mybir.ActivationFunctionType
AX = mybir.AxisListType

D = 1024
DC = 8
DEPTH = 4
ALPHA = (2.0 * DEPTH) ** 0.25
LN_EPS = 1e-5
RMS_EPS = 1e-6
DFF = 2816
FC = 22
NEXP = 8
EVEN_IN = 2776


class Res:
    __slots__ = ("ws", "rs", "name")

    def __init__(self, name=""):
        self.ws = {}
        self.rs = {}
        self.name = name


class T:
    def __init__(self, ap, name="", res=None):
        self.ap = ap
        self.name = name
        self.res = res if res is not None else Res(name)

    def __getitem__(self, idx):
        return self.ap[idx]


def _res(x):
    return x.res if isinstance(x, T) else x


class KB:
    ENG = ("pe", "act", "dve", "pool", "sp")
    NDMA = 8

    def __init__(self, nc):
        self.nc = nc
        self.stack = ExitStack()
        self.prog = {e: [] for e in self.ENG}
        self.cnt = {e: 0 for e in self.ENG}
        self.seen = {e: {} for e in self.ENG}
        self.sems = {e: self.stack.enter_context(nc.semaphore("s_" + e)) for e in self.ENG}
        self.dsem, self.dcnt, self.dnext = {}, {}, {}
        for q in ("sp", "pool", "act", "bulk"):
            self.dsem[q] = [self.stack.enter_context(nc.semaphore("d_%s%d" % (q, i))) for i in range(self.NDMA)]
            self.dcnt[q] = [0] * self.NDMA
            self.dnext[q] = 0
        self.nalloc = 0
        self.nins = 0

    def sb(self, shape, dtype, name=None):
        self.nalloc += 1
        name = name or "t%d" % self.nalloc
        h = self.stack.enter_context(self.nc.sbuf_tensor(name, list(shape), dtype))
        return T(h[tuple(slice(None) for _ in shape)], name)

    def ps(self, shape, dtype, name=None):
        self.nalloc += 1
        name = name or "p%d" % self.nalloc
        h = self.stack.enter_context(self.nc.psum_tensor(name, list(shape), dtype))
        return T(h[tuple(slice(None) for _ in shape)], name)

    def dram(self, name, shape, dtype, kind="Internal"):
        h = self.nc.dram_tensor(name, list(shape), dtype, kind=kind)
        return T(h[tuple(slice(None) for _ in shape)], name)

    def _waits(self, eng, reads, writes):
        need = {}
        seen = self.seen[eng]

        def add(d):
            for key, (val, src) in d.items():
                if src == "pe" and eng == "pe":
                    continue
                if seen.get(key, 0) >= val:
                    continue
                if need.get(key, 0) < val:
                    need[key] = val

        for r in reads:
            add(_res(r).ws)
        for w in writes:
            w = _res(w)
            add(w.ws)
            add(w.rs)
        out = []
        for key, val in need.items():
            seen[key] = val
            out.append((key, val))
        return out

    def _commit(self, ev, reads, writes):
        key, val, src = ev
        for r in reads:
            _res(r).rs[key] = (val, src)
        for w in writes:
            w = _res(w)
            w.ws[key] = (val, src)
            w.rs = {}

    def _semof(self, key):
        if isinstance(key, str):
            return self.sems[key]
        return self.dsem[key[0]][key[1]]

    def op(self, eng, fn, reads=(), writes=()):
        waits = self._waits(eng, reads, writes)
        self.cnt[eng] += 1
        ev = (eng, self.cnt[eng], eng)
        self.prog[eng].append((waits, fn, (eng, 1)))
        self._commit(ev, reads, writes)
        self.nins += 1
        return ev

    def dma(self, q, fn, reads=(), writes=(), ring=None):
        ring = ring or q
        i = self.dnext[ring]
        self.dnext[ring] = (i + 1) % self.NDMA
        key = (ring, i)
        waits = self._waits(q, reads, writes)
        prev = self.dcnt[ring][i]
        if prev and self.seen[q].get(key, 0) < prev:
            self.seen[q][key] = prev
            waits.append((key, prev))
        self.dcnt[ring][i] = prev + 16
        ev = (key, prev + 16, "dma")
        self.prog[q].append((waits, fn, (key, 16)))
        self._commit(ev, reads, writes)
        self.nins += 1
        return ev

    def barrier(self, full=False):
        for e in self.ENG:
            waits = []
            for f in self.ENG:
                if f != e and self.cnt[f] and self.seen[e].get(f, 0) < self.cnt[f]:
                    self.seen[e][f] = self.cnt[f]
                    waits.append((f, self.cnt[f]))
            if self.cnt[e] and e != "pe" and self.seen[e].get(e, 0) < self.cnt[e]:
                self.seen[e][e] = self.cnt[e]
                waits.append((e, self.cnt[e]))
            for q in self.dsem:
                if q == "bulk" and not full:
                    continue
                for i in range(self.NDMA):
                    v = self.dcnt[q][i]
                    if v and self.seen[e].get((q, i), 0) < v:
                        self.seen[e][(q, i)] = v
                        waits.append(((q, i), v))
            if waits:
                self.prog[e].append((waits, None, None))

    def emit(self):
        nc = self.nc
        with nc.Block() as block:
            def run(e, name):
                for waits, fn, inc in self.prog[name]:
                    for key, val in waits:
                        e.wait_ge(self._semof(key), val)
                    if fn is not None:
                        fn(e).then_inc(self._semof(inc[0]), inc[1])

            @block.tensor
            def _(e):
                run(e, "pe")

            @block.scalar
            def _(e):
                run(e, "act")

            @block.vector
            def _(e):
                run(e, "dve")

            @block.gpsimd
            def _(e):
                run(e, "pool")

            @block.sync
            def _(e):
                run(e, "sp")
        self.stack.close()


class Arena:
    def __init__(self, kb, nbytes):
        self.kb = kb
        self.t = kb.sb([128, nbytes], U8, name="arena")
        self.n = nbytes
        self.off = 0

    def reset(self):
        self.kb.barrier()
        self.off = 0

    def alloc(self, shape, dtype, name=""):
        nb = 4 if dtype == F32 else 2
        n = int(np.prod(shape[1:])) * nb
        n_al = (n + 63) // 64 * 64
        assert self.off + n_al <= self.n, "arena overflow %s %d+%d>%d" % (name, self.off, n_al, self.n)
        ap = self.t.ap[:, self.off:self.off + n].bitcast(dtype)
        self.off += n_al
        if len(shape) == 3:
            ap = ap.rearrange("p (a b) -> p a b", b=shape[2])
        elif len(shape) == 4:
            ap = ap.rearrange("p (a b c) -> p a b c", b=shape[2], c=shape[3])
        if shape[0] < 128:
            ap = ap[0:shape[0]]
        return T(ap, name)


class Prog:
    def __init__(self, S, NSEQ, NL, dbg=None):
        self.S, self.NSEQ, self.NL = S, NSEQ, NL
        self.N = S * NSEQ
        self.NT = S // 128
        self.NB = S // 512
        self.n_sel = min(256, S // 4)
        self.dbg = dbg
        nc = self.nc = bass.Bass("TRN2", target_bir_lowering=False)
        kb = self.kb = KB(nc)
        N = self.N
        self.inp = {}

        def ext(name, shape, dt=F32):
            self.inp[name] = kb.dram(name, shape, dt, kind="ExternalInput")
            return self.inp[name]

        ext("x", [N, D])
        ext("cst", [128, 1024])
        ext("ev_w_in", [2, D, EVEN_IN]); ext("ev_a_kv_norm_g", [2, 128]); ext("ev_a_w_uk", [2, 128, 64]); ext("ev_a_w_uv", [2, 128, 64])
        ext("ev_a_kidx_ln_g", [2, 64]); ext("ev_a_kidx_ln_b", [2, 64]); ext("ev_b_w_g2", [2, 16, 256]); ext("ev_b_b_g", [2, 256])
        ext("ev_b_norm_g", [2, 512]); ext("ev_w_out", [2, D, D]); ext("ev_ln1_g", [2, D]); ext("ev_ln1_b", [2, D])
        ext("ev_ffn_w_gate", [2, D, DFF]); ext("ev_ffn_w_up", [2, D, DFF]); ext("ev_ffn_w_down", [2, DFF, D])
        ext("ev_ln2_g", [2, D]); ext("ev_ln2_b", [2, D])
        ext("od_w_qkv", [2, D, 3072]); ext("od_lam", [2, 4, 64]); ext("od_subln_g", [2, 128]); ext("od_w_out", [2, D, D])
        ext("od_ln1_g", [2, D]); ext("od_ln1_b", [2, D]); ext("od_router_w", [2, D, 8]); ext("od_router_b", [2, 8])
        if NL > 1:
            ext("od_moe_w_gate", [2, NEXP, D, DFF]); ext("od_moe_w_up", [2, NEXP, D, DFF]); ext("od_moe_w_down", [2, NEXP, DFF, D])
        ext("od_ln2_g", [2, D]); ext("od_ln2_b", [2, D])
        self.out = kb.dram("out", [N, D], F32, kind="ExternalOutput")
        self.XA = kb.dram("XA", [N, D], F32)
        self.XB = kb.dram("XB", [N, D], F32)
        self.XT = [kb.dram("XT0", [D, N], BF16), kb.dram("XT1", [D, N], BF16)]
        self.MIXD = kb.dram("MIXD", [N, D], BF16)
        NG = self.NG = (2 * N) // 512 + NEXP
        def table(name):
            full = [kb.dram("%s%d" % (name, j), [(NEXP + 1) * 1408, 2048], BF16) for j in range(2)]
            parts = [[T(full[j].ap[sl * 1408:(sl + 1) * 1408, :], "%s%d_%d" % (name, j, sl)) for sl in range(NEXP + 1)] for j in range(2)]
            return full, parts
        self.TgF, self.Tg = table("Tg")
        self.TuF, self.Tu = table("Tu")
        self.TdF, self.Td = table("Td")
        self.XS = kb.dram("XS", [NG * 512, D], BF16)
        self.YS = kb.dram("YS", [NG * 512, D], F32)
        self.PB = [kb.ps([128, 512], F32, name="bank%d" % i) for i in range(8)]
        self.cstf = kb.sb([128, 1024], F32, name="cstf")
        self.identb = kb.sb([128, 128], BF16, name="identb")
        self.trib = kb.sb([128, 128], BF16, name="trib")
        self.trigb = kb.sb([128, 64], BF16, name="trigb")
        self.zb = kb.sb([128, 512], BF16, name="zb")
        NTT = self.NTT = NSEQ * self.NT
        self.comb = kb.sb([128, NTT, 8], F32, name="comb")
        self.sel12 = kb.sb([128, NTT, 16], F32, name="sel12")
        self.gates = kb.sb([128, NTT, 2], F32, name="gates")
        self.pidx = kb.sb([128, NTT, 2], mybir.dt.int32, name="pidx")
        self.idxq = kb.sb([128, NG, 11], mybir.dt.int32, name="idxq")
        self.onesb = kb.sb([128, 128], BF16, name="onesb")
        self.onesf = kb.sb([128, 8], F32, name="onesf")
        self.ustb = kb.sb([128, 128], BF16, name="ustb")
        self.ar = Arena(kb, 192 * 1024)
        self.identf = T(self.cstf[:, 0:128], "identf", self.cstf.res)
        self.cbias = T(self.cstf[:, 128:256], "cbias", self.cstf.res)
        self.rst = T(self.cstf[:, 384:896], "rst", self.cstf.res)
        kb.dma("sp", lambda e: e.dma_start(out=self.cstf[:, :], in_=self.inp["cst"][:, :]), reads=[self.inp["cst"]], writes=[self.cstf])
        kb.op("dve", lambda e: e.tensor_copy(out=self.identb[:, :], in_=self.cstf[:, 0:128]), reads=[self.cstf], writes=[self.identb])
        kb.op("dve", lambda e: e.tensor_copy(out=self.trib[:, :], in_=self.cstf[:, 256:384]), reads=[self.cstf], writes=[self.trib])
        kb.op("dve", lambda e: e.tensor_copy(out=self.trigb[:, :], in_=self.cstf[:, 896:960]), reads=[self.cstf], writes=[self.trigb])
        kb.op("pool", lambda e: e.memset(self.zb[:, :], 0.0), writes=[self.zb])
        kb.op("pool", lambda e: e.memset(self.onesb[:, :], 1.0), writes=[self.onesb])
        kb.op("pool", lambda e: e.memset(self.onesf[:, :], 1.0), writes=[self.onesf])
        kb.op("dve", lambda e: e.tensor_tensor(out=self.ustb[:, :], in0=self.cstf[:, 256:384], in1=self.cstf[:, 0:128], op=ALU.subtract),
              reads=[self.cstf], writes=[self.ustb])
        self.conv_q = []

    def mm(self, out, lhsT, rhs, start, stop, reads, writes, skip=False):
        self.kb.op("pe", lambda e: e.matmul(out, lhsT=lhsT, rhs=rhs, start=start, stop=stop, skip_group_check=skip), reads=reads, writes=writes)

    def tp(self, out, in_, ident, reads, writes):
        self.kb.op("pe", lambda e: e.transpose(out, in_, ident), reads=reads, writes=writes)

    def act(self, out, in_, func, reads, writes, **kw):
        self.kb.op("act", lambda e: e.activation(out=out, in_=in_, func=func, **kw), reads=reads, writes=writes)

    def tt(self, eng, out, in0, in1, op, reads, writes):
        self.kb.op(eng, lambda e: e.tensor_tensor(out=out, in0=in0, in1=in1, op=op), reads=reads, writes=writes)

    def ts(self, eng, out, in0, s1, s2, op0, op1, reads, writes, accum=None):
        if accum is None:
            self.kb.op(eng, lambda e: e.tensor_scalar(out=out, in0=in0, scalar1=s1, scalar2=s2, op0=op0, op1=op1), reads=reads, writes=writes)
        else:
            self.kb.op(eng, lambda e: e.tensor_scalar(out=out, in0=in0, scalar1=s1, scalar2=s2, op0=op0, op1=op1, accum_out=accum), reads=reads, writes=writes)

    def stt(self, out, in0, scalar, in1, op0, op1, reads, writes):
        self.kb.op("dve", lambda e: e.scalar_tensor_tensor(out=out, in0=in0, scalar=scalar, in1=in1, op0=op0, op1=op1), reads=reads, writes=writes)

    def cp(self, eng, out, in_, reads, writes):
        if eng == "act":
            self.kb.op("act", lambda e: e.copy(out=out, in_=in_), reads=reads, writes=writes)
        else:
            self.kb.op(eng, lambda e: e.tensor_copy(out=out, in_=in_), reads=reads, writes=writes)

    def ld(self, q, out, in_, reads, writes):
        return self.kb.dma(q, lambda e: e.dma_start(out=out, in_=in_), reads=reads, writes=writes)

    def bcast_load(self, dst, src_row, reads):
        self.ld("sp", dst[:, :], src_row.partition_broadcast(128), reads, [dst])

    def load_w_cast(self, dst, src2d, ncols, reads):
        step = 1024
        for c0 in range(0, ncols, step):
            c1 = min(ncols, c0 + step)
            self.ld("pool", dst[:, :, c0:c1], src2d[:, c0:c1].rearrange("(c p) n -> p c n", p=128), reads, [dst])

    def queue_conversions(self):
        inp = self.inp
        for j in range(2):
            srcs = []
            if 2 * j < self.NL:
                srcs.append((NEXP, inp["ev_ffn_w_gate"][j], inp["ev_ffn_w_up"][j], inp["ev_ffn_w_down"][j]))
            if 2 * j + 1 < self.NL:
                for e in range(NEXP):
                    srcs.append((e, inp["od_moe_w_gate"][j, e], inp["od_moe_w_up"][j, e], inp["od_moe_w_down"][j, e]))
            for (slot, g, u, d) in srcs:
                for (tab, src) in ((self.Tg, g), (self.Tu, u)):
                    for c0 in (0, 4):
                        self.conv_q.append((j, slot, tab[j][slot],
                                            tab[j][slot][:, :].rearrange("(p q) (c f) -> p q c f", q=11, f=256)[:, :, c0:c0 + 4, :],
                                            src.rearrange("(c p) (q f) -> p q c f", p=128, f=256)[:, :, c0:c0 + 4, :]))
                self.conv_q.append((j, slot, self.Td[j][slot],
                                    self.Td[j][slot][:, :].rearrange("(p q) (a n) -> p q a n", q=11, n=1024),
                                    d.rearrange("(q a p) n -> p q a n", a=2, p=128)))

    def pump(self, n=1, upto=None):
        while self.conv_q and (n > 0 or (upto is not None and (self.conv_q[0][0], self.conv_q[0][1] != NEXP) <= upto)):
            j, slot, t, dst, src = self.conv_q.pop(0)
            self.kb.dma("pool", lambda e, dst=dst, src=src: e.dma_start(out=dst, in_=src), reads=[], writes=[t], ring="bulk")
            n -= 1

    def ln_tile(self, hsrc, hres, Xold, Xnew, XTnew, n0, gbc, bbc, bufs, router=None):
        kb = self.kb
        xo, z, st, xtb = bufs["xo"], bufs["z"], bufs["st"], bufs["xtb"]
        self.ld("sp", xo[:, :], Xold[n0:n0 + 128, :], [Xold], [xo])
        for h in range(2):
            self.stt(z[:, h * 512:(h + 1) * 512], xo[:, h * 512:(h + 1) * 512], ALPHA, hsrc[h], ALU.mult, ALU.add, [xo] + hres, [z])
        for h in range(2):
            kb.op("dve", lambda e, h=h: e.bn_stats(out=st[:, h * 6:(h + 1) * 6], in_=z[:, h * 512:(h + 1) * 512]), reads=[z], writes=[st])
        kb.op("dve", lambda e: e.bn_aggr(out=st[:, 12:14], in_=st[:, 0:12]), reads=[st], writes=[st])
        self.act(st[:, 14:15], st[:, 13:14], AF.Sqrt, [st], [st], bias=bufs["eps"][:, 0:1], scale=1.0)
        kb.op("dve", lambda e: e.reciprocal(out=st[:, 15:16], in_=st[:, 14:15]), reads=[st], writes=[st])
        self.ts("dve", st[:, 16:17], st[:, 12:13], -1.0, st[:, 15:16], ALU.mult, ALU.mult, [st], [st])
        self.act(xo[:, :], z[:, :], AF.Identity, [z, st], [xo], scale=st[:, 15:16], bias=st[:, 16:17])
        self.tt("dve", xo[:, :], xo[:, :], gbc[:, :], ALU.mult, [xo, gbc], [xo])
        self.tt("pool", xo[:, :], xo[:, :], bbc[:, :], ALU.add, [xo, bbc], [xo])
        self.ld("sp", Xnew[n0:n0 + 128, :], xo[:, :], [xo], [Xnew])
        if XTnew is not None:
            pb = [self.PB[6], self.PB[7]]
            for c in range(DC):
                self.tp(pb[c // 4][:, (c % 4) * 128:(c % 4 + 1) * 128], xo[:, c * 128:(c + 1) * 128], self.identf[:, :], [xo, self.identf], [pb[c // 4]])
            for h in range(2):
                self.cp("act", xtb[:, h * 4:(h + 1) * 4, :], pb[h][:, :].rearrange("p (c t) -> p c t", t=128), [pb[h]], [xtb])
            self.ld("sp", XTnew[:, n0:n0 + 128].rearrange("(c p) t -> p c t", p=128), xtb[:, :, :], [xtb], [XTnew])
            if router is not None:
                self.router_tile(pb, router, n0, bufs)

    def router_tile(self, pb, router, n0, bufs):
        kb = self.kb
        wrh, wrl, brb = router
        xtl, st, xtb = bufs["xtl"], bufs["st"], bufs["xtb"]
        for h in range(2):
            self.tt("dve", xtl[:, h * 4:(h + 1) * 4, :], pb[h][:, :].rearrange("p (c t) -> p c t", t=128), xtb[:, h * 4:(h + 1) * 4, :],
                    ALU.subtract, [pb[h], xtb], [xtl])
        lg = self.PB[5]
        k = 0
        for c in range(DC):
            for (xa, wa) in ((xtb, wrh), (xtb, wrl), (xtl, wrh)):
                self.mm(lg[:, 0:8], xa[:, c, :], wa[:, c, :], k == 0, k == 3 * DC - 1, [xa, wa], [lg])
                k += 1
        tix = n0 // 128
        L = st[:, 24:32]
        self.tt("dve", L, lg[:, 0:8], brb[:, :], ALU.add, [lg, brb], [st])
        kb.op("dve", lambda e: e.max(out=st[:, 32:40], in_=L), reads=[st], writes=[st])
        self.tt("dve", st[:, 40:41], st[:, 33:34], st[:, 32:33], ALU.subtract, [st], [st])
        self.act(st[:, 41:42], st[:, 40:41], AF.Exp, [st], [st])
        self.ts("dve", st[:, 42:43], st[:, 41:42], 1.0, None, ALU.add, ALU.bypass, [st], [st])
        kb.op("dve", lambda e: e.reciprocal(out=st[:, 43:44], in_=st[:, 42:43]), reads=[st], writes=[st])
        self.tt("dve", st[:, 44:45], st[:, 41:42], st[:, 43:44], ALU.mult, [st], [st])
        self.ts("dve", self.sel12[:, tix, 0:8], L, st[:, 32:33], None, ALU.is_equal, ALU.bypass, [st], [self.sel12])
        self.ts("dve", self.sel12[:, tix, 8:16], L, st[:, 33:34], None, ALU.is_equal, ALU.bypass, [st], [self.sel12])
        self.cp("dve", self.gates[:, tix, :], st[:, 43:45], [st], [self.gates])

    def ln_bufs(self):
        ar = self.ar
        b = {"xo": ar.alloc([128, D], F32, "xo"), "z": ar.alloc([128, D], F32, "z"), "st": ar.alloc([128, 64], F32, "st"),
             "xtb": ar.alloc([128, DC, 128], BF16, "xtb"), "eps": ar.alloc([128, 1], F32, "eps"),
             "gbc": ar.alloc([128, D], F32, "gbc"), "bbc": ar.alloc([128, D], F32, "bbc")}
        self.kb.op("pool", lambda e: e.memset(b["eps"][:, :], LN_EPS), writes=[b["eps"]])
        return b

    def ffn_setup(self):
        ar = self.ar
        f = {"xT": [ar.alloc([128, DC, 512], BF16, "xT%d" % i) for i in range(2)],
             "hT": ar.alloc([128, FC, 512], BF16, "hT"),
             "ring": [ar.alloc([128, 2048], BF16, "ring%d" % i) for i in range(12)],
             "sg": [ar.alloc([128, 512], BF16, "sg%d" % i) for i in range(2)],
             "ri": 0}
        return f

    def ffn_block(self, f, xT, load):
        self.ffn_p1(f, xT, load)
        self.ffn_p2(f, load)

    def ffn_p1(self, f, xT, load):
        PB = self.PB
        hT = f["hT"]
        k = 0
        for q in range(11):
            gs = f["ring"][f["ri"] % 12]; f["ri"] += 1
            us = f["ring"][f["ri"] % 12]; f["ri"] += 1
            load("g", q, gs)
            load("u", q, us)
            gv = gs[:, :].rearrange("p (c n) -> p c n", n=256)
            uv = us[:, :].rearrange("p (c n) -> p c n", n=256)
            for fl in range(2):
                fi = 2 * q + fl
                pg, pu = PB[(k % 2) * 2], PB[(k % 2) * 2 + 1]
                for c in range(DC):
                    self.mm(pg[:, :], gv[:, c, fl * 128:(fl + 1) * 128], xT[:, c, :], c == 0, c == DC - 1, [gs, xT], [pg])
                for c in range(DC):
                    self.mm(pu[:, :], uv[:, c, fl * 128:(fl + 1) * 128], xT[:, c, :], c == 0, c == DC - 1, [us, xT], [pu])
                sg = f["sg"][k % 2]
                self.act(sg[:, :], pg[:, :], AF.Silu, [pg], [sg])
                self.tt("dve", hT[:, fi, :], sg[:, :], pu[:, :], ALU.mult, [sg, pu], [hT])
                k += 1

    def ffn_p2(self, f, load):
        PB = self.PB
        hT = f["hT"]
        for q in range(11):
            ds = f["ring"][f["ri"] % 12]; f["ri"] += 1
            load("d", q, ds)
            dv = ds[:, :].rearrange("p (a n) -> p a n", n=D)
            for a_ in range(2):
                fi = 2 * q + a_
                for u in range(4):
                    for h in range(2):
                        self.mm(PB[2 * u + h][:, :], hT[:, fi, u * 128:(u + 1) * 128], dv[:, a_, h * 512:(h + 1) * 512],
                                fi == 0, fi == FC - 1, [hT, ds], [PB[2 * u + h]])

    def static_loader(self, j, slot):
        tabs = {"g": self.Tg[j][slot], "u": self.Tu[j][slot], "d": self.Td[j][slot]}

        def load(kind, q, dst):
            t = tabs[kind]
            self.ld("sp", dst[:, :], t[:, :].rearrange("(p q) n -> p q n", q=11)[:, q, :], [t], [dst])
        return load

    def ffn_phase(self, j, Xold, Xnew, XTold, XTnew, lng, lnb):
        kb, ar = self.kb, self.ar
        self.pump(0, upto=(j, 0))
        ar.reset()
        f = self.ffn_setup()
        lb = self.ln_bufs()
        self.bcast_load(lb["gbc"], lng, [])
        self.bcast_load(lb["bbc"], lnb, [])
        accs = [ar.alloc([128, 4, D], F32, "acc%d" % i) for i in range(2)]
        load = self.static_loader(j, NEXP)
        nblk = self.N // 512

        def lns(blk):
            acc = accs[blk % 2]
            for u in range(4):
                hs = [acc[:, u, 0:512], acc[:, u, 512:1024]]
                self.ln_tile(hs, [acc], Xold, Xnew, XTnew, blk * 512 + u * 128, lb["gbc"], lb["bbc"], lb)
        for blk in range(nblk):
            n0 = blk * 512
            xT = f["xT"][blk % 2]
            acc = accs[blk % 2]
            self.ld("sp", xT[:, :, :], XTold[:, n0:n0 + 512].rearrange("(c p) t -> p c t", p=128), [XTold], [xT])
            self.ffn_p1(f, xT, load)
            if blk > 0:
                lns(blk - 1)
            self.ffn_p2(f, load)
            self.pump(3)
            for u in range(4):
                for h in range(2):
                    self.cp("act" if h == 0 else "dve", acc[:, u, h * 512:(h + 1) * 512], self.PB[2 * u + h][:, :], [self.PB[2 * u + h]], [acc])
        lns(nblk - 1)

    def route(self, Xcur):
        kb, ar, PB = self.kb, self.ar, self.PB
        NTT, NG = self.NTT, self.NG
        I32 = mybir.dt.int32
        ar.reset()
        SEL = ar.alloc([128, NTT, 8], BF16, "SEL")
        RANK = ar.alloc([128, NTT, 8], F32, "RANK")
        carry = ar.alloc([128, 8], F32, "carry")
        w = ar.alloc([128, 256], F32, "rw")
        big = ar.alloc([128, NG, 11], F32, "rbig")
        t3 = ar.alloc([128, NTT, 8], F32, "rt3")
        pf = ar.alloc([128, NTT, 2], F32, "rpf")
        self.tt("dve", SEL[:, :, :], self.sel12[:, :, 0:8], self.sel12[:, :, 8:16], ALU.add, [self.sel12], [SEL])
        kb.op("pool", lambda e: e.memset(carry[:, :], 0.0), writes=[carry])
        for i in range(NTT):
            p = PB[i % 2]
            self.mm(p[:, 0:8], self.ustb[:, :], SEL[:, i, :], True, True, [self.ustb, SEL], [p])
            self.mm(p[:, 8:16], self.onesb[:, :], SEL[:, i, :], True, True, [self.onesb, SEL], [p])
            self.tt("dve", RANK[:, i, :], p[:, 0:8], carry[:, :], ALU.add, [p, carry], [RANK])
            self.tt("dve", carry[:, :], p[:, 8:16], carry[:, :], ALU.add, [p, carry], [carry])
        cst = self.cstf
        THR, GT, QC, P11 = cst[:, 960:968], cst[:, 968:968 + NG], cst[:, 992:1003], cst[:, 1003:1004]
        ge, pn, incl, off = w[:, 0:8], w[:, 8:16], w[:, 16:24], w[:, 24:32]
        cmp_ = w[:, 64:128].rearrange("p (e k) -> p e k", k=8)
        self.tt("dve", cmp_, carry[:, :].unsqueeze(2).to_broadcast([128, 8, 8]), THR.unsqueeze(1).to_broadcast([128, 8, 8]), ALU.is_gt, [carry, cst], [w])
        kb.op("dve", lambda e: e.tensor_reduce(out=ge, in_=cmp_, axis=AX.X, op=ALU.add), reads=[w], writes=[w])
        self.ts("dve", pn, ge, 512.0, None, ALU.mult, ALU.bypass, [w], [w])
        kb.op("dve", lambda e: e.tensor_tensor_scan(out=incl, data0=self.onesf[:, 0:8], data1=pn, initial=0.0, op0=ALU.mult, op1=ALU.add), reads=[w, self.onesf], writes=[w])
        self.tt("dve", off, incl, pn, ALU.subtract, [w], [w])
        self.tt("dve", t3[:, :, :], RANK[:, :, :], off.unsqueeze(1).to_broadcast([128, NTT, 8]), ALU.add, [RANK, w], [t3])
        for k in range(2):
            self.tt("dve", RANK[:, :, :], t3[:, :, :], self.sel12[:, :, k * 8:(k + 1) * 8], ALU.mult, [t3, self.sel12], [RANK])
            kb.op("dve", lambda e, k=k: e.tensor_reduce(out=pf[:, :, k], in_=RANK[:, :, :], axis=AX.X, op=ALU.add), reads=[RANK], writes=[pf])
        self.cp("dve", self.pidx[:, :, :], pf[:, :, :], [pf], [self.pidx])
        cmp2 = big[:, :, 0:8]
        self.tt("dve", cmp2, GT.unsqueeze(2).to_broadcast([128, NG, 8]), incl.unsqueeze(1).to_broadcast([128, NG, 8]), ALU.is_ge, [cst, w], [big])
        eg = w[:, 32:32 + NG]
        kb.op("dve", lambda e: e.tensor_reduce(out=eg, in_=cmp2, axis=AX.X, op=ALU.add), reads=[big], writes=[w])
        self.ts("dve", eg, eg, 7.0, 1408.0, ALU.min, ALU.mult, [w], [w])
        self.ts("dve", eg, eg, P11, None, ALU.add, ALU.bypass, [w, cst], [w])
        self.tt("dve", big[:, :, :], eg.unsqueeze(2).to_broadcast([128, NG, 11]), QC.unsqueeze(1).to_broadcast([128, NG, 11]), ALU.add, [w, cst], [big])
        self.cp("dve", self.idxq[:, :, :], big[:, :, :], [big], [self.idxq])
        xos = [ar.alloc([128, D], F32, "sxo%d" % i) for i in range(2)]
        xbs = [ar.alloc([128, D], BF16, "sxb%d" % i) for i in range(2)]
        for i in range(NTT):
            xo, xb = xos[i % 2], xbs[i % 2]
            self.ld("sp", xo[:, :], Xcur[i * 128:(i + 1) * 128, :], [Xcur], [xo])
            self.cp("act", xb[:, :], xo[:, :], [xo], [xb])
            for k in range(2):
                self.kb.dma("pool", lambda e, xb=xb, i=i, k=k: e.indirect_dma_start(
                    out=self.XS[:, :], out_offset=bass.IndirectOffsetOnAxis(self.pidx[:, i, k:k + 1], 0), in_=xb[:, :], in_offset=None),
                    reads=[xb, self.pidx], writes=[self.XS])

    def moe_sparse(self, j, Xold, Xnew, XTnew, lng, lnb):
        kb, ar, PB = self.kb, self.ar, self.PB
        NG = self.NG
        self.pump(0, upto=(j, 1))
        ar.reset()
        f = self.ffn_setup()
        xss = [ar.alloc([128, 4, D], BF16, "xs%d" % i) for i in range(2)]
        ys = ar.alloc([128, 4, D], F32, "ys")
        tabs = {"g": self.Tg[j], "u": self.Tu[j], "d": self.Td[j]}
        fulls = {"g": self.TgF[j], "u": self.TuF[j], "d": self.TdF[j]}
        tres = [t for k in tabs for t in tabs[k][0:NEXP]]
        for g in range(NG):
            xs = xss[g % 2]
            xT = f["xT"][g % 2]
            self.ld("sp", xs[:, :, :], self.XS[g * 512:(g + 1) * 512, :].rearrange("(u p) d -> p u d", p=128), [self.XS], [xs])
            for u in range(4):
                pt = PB[4 + u]
                ptb = pt[:, :].bitcast(BF16)
                for c in range(DC):
                    self.tp(ptb[:, c * 128:(c + 1) * 128], xs[:, u, c * 128:(c + 1) * 128], self.identb[:, :], [xs, self.identb], [pt])
                self.cp("act" if u % 2 == 0 else "dve", xT[:, :, u * 128:(u + 1) * 128], ptb.rearrange("p (c t) -> p c t", t=128), [pt], [xT])

            def load(kind, q, dst, g=g):
                src = fulls[kind].ap[:, :]
                self.kb.dma("pool", lambda e, src=src, dst=dst, q=q: e.indirect_dma_start(
                    out=dst[:, :], out_offset=None, in_=src, in_offset=bass.IndirectOffsetOnAxis(self.idxq[:, g, q:q + 1], 0)),
                    reads=tres + [self.idxq], writes=[dst])
            self.ffn_block(f, xT, load)
            for u in range(4):
                for h in range(2):
                    self.cp("act" if h == 0 else "dve", ys[:, u, h * 512:(h + 1) * 512], PB[2 * u + h][:, :], [PB[2 * u + h]], [ys])
            self.ld("sp", self.YS[g * 512:(g + 1) * 512, :].rearrange("(u p) d -> p u d", p=128), ys[:, :, :], [ys], [self.YS])
        ar.reset()
        lb = self.ln_bufs()
        self.bcast_load(lb["gbc"], lng, [])
        self.bcast_load(lb["bbc"], lnb, [])
        y1s = [ar.alloc([128, D], F32, "y1_%d" % i) for i in range(2)]
        y2s = [ar.alloc([128, D], F32, "y2_%d" % i) for i in range(2)]
        for i in range(self.NTT):
            y1, y2 = y1s[i % 2], y2s[i % 2]
            for k, y in ((0, y1), (1, y2)):
                self.kb.dma("pool", lambda e, y=y, i=i, k=k: e.indirect_dma_start(
                    out=y[:, :], out_offset=None, in_=self.YS[:, :], in_offset=bass.IndirectOffsetOnAxis(self.pidx[:, i, k:k + 1], 0)),
                    reads=[self.YS, self.pidx], writes=[y])
            self.ts("dve", y1[:, :], y1[:, :], self.gates[:, i, 0:1], None, ALU.mult, ALU.bypass, [y1, self.gates], [y1])
            self.stt(y1[:, :], y2[:, :], self.gates[:, i, 1:2], y1[:, :], ALU.mult, ALU.add, [y2, self.gates, y1], [y1])
            self.ln_tile([y1[:, 0:512], y1[:, 512:1024]], [y1], Xold, Xnew, XTnew, i * 128, lb["gbc"], lb["bbc"], lb)

    def outproj_phase(self, w_out, Xold, Xnew, XTnew, lng, lnb, router=None):
        kb, ar = self.kb, self.ar
        ar.reset()
        lb = self.ln_bufs()
        self.bcast_load(lb["gbc"], lng, [])
        self.bcast_load(lb["bbc"], lnb, [])
        W = ar.alloc([128, DC, D], BF16, "wout")
        self.load_w_cast(W, w_out, D, [])
        rt = None
        if router is not None:
            wr = ar.alloc([128, DC, 8], F32, "wr")
            wrh = ar.alloc([128, DC, 8], BF16, "wrh")
            wrl = ar.alloc([128, DC, 8], BF16, "wrl")
            brb = ar.alloc([128, 8], F32, "brb")
            lb["xtl"] = ar.alloc([128, DC, 128], BF16, "xtl")
            self.ld("sp", wr[:, :, :], router[0].rearrange("(c p) n -> p c n", p=128), [], [wr])
            self.cp("dve", wrh[:, :, :], wr[:, :, :], [wr], [wrh])
            self.tt("dve", wrl[:, :, :], wr[:, :, :], wrh[:, :, :], ALU.subtract, [wr, wrh], [wrl])
            self.bcast_load(brb, router[1], [])
            rt = (wrh, wrl, brb)
        mixs = [ar.alloc([128, D], BF16, "mix%d" % i) for i in range(2)]
        mixT = [ar.alloc([128, DC, 128], BF16, "mixT%d" % i) for i in range(2)]
        PB = self.PB
        ntile = self.N // 128

        def front(t):
            n0 = t * 128
            mx, mt = mixs[t % 2], mixT[t % 2]
            self.ld("sp", mx[:, :], self.MIXD[n0:n0 + 128, :], [self.MIXD], [mx])
            pt = PB[4 + (t % 2)]
            ptb = pt[:, :].bitcast(BF16)
            for c in range(DC):
                self.tp(ptb[:, c * 128:(c + 1) * 128], mx[:, c * 128:(c + 1) * 128], self.identb[:, :], [mx, self.identb], [pt])
            self.cp("act", mt[:, :, :], ptb.rearrange("p (c t) -> p c t", t=128), [pt], [mt])
            hb = [PB[(t % 2) * 2], PB[(t % 2) * 2 + 1]]
            for h in range(2):
                for c in range(DC):
                    self.mm(hb[h][:, :], mt[:, c, :], W[:, c, h * 512:(h + 1) * 512], c == 0, c == DC - 1, [mt, W], [hb[h]])
        front(0)
        for t in range(ntile):
            if t + 1 < ntile:
                front(t + 1)
            hb = [PB[(t % 2) * 2], PB[(t % 2) * 2 + 1]]
            self.ln_tile([hb[0][:, :], hb[1][:, :]], hb, Xold, Xnew, XTnew, t * 128, lb["gbc"], lb["bbc"], lb, router=rt)

    def init_xt(self, Xin, XTnew):
        ar = self.ar
        ar.reset()
        xos = [ar.alloc([128, D], F32, "ixo%d" % i) for i in range(2)]
        xtbs = [ar.alloc([128, DC, 128], BF16, "ixt%d" % i) for i in range(2)]
        for t in range(self.N // 128):
            n0 = t * 128
            xo, xtb = xos[t % 2], xtbs[t % 2]
            self.ld("sp", xo[:, :], Xin[n0:n0 + 128, :], [Xin], [xo])
            pb = [self.PB[(t % 2) * 2], self.PB[(t % 2) * 2 + 1]]
            for c in range(DC):
                self.tp(pb[c // 4][:, (c % 4) * 128:(c % 4 + 1) * 128], xo[:, c * 128:(c + 1) * 128], self.identf[:, :], [xo, self.identf], [pb[c // 4]])
            for h in range(2):
                self.cp("act" if h == 0 else "dve", xtb[:, h * 4:(h + 1) * 4, :], pb[h][:, :].rearrange("p (c t) -> p c t", t=128), [pb[h]], [xtb])
            self.ld("sp", XTnew[:, n0:n0 + 128].rearrange("(c p) t -> p c t", p=128), xtb[:, :, :], [xtb], [XTnew])

    def even_scratch(self):
        kb, N = self.kb, self.N
        if hasattr(self, "PQ"):
            return
        self.PQ = kb.dram("PQ", [2048, N], BF16)
        self.PBQK = kb.dram("PBQK", [512, N], F32)
        self.CNTd = kb.dram("CNTd", [128, N], BF16)
        self.KITd = kb.dram("KITd", [128, N], BF16)
        self.WId = kb.dram("WId", [N, 8], F32)
        self.BGTd = kb.dram("BGTd", [16, N], BF16)
        self.BVd = kb.dram("BVd", [N, 1024], BF16)

    def even_proj(self, j, XTold):
        kb, ar, PB = self.kb, self.ar, self.PB
        inp = self.inp
        self.even_scratch()
        ar.reset()
        WIN = ar.alloc([128, DC, EVEN_IN], BF16, "WIN")
        self.load_w_cast(WIN, inp["ev_w_in"][j], EVEN_IN, [])
        kvg = ar.alloc([128, 128], F32, "kvg"); self.bcast_load(kvg, inp["ev_a_kv_norm_g"][j:j + 1, :], [])
        klg = ar.alloc([128, 64], F32, "klg"); self.bcast_load(klg, inp["ev_a_kidx_ln_g"][j:j + 1, :], [])
        klb = ar.alloc([128, 64], F32, "klb"); self.bcast_load(klb, inp["ev_a_kidx_ln_b"][j:j + 1, :], [])
        epsr = ar.alloc([128, 2], F32, "epsr")
        kb.op("pool", lambda e: e.memset(epsr[:, 0:1], RMS_EPS), writes=[epsr])
        kb.op("pool", lambda e: e.memset(epsr[:, 1:2], LN_EPS), writes=[epsr])
        xTs = [ar.alloc([128, DC, 512], BF16, "xT%d" % i) for i in range(2)]
        pqs = [ar.alloc([128, 8, 512], BF16, "pqs%d" % i) for i in range(2)]
        pbs = [ar.alloc([128, 4, 512], F32, "pbs%d" % i) for i in range(2)]
        bgs = [ar.alloc([16, 512], BF16, "bgs%d" % i) for i in range(2)]
        cns = [ar.alloc([128, 512], BF16, "cns%d" % i) for i in range(2)]
        kis = [ar.alloc([128, 512], BF16, "kis%d" % i) for i in range(2)]
        wis = [ar.alloc([128, 4, 8], F32, "wis%d" % i) for i in range(2)]
        bvs = [ar.alloc([128, 4, 1024], BF16, "bvs%d" % i) for i in range(2)]
        st = ar.alloc([128, 32], F32, "st")
        junk = ar.alloc([128, 128], F32, "junk")
        cn = ar.alloc([128, 128], BF16, "cn")
        kin = ar.alloc([128, 64], F32, "kin")
        kin2 = ar.alloc([128, 128], BF16, "kin2")
        WSC = (8.0 ** -0.5) * (64.0 ** -0.5)
        for blk in range(self.N // 512):
            n0 = blk * 512
            b2 = blk % 2
            xT = xTs[b2]
            self.pump(2)
            self.ld("sp", xT[:, :, :], XTold[:, n0:n0 + 512].rearrange("(c p) t -> p c t", p=128), [XTold], [xT])
            fm = [(g * 128, 128, pqs[b2], g, 1.0) for g in range(4)] + [(640 + g * 128, 128, pqs[b2], 4 + g, 1.0) for g in range(4)] + \
                 [(1224 + g * 128, 128, pbs[b2], g, 0.125) for g in range(2)] + [(1480 + g * 128, 128, pbs[b2], 2 + g, 1.0) for g in range(2)] + \
                 [(2248, 16, bgs[b2], None, 1.0)]
            for gi, (c0, M, dst, slot, sc) in enumerate(fm):
                pb = PB[gi % 2]
                for c in range(DC):
                    self.mm(pb[0:M, :], WIN[:, c, c0:c0 + M], xT[:, c, :], c == 0, c == DC - 1, [WIN, xT], [pb])
                o = dst[0:M, :] if slot is None else dst[:, slot, :]
                if gi % 2 == 0:
                    self.act(o, pb[0:M, :], AF.Copy, [pb], [dst], scale=sc)
                else:
                    self.ts("dve", o, pb[0:M, :], sc, None, ALU.mult, ALU.bypass, [pb], [dst])
            self.ld("sp", self.PQ[0:1024, n0:n0 + 512].rearrange("(g p) t -> p g t", p=128), pqs[b2][:, :, :], [pqs[b2]], [self.PQ])
            self.ld("sp", self.PBQK[:, n0:n0 + 512].rearrange("(g p) t -> p g t", p=128), pbs[b2][:, :, :], [pbs[b2]], [self.PBQK])
            self.ld("sp", self.BGTd[:, n0:n0 + 512], bgs[b2][:, :], [bgs[b2]], [self.BGTd])
            for u in range(4):
                xs = lambda c: xT[:, c, u * 128:(u + 1) * 128]
                p2 = PB[2]
                for c in range(DC):
                    self.mm(p2[:, 0:128], xs(c), WIN[:, c, 512:640], c == 0, c == DC - 1, [xT, WIN], [p2])
                for c in range(DC):
                    self.mm(p2[:, 128:200], xs(c), WIN[:, c, 1152:1224], c == 0, c == DC - 1, [xT, WIN], [p2])
                self.act(junk[:, :], p2[:, 0:128], AF.Square, [p2], [junk, st], accum_out=st[:, 0:1])
                self.act(st[:, 1:2], st[:, 0:1], AF.Sqrt, [st, epsr], [st], scale=1.0 / 128, bias=epsr[:, 0:1])
                kb.op("dve", lambda e: e.reciprocal(out=st[:, 2:3], in_=st[:, 1:2]), reads=[st], writes=[st])
                self.stt(cn[:, :], p2[:, 0:128], st[:, 2:3], kvg[:, :], ALU.mult, ALU.mult, [p2, st, kvg], [cn])
                p3 = PB[3]
                p3b = p3[:, :].bitcast(BF16)
                self.tp(p3b[:, 0:128], cn[:, :], self.identb[:, :], [cn, self.identb], [p3])
                kb.op("dve", lambda e: e.bn_stats(out=st[:, 4:10], in_=p2[:, 128:192]), reads=[p2], writes=[st])
                kb.op("dve", lambda e: e.bn_aggr(out=st[:, 10:12], in_=st[:, 4:10]), reads=[st], writes=[st])
                self.act(st[:, 12:13], st[:, 11:12], AF.Sqrt, [st, epsr], [st], scale=1.0, bias=epsr[:, 1:2])
                kb.op("dve", lambda e: e.reciprocal(out=st[:, 13:14], in_=st[:, 12:13]), reads=[st], writes=[st])
                self.ts("dve", st[:, 14:15], st[:, 10:11], -1.0, st[:, 13:14], ALU.mult, ALU.mult, [st], [st])
                self.act(kin[:, :], p2[:, 128:192], AF.Identity, [p2, st], [kin], scale=st[:, 13:14], bias=st[:, 14:15])
                self.tt("dve", kin[:, :], kin[:, :], klg[:, :], ALU.mult, [kin, klg], [kin])
                self.tt("dve", kin2[:, 0:64], kin[:, :], klb[:, :], ALU.add, [kin, klb], [kin2])
                self.tt("dve", kin2[:, 64:128], kin[:, :], klb[:, :], ALU.add, [kin, klb], [kin2])
                self.tp(p3b[:, 128:256], kin2[:, :], self.identb[:, :], [kin2, self.identb], [p3])
                self.cp("act", cns[b2][:, u * 128:(u + 1) * 128], p3b[:, 0:128], [p3], [cns[b2]])
                self.cp("act", kis[b2][:, u * 128:(u + 1) * 128], p3b[:, 128:256], [p3], [kis[b2]])
                self.ts("dve", wis[b2][:, u, :], p2[:, 192:200], WSC, None, ALU.mult, ALU.bypass, [p2], [wis[b2]])
                p4, p5 = PB[4], PB[5]
                for c in range(DC):
                    self.mm(p4[:, :], xs(c), WIN[:, c, 1736:2248], c == 0, c == DC - 1, [xT, WIN], [p4])
                for c in range(DC):
                    self.mm(p5[:, :], xs(c), WIN[:, c, 2264:2776], c == 0, c == DC - 1, [xT, WIN], [p5])
                self.cp("dve", bvs[b2][:, u, 0:512], p4[:, :], [p4], [bvs[b2]])
                self.act(bvs[b2][:, u, 512:1024], p5[:, :], AF.Silu, [p5], [bvs[b2]])
            self.ld("sp", self.CNTd[:, n0:n0 + 512], cns[b2][:, :], [cns[b2]], [self.CNTd])
            self.ld("sp", self.KITd[:, n0:n0 + 512], kis[b2][:, :], [kis[b2]], [self.KITd])
            self.ld("sp", self.WId[n0:n0 + 512, :].rearrange("(u p) e -> p u e", p=128), wis[b2][:, :, :], [wis[b2]], [self.WId])
            self.ld("sp", self.BVd[n0:n0 + 512, :].rearrange("(u p) f -> p u f", p=128), bvs[b2][:, :, :], [bvs[b2]], [self.BVd])

    def dsa(self, j, sq):
        kb, ar, PB = self.kb, self.ar, self.PB
        inp = self.inp
        S, NT = self.S, self.NT
        nb = sq * S
        ar.reset()
        QT = ar.alloc([128, 4, S], BF16, "QT")
        QIT = ar.alloc([128, 4, S], BF16, "QIT")
        CNT = ar.alloc([128, S], BF16, "CNT")
        KIT = ar.alloc([128, S], BF16, "KIT")
        KT2 = ar.alloc([128, S], BF16, "KT2")
        V1 = ar.alloc([128, NT, 65], BF16, "V1")
        WI = ar.alloc([128, NT, 8], F32, "WI")
        WUK2 = ar.alloc([128, 128], BF16, "WUK2")
        WUV = ar.alloc([128, 64], BF16, "WUV")
        self.ld("sp", QT[:, :, :], self.PQ[0:512, nb:nb + S].rearrange("(g p) t -> p g t", p=128), [self.PQ], [QT])
        self.ld("sp", QIT[:, :, :], self.PQ[512:1024, nb:nb + S].rearrange("(g p) t -> p g t", p=128), [self.PQ], [QIT])
        self.ld("sp", CNT[:, :], self.CNTd[:, nb:nb + S], [self.CNTd], [CNT])
        self.ld("sp", KIT[:, :], self.KITd[:, nb:nb + S], [self.KITd], [KIT])
        self.ld("sp", WI[:, :, :], self.WId[nb:nb + S, :].rearrange("(t p) e -> p t e", p=128), [self.WId], [WI])
        self.ld("pool", WUK2[:, 0:64], inp["ev_a_w_uk"][j], [], [WUK2])
        self.ld("pool", WUK2[:, 64:128], inp["ev_a_w_uk"][j], [], [WUK2])
        self.ld("pool", WUV[:, :], inp["ev_a_w_uv"][j], [], [WUV])
        kb.op("pool", lambda e: e.memset(V1[:, :, 64:65], 1.0), writes=[V1])
        for blk in range(S // 512):
            pb = PB[blk % 2]
            self.mm(pb[:, :], WUK2[:, :], CNT[:, blk * 512:(blk + 1) * 512], True, True, [WUK2, CNT], [pb])
            self.cp("act", KT2[:, blk * 512:(blk + 1) * 512], pb[:, :], [pb], [KT2])
        for t in range(NT):
            pb = PB[2 + t % 2]
            self.mm(pb[:, 0:64], CNT[:, t * 128:(t + 1) * 128], WUV[:, :], True, True, [CNT, WUV], [pb])
            self.cp("dve", V1[:, t, 0:64], pb[:, 0:64], [pb], [V1])
        SC = ar.alloc([128, S], F32, "SC")
        JK = ar.alloc([128, S], BF16, "JK")
        MK = ar.alloc([128, S], BF16, "MK")
        MKT = ar.alloc([128, NT, 128], BF16, "MKT")
        Rs = [ar.alloc([128, 512], F32, "R%d" % i) for i in range(2)]
        PTs = [ar.alloc([128, 512], BF16, "PT%d" % i) for i in range(4)]
        bs = ar.alloc([128, 16], F32, "bs")
        mixs = [ar.alloc([128, 512], BF16, "amix%d" % i) for i in range(2)]
        NIT = 16
        K = float(self.n_sel)
        rk = 0
        pk = 0
        for jt in range(NT):
            n = (jt + 1) * 128
            self.pump(1)
            for h in range(8):
                g, hf = h // 2, h % 2
                for k0 in range(0, n, 512):
                    w = min(512, n - k0)
                    pb = PB[rk % 2]
                    R = Rs[rk % 2]
                    rk += 1
                    self.mm(pb[:, 0:w], QIT[hf * 64:(hf + 1) * 64, g, jt * 128:(jt + 1) * 128], KIT[hf * 64:(hf + 1) * 64, k0:k0 + w], True, True, [QIT, KIT], [pb])
                    self.act(R[:, 0:w], pb[:, 0:w], AF.Relu, [pb], [R])
                    if h == 0:
                        self.ts("dve", SC[:, k0:k0 + w], R[:, 0:w], WI[:, jt, 0:1], None, ALU.mult, ALU.bypass, [R, WI], [SC])
                    else:
                        self.stt(SC[:, k0:k0 + w], R[:, 0:w], WI[:, jt, h:h + 1], SC[:, k0:k0 + w], ALU.mult, ALU.add, [R, WI, SC], [SC])
            thr = jt * 128 >= self.n_sel
            if thr:
                kb.op("dve", lambda e, n=n: e.tensor_reduce(out=bs[:, 0:1], in_=SC[:, 0:n], axis=AX.X, op=ALU.max), reads=[SC], writes=[bs])
                kb.op("dve", lambda e, n=n: e.tensor_reduce(out=bs[:, 1:2], in_=SC[:, 0:n], axis=AX.X, op=ALU.min), reads=[SC], writes=[bs])
                self.tt("dve", bs[:, 2:3], bs[:, 0:1], bs[:, 1:2], ALU.subtract, [bs], [bs])
            self.tt("dve", SC[:, jt * 128:n], SC[:, jt * 128:n], self.cbias[:, :], ALU.add, [SC, self.cbias], [SC])
            if thr:
                lo = bs[:, 1:2]
                for it in range(NIT):
                    f = 2.0 ** -(it + 1)
                    self.stt(bs[:, 3:4], bs[:, 2:3], f, lo, ALU.mult, ALU.add, [bs], [bs])
                    self.ts("dve", JK[:, 0:n], SC[:, 0:n], bs[:, 3:4], None, ALU.is_ge, ALU.add, [SC, bs], [JK, bs], accum=bs[:, 4:5])
                    self.ts("dve", bs[:, 5:6], bs[:, 4:5], K - 0.5, f, ALU.is_ge, ALU.mult, [bs], [bs])
                    self.stt(lo, bs[:, 5:6], bs[:, 2:3], lo, ALU.mult, ALU.add, [bs], [bs])
                self.ts("dve", MK[:, 0:n], SC[:, 0:n], lo, None, ALU.is_ge, ALU.bypass, [SC, bs], [MK])
            else:
                self.ts("dve", MK[:, 0:n], SC[:, 0:n], -1e29, None, ALU.is_ge, ALU.bypass, [SC], [MK])
            for i0 in range(0, jt + 1, 8):
                i1 = min(jt + 1, i0 + 8)
                p2 = PB[2]
                p2b = p2[:, :].bitcast(BF16)
                for i in range(i0, i1):
                    self.tp(p2b[:, (i - i0) * 128:(i - i0 + 1) * 128], MK[:, i * 128:(i + 1) * 128], self.identb[:, :], [MK, self.identb], [p2])
                self.cp("act", MKT[:, i0:i1, :], p2b[:, 0:(i1 - i0) * 128].rearrange("p (i t) -> p i t", t=128), [p2], [MKT])
            for b in range(2):
                self.mm(PB[6 + b][:, 0:260], self.zb[:, 0:128], self.zb[:, 0:260], True, False, [self.zb], [PB[6 + b]], skip=True)
            steps = [(i, hf) for i in range(jt + 1) for hf in range(2)]
            pts = {}

            def stA(k, jt=jt):
                nonlocal pk
                i, hf = steps[k]
                ps = PB[2 + pk % 4]
                PT = PTs[pk % 4]
                pk += 1
                pts[k] = PT
                self.mm(ps[:, :], KT2[hf * 64:(hf + 1) * 64, i * 128:(i + 1) * 128], QT[hf * 64:(hf + 1) * 64, :, jt * 128:(jt + 1) * 128], True, True, [KT2, QT], [ps])
                self.act(PT[:, :], ps[:, :], AF.Exp, [ps], [PT], scale=0.125)
                pv = PT[:, :].rearrange("p (g t) -> p g t", t=128)
                self.tt("dve", pv, pv, MKT[:, i, :].unsqueeze(1).to_broadcast([128, 4, 128]), ALU.mult, [PT, MKT], [PT])

            def stB(k, jt=jt):
                i, hf = steps[k]
                PT = pts.pop(k)
                for g in range(4):
                    h = 2 * g + hf
                    ob = PB[6 + h // 4]
                    self.mm(ob[:, (h % 4) * 65:(h % 4) * 65 + 65], PT[:, g * 128:(g + 1) * 128], V1[:, i, :], False, i == jt, [PT, V1], [ob], skip=True)
            LA_ = 2
            for k in range(min(LA_, len(steps))):
                stA(k)
            for k in range(len(steps)):
                if k + LA_ < len(steps):
                    stA(k + LA_)
                stB(k)
            mx = mixs[jt % 2]
            for b in range(2):
                ov = PB[6 + b][:, 0:260].rearrange("p (h d) -> p h d", d=65)
                kb.op("dve", lambda e, ov=ov, b=b: e.reciprocal(out=bs[:, 8 + b * 4:12 + b * 4].unsqueeze(2), in_=ov[:, :, 64:65]), reads=[PB[6 + b]], writes=[bs])
                self.tt("dve", mx[:, b * 256:(b + 1) * 256].rearrange("p (h d) -> p h d", d=64), ov[:, :, 0:64],
                        bs[:, 8 + b * 4:12 + b * 4].unsqueeze(2).to_broadcast([128, 4, 64]), ALU.mult, [PB[6 + b], bs], [mx])
            self.ld("sp", self.MIXD[nb + jt * 128:nb + (jt + 1) * 128, 0:512], mx[:, :], [mx], [self.MIXD])

    def gla(self, j, sq):
        kb, ar, PB = self.kb, self.ar, self.PB
        inp = self.inp
        S = self.S
        NCH = S // 64
        nb = sq * S
        ar.reset()
        BQs = [ar.alloc([64, S], F32, "BQ%d" % i) for i in range(2)]
        BKs = [ar.alloc([64, S], F32, "BK%d" % i) for i in range(2)]
        BG = ar.alloc([16, S], BF16, "BG")
        BV = ar.alloc([64, NCH, 1024], BF16, "BV")
        WG2 = ar.alloc([16, 256], BF16, "WG2")
        nbias = ar.alloc([64, 4], F32, "nbias")
        GNB = ar.alloc([64, 512], F32, "GNB")
        self.ld("sp", BG[:, :], self.BGTd[:, nb:nb + S], [self.BGTd], [BG])
        self.ld("sp", BV[:, :, :], self.BVd[nb:nb + S, :].rearrange("(c p) f -> p c f", p=64), [self.BVd], [BV])
        self.ld("pool", WG2[:, :], inp["ev_b_w_g2"][j], [], [WG2])
        for h in range(4):
            self.ld("sp", nbias[:, h:h + 1], inp["ev_b_b_g"][j, h * 64:(h + 1) * 64].rearrange("(p o) -> p o", o=1), [], [nbias])
        kb.op("dve", lambda e: e.tensor_scalar(out=nbias[:, :], in0=nbias[:, :], scalar1=-1.0, scalar2=None, op0=ALU.mult), reads=[nbias], writes=[nbias])
        self.ld("sp", GNB[:, :], inp["ev_b_norm_g"][j:j + 1, :].partition_broadcast(64), [], [GNB])
        LA = ar.alloc([64, 512], F32, "LA")
        BP = ar.alloc([64, 512], F32, "BP")
        EQ = ar.alloc([64, 512], F32, "EQ")
        EBL = ar.alloc([64, 4, NCH], F32, "EBL")
        QG = ar.alloc([64, 4, S], BF16, "QG")
        KG = ar.alloc([64, 4, S], BF16, "KG")
        KTM = ar.alloc([64, NCH, 256], BF16, "KTM")
        epsr = ar.alloc([64, 1], F32, "epsr")
        kb.op("pool", lambda e: e.memset(epsr[:, :], RMS_EPS), writes=[epsr])
        rst = self.rst[0:64, :]
        for h in range(4):
            BQ, BK = BQs[h % 2], BKs[h % 2]
            self.ld("sp", BQ[:, :], self.PBQK[h * 64:(h + 1) * 64, nb:nb + S], [self.PBQK], [BQ])
            self.ld("sp", BK[:, :], self.PBQK[256 + h * 64:256 + (h + 1) * 64, nb:nb + S], [self.PBQK], [BK])
            for blk in range(S // 512):
                sl = slice(blk * 512, (blk + 1) * 512)
                pb = PB[blk % 2]
                self.mm(pb[0:64, :], WG2[0:16, h * 64:(h + 1) * 64], BG[0:16, sl], True, True, [WG2, BG], [pb])
                self.act(LA[:, :], pb[0:64, :], AF.Exp, [pb, nbias], [LA], scale=-1.0, bias=nbias[:, h:h + 1])
                self.act(LA[:, :], LA[:, :], AF.Ln, [LA], [LA], bias=1.0)
                kb.op("dve", lambda e: e.tensor_tensor_scan(out=BP[:, :], data0=rst, data1=LA[:, :], initial=0.0, op0=ALU.mult, op1=ALU.add),
                      reads=[self.rst, LA], writes=[BP])
                self.act(EQ[:, :], BP[:, :], AF.Exp, [BP], [EQ], scale=-1.0 / 16)
                self.tt("dve", QG[:, h, sl], BQ[:, sl], EQ[:, :], ALU.mult, [BQ, EQ], [QG])
                self.cp("dve", EBL[:, h, blk * 8:(blk + 1) * 8], EQ[:, :].rearrange("p (c j) -> p c j", j=64)[:, :, 63], [EQ], [EBL])
                self.act(EQ[:, :], BP[:, :], AF.Exp, [BP], [EQ], scale=1.0 / 16)
                self.tt("dve", KG[:, h, sl], BK[:, sl], EQ[:, :], ALU.mult, [BK, EQ], [KG])
        for c in range(NCH):
            pb = PB[2 + c % 2]
            pbb = pb[:, :].bitcast(BF16)
            for h in range(4):
                self.tp(pbb[0:64, h * 64:(h + 1) * 64], KG[:, h, c * 64:(c + 1) * 64], self.identb[0:64, 0:64], [KG, self.identb], [pb])
            self.cp("act", KTM[:, c, :], pbb[0:64, 0:256], [pb], [KTM])
            self.tt("pool", BV[:, c, 512:1024], BV[:, c, 512:1024], GNB[:, :], ALU.mult, [BV, GNB], [BV])
        ST32 = ar.alloc([64, 4, 128], F32, "ST32")
        STB = ar.alloc([64, 4, 128], BF16, "STB")
        TMP = ar.alloc([64, 4, 128], F32, "TMP")
        SCMs = [ar.alloc([64, 256], BF16, "SCM%d" % i) for i in range(2)]
        ss = ar.alloc([64, 16], F32, "ss")
        junk = ar.alloc([64, 128], F32, "gjunk")
        mixs = [ar.alloc([64, 512], BF16, "bmix%d" % i) for i in range(2)]
        kb.op("pool", lambda e: e.memset(ST32[:, :, :], 0.0), writes=[ST32])
        kb.op("pool", lambda e: e.memset(STB[:, :, :], 0.0), writes=[STB])
        trig = self.trigb[0:64, :]
        for c in range(NCH):
            cs = slice(c * 64, (c + 1) * 64)
            psc = PB[c % 2]
            for h in range(4):
                self.mm(psc[0:64, h * 64:(h + 1) * 64], KG[:, h, cs], QG[:, h, cs], True, True, [KG, QG], [psc])
            SCM = SCMs[c % 2]
            self.tt("dve", SCM[:, :].rearrange("p (h i) -> p h i", i=64), psc[0:64, 0:256].rearrange("p (h i) -> p h i", i=64),
                    trig.unsqueeze(1).to_broadcast([64, 4, 64]), ALU.mult, [psc, self.trigb], [SCM])
            og = PB[2 + c % 2]
            for h in range(4):
                self.mm(og[0:64, h * 128:(h + 1) * 128], SCM[:, h * 64:(h + 1) * 64], BV[:, c, h * 128:(h + 1) * 128], True, c == 0, [SCM, BV], [og])
                if c > 0:
                    self.mm(og[0:64, h * 128:(h + 1) * 128], QG[:, h, cs], STB[:, h, :], False, True, [QG, STB], [og])
            if c < NCH - 1:
                pst = PB[4 + c % 2]
                for h in range(4):
                    self.mm(pst[0:64, h * 128:(h + 1) * 128], KTM[:, c, h * 64:(h + 1) * 64], BV[:, c, h * 128:(h + 1) * 128], True, True, [KTM, BV], [pst])
                for h in range(4):
                    self.act(TMP[:, h, :], pst[0:64, h * 128:(h + 1) * 128], AF.Identity, [pst, EBL], [TMP], scale=EBL[:, h, c:c + 1])
                    self.stt(ST32[:, h, :], ST32[:, h, :], EBL[:, h, c:c + 1], TMP[:, h, :], ALU.mult, ALU.add, [ST32, EBL, TMP], [ST32])
                self.cp("pool", STB[:, :, :], ST32[:, :, :], [ST32], [STB])
            mx = mixs[c % 2]
            for h in range(4):
                self.act(junk[:, :], og[0:64, h * 128:(h + 1) * 128], AF.Square, [og], [junk, ss], accum_out=ss[:, h:h + 1])
            self.act(ss[:, 4:8], ss[:, 0:4], AF.Sqrt, [ss, epsr], [ss], scale=1.0 / 128, bias=epsr[:, 0:1])
            kb.op("dve", lambda e: e.reciprocal(out=ss[:, 8:12], in_=ss[:, 4:8]), reads=[ss], writes=[ss])
            for h in range(4):
                self.stt(mx[:, h * 128:(h + 1) * 128], og[0:64, h * 128:(h + 1) * 128], ss[:, 8 + h:9 + h], BV[:, c, 512 + h * 128:512 + (h + 1) * 128],
                         ALU.mult, ALU.mult, [og, ss, BV], [mx])
            self.ld("sp", self.MIXD[nb + c * 64:nb + (c + 1) * 64, 512:1024], mx[:, :], [mx], [self.MIXD])

    def odd_proj(self, j, XTold):
        kb, ar, PB = self.kb, self.ar, self.PB
        self.even_scratch()
        ar.reset()
        W = ar.alloc([128, DC, 3072], BF16, "WQKV")
        self.load_w_cast(W, self.inp["od_w_qkv"][j], 3072, [])
        xTs = [ar.alloc([128, DC, 512], BF16, "xT%d" % i) for i in range(2)]
        pqs = [ar.alloc([128, 16, 512], BF16, "pqs%d" % i) for i in range(2)]
        vs = [ar.alloc([128, 4, 1024], BF16, "vs%d" % i) for i in range(2)]
        for blk in range(self.N // 512):
            n0 = blk * 512
            b2 = blk % 2
            xT = xTs[b2]
            self.ld("sp", xT[:, :, :], XTold[:, n0:n0 + 512].rearrange("(c p) t -> p c t", p=128), [XTold], [xT])
            for g in range(16):
                pb = PB[g % 2]
                for c in range(DC):
                    self.mm(pb[:, :], W[:, c, g * 128:(g + 1) * 128], xT[:, c, :], c == 0, c == DC - 1, [W, xT], [pb])
                self.cp("act" if g % 2 == 0 else "dve", pqs[b2][:, g, :], pb[:, :], [pb], [pqs[b2]])
            self.ld("sp", self.PQ[:, n0:n0 + 512].rearrange("(g p) t -> p g t", p=128), pqs[b2][:, :, :], [pqs[b2]], [self.PQ])
            for u in range(4):
                for h in range(2):
                    pb = PB[2 + (2 * u + h) % 4]
                    for c in range(DC):
                        self.mm(pb[:, :], xT[:, c, u * 128:(u + 1) * 128], W[:, c, 2048 + h * 512:2048 + (h + 1) * 512], c == 0, c == DC - 1, [xT, W], [pb])
                    self.cp("act" if h == 0 else "dve", vs[b2][:, u, h * 512:(h + 1) * 512], pb[:, :], [pb], [vs[b2]])
            self.ld("sp", self.BVd[n0:n0 + 512, :].rearrange("(u p) f -> p u f", p=128), vs[b2][:, :, :], [vs[b2]], [self.BVd])

    def diffattn(self, j, sq, lambda_init):
        kb, ar, PB = self.kb, self.ar, self.PB
        inp = self.inp
        S, NT, NB = self.S, self.NT, self.NB
        nb = sq * S
        ar.reset()
        QT = ar.alloc([128, 8, S], BF16, "QT")
        KT = ar.alloc([128, 8, S], BF16, "KT")
        V1 = ar.alloc([128, NT, 8, 129], BF16, "V1")
        self.ld("sp", QT[:, :, :], self.PQ[0:1024, nb:nb + S].rearrange("(g p) t -> p g t", p=128), [self.PQ], [QT])
        self.ld("sp", KT[:, :, :], self.PQ[1024:2048, nb:nb + S].rearrange("(g p) t -> p g t", p=128), [self.PQ], [KT])
        kb.op("pool", lambda e: e.memset(V1[:, :, :, 128:129], 1.0), writes=[V1])
        for t in range(NT):
            self.ld("sp", V1[:, t, :, 0:128], self.BVd[nb + t * 128:nb + (t + 1) * 128, :].rearrange("p (h e) -> p h e", e=128), [self.BVd], [V1])
        LM = ar.alloc([128, 4, 64], F32, "LM")
        lm = ar.alloc([128, 16], F32, "lm")
        SG = ar.alloc([128, 128], F32, "SG")
        epsr = ar.alloc([128, 1], F32, "epsr")
        kb.op("pool", lambda e: e.memset(epsr[:, :], RMS_EPS), writes=[epsr])
        self.ld("sp", LM[:, :, :].rearrange("p a b -> p (a b)"), inp["od_lam"][j:j + 1].rearrange("o a b -> o (a b)").partition_broadcast(128), [], [LM])
        self.bcast_load(SG, inp["od_subln_g"][j:j + 1, :], [])
        kb.op("dve", lambda e: e.tensor_scalar(out=SG[:, :], in0=SG[:, :], scalar1=1.0 - lambda_init, scalar2=None, op0=ALU.mult), reads=[SG], writes=[SG])
        junk = ar.alloc([128, 128], F32, "junk")
        for q in range(2):
            kb.op("dve", lambda e, q=q: e.tensor_tensor(out=junk[:, 0:64], in0=LM[:, 2 * q, :], in1=LM[:, 2 * q + 1, :], op=ALU.mult), reads=[LM], writes=[junk])
            kb.op("dve", lambda e, q=q: e.tensor_reduce(out=lm[:, q:q + 1], in_=junk[:, 0:64], axis=AX.X, op=ALU.add), reads=[junk], writes=[lm])
        self.act(lm[:, 2:4], lm[:, 0:2], AF.Exp, [lm], [lm])
        self.tt("dve", lm[:, 4:5], lm[:, 3:4], lm[:, 2:3], ALU.subtract, [lm], [lm])
        self.ts("dve", lm[:, 5:6], lm[:, 4:5], -lambda_init, None, ALU.add, ALU.bypass, [lm], [lm])
        PTs = [ar.alloc([128, 512], BF16, "PT%d" % i) for i in range(4)]
        dt_ = ar.alloc([128, 128], F32, "dt")
        st = ar.alloc([128, 16], F32, "st")
        mos = [ar.alloc([128, 4, 128], BF16, "mo%d" % i) for i in range(2)]
        pk = 0
        it = 0
        for h in range(8):
            for tb in range(NB):
                self.pump(1)

                def bank(m, u):
                    return PB[4 + 2 * m + u // 2], (u % 2) * 129
                for b in range(4):
                    self.mm(PB[4 + b][:, 0:258], self.zb[:, 0:128], self.zb[:, 0:258], True, False, [self.zb], [PB[4 + b]], skip=True)
                steps = [(m, i) for m in range(2) for i in range(4 * tb + 4)]
                pts = {}

                def stA(k, h=h, tb=tb):
                    nonlocal pk
                    m, i = steps[k]
                    ms = slice(m * 64, (m + 1) * 64)
                    u0 = max(0, i - 4 * tb)
                    w = 512 - u0 * 128
                    ps = PB[pk % 4]
                    PT = PTs[pk % 4]
                    pk += 1
                    pts[k] = PT
                    self.mm(ps[:, 0:w], KT[ms, h, i * 128:(i + 1) * 128], QT[ms, h, tb * 512 + u0 * 128:(tb + 1) * 512], True, True, [KT, QT], [ps])
                    self.act(PT[:, 0:w], ps[:, 0:w], AF.Exp, [ps], [PT], scale=0.125)
                    if i >= 4 * tb:
                        self.tt("pool", PT[:, 0:128], PT[:, 0:128], self.trib[:, :], ALU.mult, [PT, self.trib], [PT])

                def stB(k, h=h, tb=tb):
                    m, i = steps[k]
                    u0 = max(0, i - 4 * tb)
                    PT = pts.pop(k)
                    for u in range(u0, 4):
                        bk, off = bank(m, u)
                        self.mm(bk[:, off:off + 129], PT[:, (u - u0) * 128:(u - u0 + 1) * 128], V1[:, i, h, :], False, i == 4 * tb + u, [PT, V1], [bk], skip=True)
                LA_ = 2
                for k in range(min(LA_, len(steps))):
                    stA(k)
                for k in range(len(steps)):
                    if k + LA_ < len(steps):
                        stA(k + LA_)
                    stB(k)
                mo = mos[it % 2]
                it += 1
                for u in range(4):
                    b0, o0 = bank(0, u)
                    b1, o1 = bank(1, u)
                    kb.op("dve", lambda e, b0=b0, o0=o0: e.reciprocal(out=st[:, 0:1], in_=b0[:, o0 + 128:o0 + 129]), reads=[b0], writes=[st])
                    kb.op("dve", lambda e, b1=b1, o1=o1: e.reciprocal(out=st[:, 1:2], in_=b1[:, o1 + 128:o1 + 129]), reads=[b1], writes=[st])
                    self.tt("dve", st[:, 2:3], st[:, 1:2], lm[:, 5:6], ALU.mult, [st, lm], [st])
                    self.ts("dve", dt_[:, :], b0[:, o0:o0 + 128], st[:, 0:1], None, ALU.mult, ALU.bypass, [b0, st], [dt_])
                    self.stt(dt_[:, :], b1[:, o1:o1 + 128], st[:, 2:3], dt_[:, :], ALU.mult, ALU.add, [b1, st, dt_], [dt_])
                    self.act(junk[:, :], dt_[:, :], AF.Square, [dt_], [junk, st], accum_out=st[:, 3:4])
                    self.act(st[:, 4:5], st[:, 3:4], AF.Sqrt, [st, epsr], [st], scale=1.0 / 128, bias=epsr[:, 0:1])
                    kb.op("dve", lambda e: e.reciprocal(out=st[:, 5:6], in_=st[:, 4:5]), reads=[st], writes=[st])
                    self.stt(mo[:, u, :], dt_[:, :], st[:, 5:6], SG[:, :], ALU.mult, ALU.mult, [dt_, st, SG], [mo])
                self.ld("sp", self.MIXD[nb + tb * 512:nb + (tb + 1) * 512, h * 128:(h + 1) * 128].rearrange("(u p) e -> p u e", p=128), mo[:, :, :], [mo], [self.MIXD])

    def build(self):
        inp = self.inp
        NL = self.NL
        nsub = 2 * NL
        xs = [inp["x"]] + [self.XA if k % 2 == 0 else self.XB for k in range(nsub - 1)] + [self.out]
        ph = self.dbg
        on = lambda p: ph is None or p in ph
        self.queue_conversions()
        self.pump(0, upto=(0, 0))
        if on("init"):
            self.init_xt(inp["x"], self.XT[0])
        k = 0
        for li in range(NL):
            j = li // 2
            if li % 2 == 0:
                if on("eproj"):
                    self.even_proj(j, self.XT[k % 2])
                for sq in range(self.NSEQ):
                    if on("dsa"):
                        self.dsa(j, sq)
                    if on("gla"):
                        self.gla(j, sq)
                if on("oproj"):
                    self.outproj_phase(inp["ev_w_out"][j], xs[k], xs[k + 1], self.XT[(k + 1) % 2], inp["ev_ln1_g"][j:j + 1, :], inp["ev_ln1_b"][j:j + 1, :])
                k += 1
                if on("ffn"):
                    self.ffn_phase(j, xs[k], xs[k + 1], self.XT[k % 2], self.XT[(k + 1) % 2], inp["ev_ln2_g"][j:j + 1, :], inp["ev_ln2_b"][j:j + 1, :])
                k += 1
            else:
                lambda_init = 0.8 - 0.6 * math.exp(-0.3 * li)
                if on("qkv"):
                    self.odd_proj(j, self.XT[k % 2])
                for sq in range(self.NSEQ):
                    if on("dattn"):
                        self.diffattn(j, sq, lambda_init)
                if on("oproj2"):
                    self.outproj_phase(inp["od_w_out"][j], xs[k], xs[k + 1], self.XT[(k + 1) % 2], inp["od_ln1_g"][j:j + 1, :], inp["od_ln1_b"][j:j + 1, :],
                                       router=(inp["od_router_w"][j], inp["od_router_b"][j:j + 1, :]))
                k += 1
                if on("moe"):
                    self.route(xs[k])
                    self.moe_sparse(j, xs[k], xs[k + 1], self.XT[(k + 1) % 2], inp["od_ln2_g"][j:j + 1, :], inp["od_ln2_b"][j:j + 1, :])
                k += 1
        self.kb.barrier()
        self.kb.emit()
        return self.nc


def make_consts():
    c = np.zeros((128, 1024), np.float32)
    p = np.arange(128)
    c[:, 0:128] = np.eye(128, dtype=np.float32)
    c[:, 128:256] = np.where(p[None, :] <= p[:, None], 0.0, -1e30)
    c[:, 256:384] = (p[:, None] <= p[None, :]).astype(np.float32)
    c[:, 384:896] = (np.arange(512)[None, :] % 64 != 0).astype(np.float32)
    c[:, 896:960] = ((p[:, None] % 64) <= np.arange(64)[None, :]).astype(np.float32)
    c[:, 960:968] = 512.0 * np.arange(8)[None, :]
    c[:, 968:992] = 512.0 * np.arange(24)[None, :]
    c[:, 992:1003] = np.arange(11)[None, :]
    c[:, 1003] = 11.0 * p
    return c


_CACHE = {}


def run_prog(inputs, S, NSEQ, NL, ncores, trace=False):
    key = (S, NSEQ, NL)
    if key not in _CACHE:
        p_ = Prog(S, NSEQ, NL)
        _CACHE[key] = (p_.build(), set(p_.inp.keys()))
    nc, names = _CACHE[key]
    x = np.ascontiguousarray(inputs["x"], dtype=np.float32)
    shared = {"cst": make_consts()}
    for name, v in inputs.items():
        if name == "x" or name.startswith("od_lam_") or name not in names:
            continue
        v = np.ascontiguousarray(v, dtype=np.float32)
        if name == "ev_b_norm_g":
            v = v.reshape(2, 512)
        shared[name] = v
    shared["od_lam"] = np.ascontiguousarray(np.stack([inputs["od_lam_q1"], inputs["od_lam_k1"], inputs["od_lam_q2"], inputs["od_lam_k2"]], axis=1), dtype=np.float32)
    in_maps = []
    for c in range(ncores):
        m = dict(shared)
        m["x"] = np.ascontiguousarray(x[c * NSEQ:(c + 1) * NSEQ].reshape(NSEQ * S, D))
        in_maps.append(m)
    res = run_bass_kernel_spmd(nc, in_maps, core_ids=list(range(ncores)), trace=trace)
    out = np.concatenate([r["out"].reshape(NSEQ, S, D) for r in res.results], axis=0)
    return out, res


def kernel(**inputs):
    out, _ = run_prog(inputs, 2048, 2, 4, 8)
    return out.astype(np.float32)
```

```python
import math
import numpy as np
import ml_dtypes
from contextlib import ExitStack
import concourse.bass as bass
import concourse.mybir as mybir
from concourse.bass_utils import run_bass_kernel_spmd

F32 = mybir.dt.float32
BF16 = mybir.dt.bfloat16
U8 = mybir.dt.uint8
ALU = mybir.AluOpType
AF = mybir.ActivationFunctionType
AX = mybir.AxisListType

D = 1024
DC = 8
DEPTH = 4
ALPHA = (2.0 * DEPTH) ** 0.25
LN_EPS = 1e-5
RMS_EPS = 1e-6
DFF = 2816
FC = 22
NEXP = 8
EVEN_IN = 2776


class Res:
    __slots__ = ("ws", "rs", "name")

    def __init__(self, name=""):
        self.ws = {}
        self.rs = {}
        self.name = name


class T:
    def __init__(self, ap, name="", res=None):
        self.ap = ap
        self.name = name
        self.res = res if res is not None else Res(name)

    def __getitem__(self, idx):
        return self.ap[idx]


def _res(x):
    return x.res if isinstance(x, T) else x


class KB:
    ENG = ("pe", "act", "dve", "pool", "sp")
    NDMA = 8

    def __init__(self, nc):
        self.nc = nc
        self.stack = ExitStack()
        self.prog = {e: [] for e in self.ENG}
        self.cnt = {e: 0 for e in self.ENG}
        self.seen = {e: {} for e in self.ENG}
        self.sems = {e: self.stack.enter_context(nc.semaphore("s_" + e)) for e in self.ENG}
        self.dsem, self.dcnt, self.dnext = {}, {}, {}
        for q in ("sp", "pool", "act", "bulk"):
            self.dsem[q] = [self.stack.enter_context(nc.semaphore("d_%s%d" % (q, i))) for i in range(self.NDMA)]
            self.dcnt[q] = [0] * self.NDMA
            self.dnext[q] = 0
        self.nalloc = 0
        self.nins = 0

    def sb(self, shape, dtype, name=None):
        self.nalloc += 1
        name = name or "t%d" % self.nalloc
        h = self.stack.enter_context(self.nc.sbuf_tensor(name, list(shape), dtype))
        return T(h[tuple(slice(None) for _ in shape)], name)

    def ps(self, shape, dtype, name=None):
        self.nalloc += 1
        name = name or "p%d" % self.nalloc
        h = self.stack.enter_context(self.nc.psum_tensor(name, list(shape), dtype))
        return T(h[tuple(slice(None) for _ in shape)], name)

    def dram(self, name, shape, dtype, kind="Internal"):
        h = self.nc.dram_tensor(name, list(shape), dtype, kind=kind)
        return T(h[tuple(slice(None) for _ in shape)], name)

    def _waits(self, eng, reads, writes):
        need = {}
        seen = self.seen[eng]

        def add(d):
            for key, (val, src) in d.items():
                if src == "pe" and eng == "pe":
                    continue
                if seen.get(key, 0) >= val:
                    continue
                if need.get(key, 0) < val:
                    need[key] = val

        for r in reads:
            add(_res(r).ws)
        for w in writes:
            w = _res(w)
            add(w.ws)
            add(w.rs)
        out = []
        for key, val in need.items():
            seen[key] = val
            out.append((key, val))
        return out

    def _commit(self, ev, reads, writes):
        key, val, src = ev
        for r in reads:
            _res(r).rs[key] = (val, src)
        for w in writes:
            w = _res(w)
            w.ws[key] = (val, src)
            w.rs = {}

    def _semof(self, key):
        if isinstance(key, str):
            return self.sems[key]
        return self.dsem[key[0]][key[1]]

    def op(self, eng, fn, reads=(), writes=()):
        waits = self._waits(eng, reads, writes)
        self.cnt[eng] += 1
        ev = (eng, self.cnt[eng], eng)
        self.prog[eng].append((waits, fn, (eng, 1)))
        self._commit(ev, reads, writes)
        self.nins += 1
        return ev

    def dma(self, q, fn, reads=(), writes=(), ring=None):
        ring = ring or q
        i = self.dnext[ring]
        self.dnext[ring] = (i + 1) % self.NDMA
        key = (ring, i)
        waits = self._waits(q, reads, writes)
        prev = self.dcnt[ring][i]
        if prev and self.seen[q].get(key, 0) < prev:
            self.seen[q][key] = prev
            waits.append((key, prev))
        self.dcnt[ring][i] = prev + 16
        ev = (key, prev + 16, "dma")
        self.prog[q].append((waits, fn, (key, 16)))
        self._commit(ev, reads, writes)
        self.nins += 1
        return ev

    def barrier(self, full=False):
        for e in self.ENG:
            waits = []
            for f in self.ENG:
                if f != e and self.cnt[f] and self.seen[e].get(f, 0) < self.cnt[f]:
                    self.seen[e][f] = self.cnt[f]
                    waits.append((f, self.cnt[f]))
            if self.cnt[e] and e != "pe" and self.seen[e].get(e, 0) < self.cnt[e]:
                self.seen[e][e] = self.cnt[e]
                waits.append((e, self.cnt[e]))
            for q in self.dsem:
                if q == "bulk" and not full:
                    continue
                for i in range(self.NDMA):
                    v = self.dcnt[q][i]
                    if v and self.seen[e].get((q, i), 0) < v:
                        self.seen[e][(q, i)] = v
                        waits.append(((q, i), v))
            if waits:
                self.prog[e].append((waits, None, None))

    def emit(self):
        nc = self.nc
        with nc.Block() as block:
            def run(e, name):
                for waits, fn, inc in self.prog[name]:
                    for key, val in waits:
                        e.wait_ge(self._semof(key), val)
                    if fn is not None:
                        fn(e).then_inc(self._semof(inc[0]), inc[1])

            @block.tensor
            def _(e):
                run(e, "pe")

            @block.scalar
            def _(e):
                run(e, "act")

            @block.vector
            def _(e):
                run(e, "dve")

            @block.gpsimd
            def _(e):
                run(e, "pool")

            @block.sync
            def _(e):
                run(e, "sp")
        self.stack.close()


class Arena:
    def __init__(self, kb, nbytes):
        self.kb = kb
        self.t = kb.sb([128, nbytes], U8, name="arena")
        self.n = nbytes
        self.off = 0

    def reset(self):
        self.kb.barrier()
        self.off = 0

    def alloc(self, shape, dtype, name=""):
        nb = 4 if dtype == F32 else 2
        n = int(np.prod(shape[1:])) * nb
        n_al = (n + 63) // 64 * 64
        assert self.off + n_al <= self.n, "arena overflow %s %d+%d>%d" % (name, self.off, n_al, self.n)
        ap = self.t.ap[:, self.off:self.off + n].bitcast(dtype)
        self.off += n_al
        if len(shape) == 3:
            ap = ap.rearrange("p (a b) -> p a b", b=shape[2])
        elif len(shape) == 4:
            ap = ap.rearrange("p (a b c) -> p a b c", b=shape[2], c=shape[3])
        if shape[0] < 128:
            ap = ap[0:shape[0]]
        return T(ap, name)


class Prog:
    def __init__(self, S, NSEQ, NL, dbg=None):
        self.S, self.NSEQ, self.NL = S, NSEQ, NL
        self.N = S * NSEQ
        self.NT = S // 128
        self.NB = S // 512
        self.n_sel = min(256, S // 4)
        self.dbg = dbg
        nc = self.nc = bass.Bass("TRN2", target_bir_lowering=False)
        kb = self.kb = KB(nc)
        N = self.N
        self.inp = {}

        def ext(name, shape, dt=F32):
            self.inp[name] = kb.dram(name, shape, dt, kind="ExternalInput")
            return self.inp[name]

        ext("x", [N, D])
        ext("cst", [128, 1024])
        ext("ev_w_in", [2, D, EVEN_IN]); ext("ev_a_kv_norm_g", [2, 128]); ext("ev_a_w_uk", [2, 128, 64]); ext("ev_a_w_uv", [2, 128, 64])
        ext("ev_a_kidx_ln_g", [2, 64]); ext("ev_a_kidx_ln_b", [2, 64]); ext("ev_b_w_g2", [2, 16, 256]); ext("ev_b_b_g", [2, 256])
        ext("ev_b_norm_g", [2, 512]); ext("ev_w_out", [2, D, D]); ext("ev_ln1_g", [2, D]); ext("ev_ln1_b", [2, D])
        ext("ev_ffn_w_gate", [2, D, DFF]); ext("ev_ffn_w_up", [2, D, DFF]); ext("ev_ffn_w_down", [2, DFF, D])
        ext("ev_ln2_g", [2, D]); ext("ev_ln2_b", [2, D])
        ext("od_w_qkv", [2, D, 3072]); ext("od_lam", [2, 4, 64]); ext("od_subln_g", [2, 128]); ext("od_w_out", [2, D, D])
        ext("od_ln1_g", [2, D]); ext("od_ln1_b", [2, D]); ext("od_router_w", [2, D, 8]); ext("od_router_b", [2, 8])
        if NL > 1:
            ext("od_moe_w_gate", [2, NEXP, D, DFF]); ext("od_moe_w_up", [2, NEXP, D, DFF]); ext("od_moe_w_down", [2, NEXP, DFF, D])
        ext("od_ln2_g", [2, D]); ext("od_ln2_b", [2, D])
        self.out = kb.dram("out", [N, D], F32, kind="ExternalOutput")
        self.XA = kb.dram("XA", [N, D], F32)
        self.XB = kb.dram("XB", [N, D], F32)
        self.XT = [kb.dram("XT0", [D, N], BF16), kb.dram("XT1", [D, N], BF16)]
        self.MIXD = kb.dram("MIXD", [N, D], BF16)
        NG = self.NG = (2 * N) // 512 + NEXP
        def table(name):
            full = [kb.dram("%s%d" % (name, j), [(NEXP + 1) * 1408, 2048], BF16) for j in range(2)]
            parts = [[T(full[j].ap[sl * 1408:(sl + 1) * 1408, :], "%s%d_%d" % (name, j, sl)) for sl in range(NEXP + 1)] for j in range(2)]
            return full, parts
        self.TgF, self.Tg = table("Tg")
        self.TuF, self.Tu = table("Tu")
        self.TdF, self.Td = table("Td")
        self.XS = kb.dram("XS", [NG * 512, D], BF16)
        self.YS = kb.dram("YS", [NG * 512, D], F32)
        self.PB = [kb.ps([128, 512], F32, name="bank%d" % i) for i in range(8)]
        self.cstf = kb.sb([128, 1024], F32, name="cstf")
        self.identb = kb.sb([128, 128], BF16, name="identb")
        self.trib = kb.sb([128, 128], BF16, name="trib")
        self.trigb = kb.sb([128, 64], BF16, name="trigb")
        self.zb = kb.sb([128, 512], BF16, name="zb")
        NTT = self.NTT = NSEQ * self.NT
        self.comb = kb.sb([128, NTT, 8], F32, name="comb")
        self.sel12 = kb.sb([128, NTT, 16], F32, name="sel12")
        self.gates = kb.sb([128, NTT, 2], F32, name="gates")
        self.pidx = kb.sb([128, NTT, 2], mybir.dt.int32, name="pidx")
        self.idxq = kb.sb([128, NG, 11], mybir.dt.int32, name="idxq")
        self.onesb = kb.sb([128, 128], BF16, name="onesb")
        self.onesf = kb.sb([128, 8], F32, name="onesf")
        self.ustb = kb.sb([128, 128], BF16, name="ustb")
        self.ar = Arena(kb, 192 * 1024)
        self.identf = T(self.cstf[:, 0:128], "identf", self.cstf.res)
        self.cbias = T(self.cstf[:, 128:256], "cbias", self.cstf.res)
        self.rst = T(self.cstf[:, 384:896], "rst", self.cstf.res)
        kb.dma("sp", lambda e: e.dma_start(out=self.cstf[:, :], in_=self.inp["cst"][:, :]), reads=[self.inp["cst"]], writes=[self.cstf])
        kb.op("dve", lambda e: e.tensor_copy(out=self.identb[:, :], in_=self.cstf[:, 0:128]), reads=[self.cstf], writes=[self.identb])
        kb.op("dve", lambda e: e.tensor_copy(out=self.trib[:, :], in_=self.cstf[:, 256:384]), reads=[self.cstf], writes=[self.trib])
        kb.op("dve", lambda e: e.tensor_copy(out=self.trigb[:, :], in_=self.cstf[:, 896:960]), reads=[self.cstf], writes=[self.trigb])
        kb.op("pool", lambda e: e.memset(self.zb[:, :], 0.0), writes=[self.zb])
        kb.op("pool", lambda e: e.memset(self.onesb[:, :], 1.0), writes=[self.onesb])
        kb.op("pool", lambda e: e.memset(self.onesf[:, :], 1.0), writes=[self.onesf])
        kb.op("dve", lambda e: e.tensor_tensor(out=self.ustb[:, :], in0=self.cstf[:, 256:384], in1=self.cstf[:, 0:128], op=ALU.subtract),
              reads=[self.cstf], writes=[self.ustb])
        self.conv_q = []

    def mm(self, out, lhsT, rhs, start, stop, reads, writes, skip=False):
        self.kb.op("pe", lambda e: e.matmul(out, lhsT=lhsT, rhs=rhs, start=start, stop=stop, skip_group_check=skip), reads=reads, writes=writes)

    def tp(self, out, in_, ident, reads, writes):
        self.kb.op("pe", lambda e: e.transpose(out, in_, ident), reads=reads, writes=writes)

    def act(self, out, in_, func, reads, writes, **kw):
        self.kb.op("act", lambda e: e.activation(out=out, in_=in_, func=func, **kw), reads=reads, writes=writes)

    def tt(self, eng, out, in0, in1, op, reads, writes):
        self.kb.op(eng, lambda e: e.tensor_tensor(out=out, in0=in0, in1=in1, op=op), reads=reads, writes=writes)

    def ts(self, eng, out, in0, s1, s2, op0, op1, reads, writes, accum=None):
        if accum is None:
            self.kb.op(eng, lambda e: e.tensor_scalar(out=out, in0=in0, scalar1=s1, scalar2=s2, op0=op0, op1=op1), reads=reads, writes=writes)
        else:
            self.kb.op(eng, lambda e: e.tensor_scalar(out=out, in0=in0, scalar1=s1, scalar2=s2, op0=op0, op1=op1, accum_out=accum), reads=reads, writes=writes)

    def stt(self, out, in0, scalar, in1, op0, op1, reads, writes):
        self.kb.op("dve", lambda e: e.scalar_tensor_tensor(out=out, in0=in0, scalar=scalar, in1=in1, op0=op0, op1=op1), reads=reads, writes=writes)

    def cp(self, eng, out, in_, reads, writes):
        if eng == "act":
            self.kb.op("act", lambda e: e.copy(out=out, in_=in_), reads=reads, writes=writes)
        else:
            self.kb.op(eng, lambda e: e.tensor_copy(out=out, in_=in_), reads=reads, writes=writes)

    def ld(self, q, out, in_, reads, writes):
        return self.kb.dma(q, lambda e: e.dma_start(out=out, in_=in_), reads=reads, writes=writes)

    def bcast_load(self, dst, src_row, reads):
        self.ld("sp", dst[:, :], src_row.partition_broadcast(128), reads, [dst])

    def load_w_cast(self, dst, src2d, ncols, reads):
        step = 1024
        for c0 in range(0, ncols, step):
            c1 = min(ncols, c0 + step)
            self.ld("pool", dst[:, :, c0:c1], src2d[:, c0:c1].rearrange("(c p) n -> p c n", p=128), reads, [dst])

    def queue_conversions(self):
        inp = self.inp
        for j in range(2):
            srcs = []
            if 2 * j < self.NL:
                srcs.append((NEXP, inp["ev_ffn_w_gate"][j], inp["ev_ffn_w_up"][j], inp["ev_ffn_w_down"][j]))
            if 2 * j + 1 < self.NL:
                for e in range(NEXP):
                    srcs.append((e, inp["od_moe_w_gate"][j, e], inp["od_moe_w_up"][j, e], inp["od_moe_w_down"][j, e]))
            for (slot, g, u, d) in srcs:
                for (tab, src) in ((self.Tg, g), (self.Tu, u)):
                    for c0 in (0, 4):
                        self.conv_q.append((j, slot, tab[j][slot],
                                            tab[j][slot][:, :].rearrange("(p q) (c f) -> p q c f", q=11, f=256)[:, :, c0:c0 + 4, :],
                                            src.rearrange("(c p) (q f) -> p q c f", p=128, f=256)[:, :, c0:c0 + 4, :]))
                self.conv_q.append((j, slot, self.Td[j][slot],
                                    self.Td[j][slot][:, :].rearrange("(p q) (a n) -> p q a n", q=11, n=1024),
                                    d.rearrange("(q a p) n -> p q a n", a=2, p=128)))

    def pump(self, n=1, upto=None):
        while self.conv_q and (n > 0 or (upto is not None and (self.conv_q[0][0], self.conv_q[0][1] != NEXP) <= upto)):
            j, slot, t, dst, src = self.conv_q.pop(0)
            self.kb.dma("pool", lambda e, dst=dst, src=src: e.dma_start(out=dst, in_=src), reads=[], writes=[t], ring="bulk")
            n -= 1

    def ln_tile(self, hsrc, hres, Xold, Xnew, XTnew, n0, gbc, bbc, bufs, router=None):
        kb = self.kb
        par = (n0 // 128) % 2
        bufs = dict(bufs)
        for nm in ("xo", "z", "st", "xtb", "xtl"):
            if nm in bufs and isinstance(bufs[nm], list):
                bufs[nm] = bufs[nm][par]
        xo, z, st, xtb = bufs["xo"], bufs["z"], bufs["st"], bufs["xtb"]
        self.ld("sp", xo[:, :], Xold[n0:n0 + 128, :], [Xold], [xo])
        for h in range(2):
            self.stt(z[:, h * 512:(h + 1) * 512], xo[:, h * 512:(h + 1) * 512], ALPHA, hsrc[h], ALU.mult, ALU.add, [xo] + hres, [z])
        for h in range(2):
            kb.op("dve", lambda e, h=h: e.bn_stats(out=st[:, h * 6:(h + 1) * 6], in_=z[:, h * 512:(h + 1) * 512]), reads=[z], writes=[st])
        kb.op("dve", lambda e: e.bn_aggr(out=st[:, 12:14], in_=st[:, 0:12]), reads=[st], writes=[st])
        self.act(st[:, 14:15], st[:, 13:14], AF.Sqrt, [st], [st], bias=bufs["eps"][:, 0:1], scale=1.0)
        kb.op("dve", lambda e: e.reciprocal(out=st[:, 15:16], in_=st[:, 14:15]), reads=[st], writes=[st])
        self.ts("dve", st[:, 16:17], st[:, 12:13], -1.0, st[:, 15:16], ALU.mult, ALU.mult, [st], [st])
        self.act(xo[:, :], z[:, :], AF.Identity, [z, st], [xo], scale=st[:, 15:16], bias=st[:, 16:17])
        self.tt("dve", xo[:, :], xo[:, :], gbc[:, :], ALU.mult, [xo, gbc], [xo])
        self.tt("pool", xo[:, :], xo[:, :], bbc[:, :], ALU.add, [xo, bbc], [xo])
        self.ld("sp", Xnew[n0:n0 + 128, :], xo[:, :], [xo], [Xnew])
        if XTnew is not None:
            pb = [self.PB[6], self.PB[7]]
            for c in range(DC):
                self.tp(pb[c // 4][:, (c % 4) * 128:(c % 4 + 1) * 128], xo[:, c * 128:(c + 1) * 128], self.identf[:, :], [xo, self.identf], [pb[c // 4]])
            for h in range(2):
                self.cp("act", xtb[:, h * 4:(h + 1) * 4, :], pb[h][:, :].rearrange("p (c t) -> p c t", t=128), [pb[h]], [xtb])
            self.ld("sp", XTnew[:, n0:n0 + 128].rearrange("(c p) t -> p c t", p=128), xtb[:, :, :], [xtb], [XTnew])
            if router is not None:
                self.router_tile(pb, router, n0, bufs)

    def router_tile(self, pb, router, n0, bufs):
        kb = self.kb
        wrh, wrl, brb = router
        xtl, st, xtb = bufs["xtl"], bufs["st"], bufs["xtb"]
        for h in range(2):
            self.tt("dve", xtl[:, h * 4:(h + 1) * 4, :], pb[h][:, :].rearrange("p (c t) -> p c t", t=128), xtb[:, h * 4:(h + 1) * 4, :],
                    ALU.subtract, [pb[h], xtb], [xtl])
        lg = self.PB[5]
        k = 0
        for c in range(DC):
            for (xa, wa) in ((xtb, wrh), (xtb, wrl), (xtl, wrh)):
                self.mm(lg[:, 0:8], xa[:, c, :], wa[:, c, :], k == 0, k == 3 * DC - 1, [xa, wa], [lg])
                k += 1
        tix = n0 // 128
        L = st[:, 24:32]
        self.tt("dve", L, lg[:, 0:8], brb[:, :], ALU.add, [lg, brb], [st])
        kb.op("dve", lambda e: e.max(out=st[:, 32:40], in_=L), reads=[st], writes=[st])
        self.tt("dve", st[:, 40:41], st[:, 33:34], st[:, 32:33], ALU.subtract, [st], [st])
        self.act(st[:, 41:42], st[:, 40:41], AF.Exp, [st], [st])
        self.ts("dve", st[:, 42:43], st[:, 41:42], 1.0, None, ALU.add, ALU.bypass, [st], [st])
        kb.op("dve", lambda e: e.reciprocal(out=st[:, 43:44], in_=st[:, 42:43]), reads=[st], writes=[st])
        self.tt("dve", st[:, 44:45], st[:, 41:42], st[:, 43:44], ALU.mult, [st], [st])
        self.ts("dve", self.sel12[:, tix, 0:8], L, st[:, 32:33], None, ALU.is_equal, ALU.bypass, [st], [self.sel12])
        self.ts("dve", self.sel12[:, tix, 8:16], L, st[:, 33:34], None, ALU.is_equal, ALU.bypass, [st], [self.sel12])
        self.cp("dve", self.gates[:, tix, :], st[:, 43:45], [st], [self.gates])

    def ln_bufs(self):
        ar = self.ar
        b = {"xo": [ar.alloc([128, D], F32, "xo%d" % i) for i in range(2)], "z": [ar.alloc([128, D], F32, "z%d" % i) for i in range(2)],
             "st": [ar.alloc([128, 64], F32, "st%d" % i) for i in range(2)],
             "xtb": [ar.alloc([128, DC, 128], BF16, "xtb%d" % i) for i in range(2)], "eps": ar.alloc([128, 1], F32, "eps"),
             "gbc": ar.alloc([128, D], F32, "gbc"), "bbc": ar.alloc([128, D], F32, "bbc")}
        self.kb.op("pool", lambda e: e.memset(b["eps"][:, :], LN_EPS), writes=[b["eps"]])
        return b

    def ffn_setup(self):
        ar = self.ar
        f = {"xT": [ar.alloc([128, DC, 512], BF16, "xT%d" % i) for i in range(2)],
             "hT": ar.alloc([128, FC, 512], BF16, "hT"),
             "ring": [ar.alloc([128, 2048], BF16, "ring%d" % i) for i in range(12)],
             "sg": [ar.alloc([128, 512], BF16, "sg%d" % i) for i in range(2)],
             "ri": 0}
        return f

    def ffn_block(self, f, xT, load):
        self.ffn_p1(f, xT, load)
        self.ffn_p2(f, load)

    def ffn_p1(self, f, xT, load):
        PB = self.PB
        hT = f["hT"]
        k = 0
        for q in range(11):
            gs = f["ring"][f["ri"] % 12]; f["ri"] += 1
            us = f["ring"][f["ri"] % 12]; f["ri"] += 1
            load("g", q, gs)
            load("u", q, us)
            gv = gs[:, :].rearrange("p (c n) -> p c n", n=256)
            uv = us[:, :].rearrange("p (c n) -> p c n", n=256)
            for fl in range(2):
                fi = 2 * q + fl
                pg, pu = PB[(k % 2) * 2], PB[(k % 2) * 2 + 1]
                for c in range(DC):
                    self.mm(pg[:, :], gv[:, c, fl * 128:(fl + 1) * 128], xT[:, c, :], c == 0, c == DC - 1, [gs, xT], [pg])
                for c in range(DC):
                    self.mm(pu[:, :], uv[:, c, fl * 128:(fl + 1) * 128], xT[:, c, :], c == 0, c == DC - 1, [us, xT], [pu])
                sg = f["sg"][k % 2]
                self.act(sg[:, :], pg[:, :], AF.Silu, [pg], [sg])
                self.tt("dve", hT[:, fi, :], sg[:, :], pu[:, :], ALU.mult, [sg, pu], [hT])
                k += 1

    def ffn_p2(self, f, load):
        PB = self.PB
        hT = f["hT"]
        for q in range(11):
            ds = f["ring"][f["ri"] % 12]; f["ri"] += 1
            load("d", q, ds)
            dv = ds[:, :].rearrange("p (a n) -> p a n", n=D)
            for a_ in range(2):
                fi = 2 * q + a_
                for u in range(4):
                    for h in range(2):
                        self.mm(PB[2 * u + h][:, :], hT[:, fi, u * 128:(u + 1) * 128], dv[:, a_, h * 512:(h + 1) * 512],
                                fi == 0, fi == FC - 1, [hT, ds], [PB[2 * u + h]])

    def static_loader(self, j, slot):
        tabs = {"g": self.Tg[j][slot], "u": self.Tu[j][slot], "d": self.Td[j][slot]}

        def load(kind, q, dst):
            t = tabs[kind]
            self.ld("sp", dst[:, :], t[:, :].rearrange("(p q) n -> p q n", q=11)[:, q, :], [t], [dst])
        return load

    def ffn_phase(self, j, Xold, Xnew, XTold, XTnew, lng, lnb):
        kb, ar = self.kb, self.ar
        self.pump(0, upto=(j, 0))
        ar.reset()
        f = self.ffn_setup()
        lb = self.ln_bufs()
        self.bcast_load(lb["gbc"], lng, [])
        self.bcast_load(lb["bbc"], lnb, [])
        accs = [ar.alloc([128, 4, D], F32, "acc%d" % i) for i in range(2)]
        load = self.static_loader(j, NEXP)
        nblk = self.N // 512

        def lns(blk):
            acc = accs[blk % 2]
            for u in range(4):
                hs = [acc[:, u, 0:512], acc[:, u, 512:1024]]
                self.ln_tile(hs, [acc], Xold, Xnew, XTnew, blk * 512 + u * 128, lb["gbc"], lb["bbc"], lb)
        for blk in range(nblk):
            n0 = blk * 512
            xT = f["xT"][blk % 2]
            acc = accs[blk % 2]
            self.ld("sp", xT[:, :, :], XTold[:, n0:n0 + 512].rearrange("(c p) t -> p c t", p=128), [XTold], [xT])
            self.ffn_p1(f, xT, load)
            if blk > 0:
                lns(blk - 1)
            self.ffn_p2(f, load)
            self.pump(3)
            for u in range(4):
                for h in range(2):
                    self.cp("act" if h == 0 else "dve", acc[:, u, h * 512:(h + 1) * 512], self.PB[2 * u + h][:, :], [self.PB[2 * u + h]], [acc])
        lns(nblk - 1)

    def route(self, Xcur):
        kb, ar, PB = self.kb, self.ar, self.PB
        NTT, NG = self.NTT, self.NG
        I32 = mybir.dt.int32
        ar.reset()
        SEL = ar.alloc([128, NTT, 8], BF16, "SEL")
        RANK = ar.alloc([128, NTT, 8], F32, "RANK")
        carry = ar.alloc([128, 8], F32, "carry")
        w = ar.alloc([128, 256], F32, "rw")
        big = ar.alloc([128, NG, 11], F32, "rbig")
        t3 = ar.alloc([128, NTT, 8], F32, "rt3")
        pf = ar.alloc([128, NTT, 2], F32, "rpf")
        self.tt("dve", SEL[:, :, :], self.sel12[:, :, 0:8], self.sel12[:, :, 8:16], ALU.add, [self.sel12], [SEL])
        kb.op("pool", lambda e: e.memset(carry[:, :], 0.0), writes=[carry])
        for i in range(NTT):
            p = PB[i % 2]
            self.mm(p[:, 0:8], self.ustb[:, :], SEL[:, i, :], True, True, [self.ustb, SEL], [p])
            self.mm(p[:, 8:16], self.onesb[:, :], SEL[:, i, :], True, True, [self.onesb, SEL], [p])
            self.tt("dve", RANK[:, i, :], p[:, 0:8], carry[:, :], ALU.add, [p, carry], [RANK])
            self.tt("dve", carry[:, :], p[:, 8:16], carry[:, :], ALU.add, [p, carry], [carry])
        cst = self.cstf
        THR, GT, QC, P11 = cst[:, 960:968], cst[:, 968:968 + NG], cst[:, 992:1003], cst[:, 1003:1004]
        ge, pn, incl, off = w[:, 0:8], w[:, 8:16], w[:, 16:24], w[:, 24:32]
        cmp_ = w[:, 64:128].rearrange("p (e k) -> p e k", k=8)
        self.tt("dve", cmp_, carry[:, :].unsqueeze(2).to_broadcast([128, 8, 8]), THR.unsqueeze(1).to_broadcast([128, 8, 8]), ALU.is_gt, [carry, cst], [w])
        kb.op("dve", lambda e: e.tensor_reduce(out=ge, in_=cmp_, axis=AX.X, op=ALU.add), reads=[w], writes=[w])
        self.ts("dve", pn, ge, 512.0, None, ALU.mult, ALU.bypass, [w], [w])
        kb.op("dve", lambda e: e.tensor_tensor_scan(out=incl, data0=self.onesf[:, 0:8], data1=pn, initial=0.0, op0=ALU.mult, op1=ALU.add), reads=[w, self.onesf], writes=[w])
        self.tt("dve", off, incl, pn, ALU.subtract, [w], [w])
        self.tt("dve", t3[:, :, :], RANK[:, :, :], off.unsqueeze(1).to_broadcast([128, NTT, 8]), ALU.add, [RANK, w], [t3])
        for k in range(2):
            self.tt("dve", RANK[:, :, :], t3[:, :, :], self.sel12[:, :, k * 8:(k + 1) * 8], ALU.mult, [t3, self.sel12], [RANK])
            kb.op("dve", lambda e, k=k: e.tensor_reduce(out=pf[:, :, k], in_=RANK[:, :, :], axis=AX.X, op=ALU.add), reads=[RANK], writes=[pf])
        self.cp("dve", self.pidx[:, :, :], pf[:, :, :], [pf], [self.pidx])
        cmp2 = big[:, :, 0:8]
        self.tt("dve", cmp2, GT.unsqueeze(2).to_broadcast([128, NG, 8]), incl.unsqueeze(1).to_broadcast([128, NG, 8]), ALU.is_ge, [cst, w], [big])
        eg = w[:, 32:32 + NG]
        kb.op("dve", lambda e: e.tensor_reduce(out=eg, in_=cmp2, axis=AX.X, op=ALU.add), reads=[big], writes=[w])
        self.ts("dve", eg, eg, 7.0, 1408.0, ALU.min, ALU.mult, [w], [w])
        self.ts("dve", eg, eg, P11, None, ALU.add, ALU.bypass, [w, cst], [w])
        self.tt("dve", big[:, :, :], eg.unsqueeze(2).to_broadcast([128, NG, 11]), QC.unsqueeze(1).to_broadcast([128, NG, 11]), ALU.add, [w, cst], [big])
        self.cp("dve", self.idxq[:, :, :], big[:, :, :], [big], [self.idxq])
        xos = [ar.alloc([128, D], F32, "sxo%d" % i) for i in range(2)]
        xbs = [ar.alloc([128, D], BF16, "sxb%d" % i) for i in range(2)]
        for i in range(NTT):
            xo, xb = xos[i % 2], xbs[i % 2]
            self.ld("sp", xo[:, :], Xcur[i * 128:(i + 1) * 128, :], [Xcur], [xo])
            self.cp("act", xb[:, :], xo[:, :], [xo], [xb])
            for k in range(2):
                self.kb.dma("pool", lambda e, xb=xb, i=i, k=k: e.indirect_dma_start(
                    out=self.XS[:, :], out_offset=bass.IndirectOffsetOnAxis(self.pidx[:, i, k:k + 1], 0), in_=xb[:, :], in_offset=None),
                    reads=[xb, self.pidx], writes=[self.XS])

    def moe_sparse(self, j, Xold, Xnew, XTnew, lng, lnb):
        kb, ar, PB = self.kb, self.ar, self.PB
        NG = self.NG
        self.pump(0, upto=(j, 1))
        ar.reset()
        f = self.ffn_setup()
        xss = [ar.alloc([128, 4, D], BF16, "xs%d" % i) for i in range(2)]
        ys = ar.alloc([128, 4, D], F32, "ys")
        tabs = {"g": self.Tg[j], "u": self.Tu[j], "d": self.Td[j]}
        fulls = {"g": self.TgF[j], "u": self.TuF[j], "d": self.TdF[j]}
        tres = [t for k in tabs for t in tabs[k][0:NEXP]]
        for g in range(NG):
            xs = xss[g % 2]
            xT = f["xT"][g % 2]
            self.ld("sp", xs[:, :, :], self.XS[g * 512:(g + 1) * 512, :].rearrange("(u p) d -> p u d", p=128), [self.XS], [xs])
            for u in range(4):
                pt = PB[4 + u]
                ptb = pt[:, :].bitcast(BF16)
                for c in range(DC):
                    self.tp(ptb[:, c * 128:(c + 1) * 128], xs[:, u, c * 128:(c + 1) * 128], self.identb[:, :], [xs, self.identb], [pt])
                self.cp("act" if u % 2 == 0 else "dve", xT[:, :, u * 128:(u + 1) * 128], ptb.rearrange("p (c t) -> p c t", t=128), [pt], [xT])

            def load(kind, q, dst, g=g):
                src = fulls[kind].ap[:, :]
                self.kb.dma("pool", lambda e, src=src, dst=dst, q=q: e.indirect_dma_start(
                    out=dst[:, :], out_offset=None, in_=src, in_offset=bass.IndirectOffsetOnAxis(self.idxq[:, g, q:q + 1], 0)),
                    reads=tres + [self.idxq], writes=[dst])
            self.ffn_block(f, xT, load)
            for u in range(4):
                for h in range(2):
                    self.cp("act" if h == 0 else "dve", ys[:, u, h * 512:(h + 1) * 512], PB[2 * u + h][:, :], [PB[2 * u + h]], [ys])
            self.ld("sp", self.YS[g * 512:(g + 1) * 512, :].rearrange("(u p) d -> p u d", p=128), ys[:, :, :], [ys], [self.YS])
        ar.reset()
        lb = self.ln_bufs()
        self.bcast_load(lb["gbc"], lng, [])
        self.bcast_load(lb["bbc"], lnb, [])
        y1s = [ar.alloc([128, D], F32, "y1_%d" % i) for i in range(2)]
        y2s = [ar.alloc([128, D], F32, "y2_%d" % i) for i in range(2)]
        for i in range(self.NTT):
            y1, y2 = y1s[i % 2], y2s[i % 2]
            for k, y in ((0, y1), (1, y2)):
                self.kb.dma("pool", lambda e, y=y, i=i, k=k: e.indirect_dma_start(
                    out=y[:, :], out_offset=None, in_=self.YS[:, :], in_offset=bass.IndirectOffsetOnAxis(self.pidx[:, i, k:k + 1], 0)),
                    reads=[self.YS, self.pidx], writes=[y])
            self.ts("dve", y1[:, :], y1[:, :], self.gates[:, i, 0:1], None, ALU.mult, ALU.bypass, [y1, self.gates], [y1])
            self.stt(y1[:, :], y2[:, :], self.gates[:, i, 1:2], y1[:, :], ALU.mult, ALU.add, [y2, self.gates, y1], [y1])
            self.ln_tile([y1[:, 0:512], y1[:, 512:1024]], [y1], Xold, Xnew, XTnew, i * 128, lb["gbc"], lb["bbc"], lb)

    def outproj_phase(self, w_out, Xold, Xnew, XTnew, lng, lnb, router=None):
        kb, ar = self.kb, self.ar
        ar.reset()
        lb = self.ln_bufs()
        self.bcast_load(lb["gbc"], lng, [])
        self.bcast_load(lb["bbc"], lnb, [])
        W = ar.alloc([128, DC, D], BF16, "wout")
        self.load_w_cast(W, w_out, D, [])
        rt = None
        if router is not None:
            wr = ar.alloc([128, DC, 8], F32, "wr")
            wrh = ar.alloc([128, DC, 8], BF16, "wrh")
            wrl = ar.alloc([128, DC, 8], BF16, "wrl")
            brb = ar.alloc([128, 8], F32, "brb")
            lb["xtl"] = [ar.alloc([128, DC, 128], BF16, "xtl%d" % i) for i in range(2)]
            self.ld("sp", wr[:, :, :], router[0].rearrange("(c p) n -> p c n", p=128), [], [wr])
            self.cp("dve", wrh[:, :, :], wr[:, :, :], [wr], [wrh])
            self.tt("dve", wrl[:, :, :], wr[:, :, :], wrh[:, :, :], ALU.subtract, [wr, wrh], [wrl])
            self.bcast_load(brb, router[1], [])
            rt = (wrh, wrl, brb)
        mixs = [ar.alloc([128, D], BF16, "mix%d" % i) for i in range(2)]
        mixT = [ar.alloc([128, DC, 128], BF16, "mixT%d" % i) for i in range(2)]
        PB = self.PB
        ntile = self.N // 128

        def front(t):
            n0 = t * 128
            mx, mt = mixs[t % 2], mixT[t % 2]
            self.ld("sp", mx[:, :], self.MIXD[n0:n0 + 128, :], [self.MIXD], [mx])
            pt = PB[4 + (t % 2)]
            ptb = pt[:, :].bitcast(BF16)
            for c in range(DC):
                self.tp(ptb[:, c * 128:(c + 1) * 128], mx[:, c * 128:(c + 1) * 128], self.identb[:, :], [mx, self.identb], [pt])
            self.cp("act", mt[:, :, :], ptb.rearrange("p (c t) -> p c t", t=128), [pt], [mt])
            hb = [PB[(t % 2) * 2], PB[(t % 2) * 2 + 1]]
            for h in range(2):
                for c in range(DC):
                    self.mm(hb[h][:, :], mt[:, c, :], W[:, c, h * 512:(h + 1) * 512], c == 0, c == DC - 1, [mt, W], [hb[h]])
        front(0)
        for t in range(ntile):
            if t + 1 < ntile:
                front(t + 1)
            hb = [PB[(t % 2) * 2], PB[(t % 2) * 2 + 1]]
            self.ln_tile([hb[0][:, :], hb[1][:, :]], hb, Xold, Xnew, XTnew, t * 128, lb["gbc"], lb["bbc"], lb, router=rt)

    def init_xt(self, Xin, XTnew):
        ar = self.ar
        ar.reset()
        xos = [ar.alloc([128, D], F32, "ixo%d" % i) for i in range(2)]
        xtbs = [ar.alloc([128, DC, 128], BF16, "ixt%d" % i) for i in range(2)]
        for t in range(self.N // 128):
            n0 = t * 128
            xo, xtb = xos[t % 2], xtbs[t % 2]
            self.ld("sp", xo[:, :], Xin[n0:n0 + 128, :], [Xin], [xo])
            pb = [self.PB[(t % 2) * 2], self.PB[(t % 2) * 2 + 1]]
            for c in range(DC):
                self.tp(pb[c // 4][:, (c % 4) * 128:(c % 4 + 1) * 128], xo[:, c * 128:(c + 1) * 128], self.identf[:, :], [xo, self.identf], [pb[c // 4]])
            for h in range(2):
                self.cp("act" if h == 0 else "dve", xtb[:, h * 4:(h + 1) * 4, :], pb[h][:, :].rearrange("p (c t) -> p c t", t=128), [pb[h]], [xtb])
            self.ld("sp", XTnew[:, n0:n0 + 128].rearrange("(c p) t -> p c t", p=128), xtb[:, :, :], [xtb], [XTnew])

    def even_scratch(self):
        kb, N = self.kb, self.N
        if hasattr(self, "PQ"):
            return
        self.PQ = kb.dram("PQ", [2048, N], BF16)
        self.PBQK = kb.dram("PBQK", [512, N], F32)
        self.CNTd = kb.dram("CNTd", [128, N], BF16)
        self.KITd = kb.dram("KITd", [128, N], BF16)
        self.WId = kb.dram("WId", [N, 8], F32)
        self.BGTd = kb.dram("BGTd", [16, N], BF16)
        self.BVd = kb.dram("BVd", [N, 1024], BF16)

    def even_proj(self, j, XTold):
        kb, ar, PB = self.kb, self.ar, self.PB
        inp = self.inp
        self.even_scratch()
        ar.reset()
        WIN = ar.alloc([128, DC, EVEN_IN], BF16, "WIN")
        self.load_w_cast(WIN, inp["ev_w_in"][j], EVEN_IN, [])
        kvg = ar.alloc([128, 128], F32, "kvg"); self.bcast_load(kvg, inp["ev_a_kv_norm_g"][j:j + 1, :], [])
        klg = ar.alloc([128, 64], F32, "klg"); self.bcast_load(klg, inp["ev_a_kidx_ln_g"][j:j + 1, :], [])
        klb = ar.alloc([128, 64], F32, "klb"); self.bcast_load(klb, inp["ev_a_kidx_ln_b"][j:j + 1, :], [])
        epsr = ar.alloc([128, 2], F32, "epsr")
        kb.op("pool", lambda e: e.memset(epsr[:, 0:1], RMS_EPS), writes=[epsr])
        kb.op("pool", lambda e: e.memset(epsr[:, 1:2], LN_EPS), writes=[epsr])
        xTs = [ar.alloc([128, DC, 512], BF16, "xT%d" % i) for i in range(2)]
        pqs = [ar.alloc([128, 8, 512], BF16, "pqs%d" % i) for i in range(2)]
        pbs = [ar.alloc([128, 4, 512], F32, "pbs%d" % i) for i in range(2)]
        bgs = [ar.alloc([16, 512], BF16, "bgs%d" % i) for i in range(2)]
        cns = [ar.alloc([128, 512], BF16, "cns%d" % i) for i in range(2)]
        kis = [ar.alloc([128, 512], BF16, "kis%d" % i) for i in range(2)]
        wis = [ar.alloc([128, 4, 8], F32, "wis%d" % i) for i in range(2)]
        bvs = [ar.alloc([128, 4, 1024], BF16, "bvs%d" % i) for i in range(2)]
        st = ar.alloc([128, 32], F32, "st")
        junk = ar.alloc([128, 128], F32, "junk")
        cn = ar.alloc([128, 128], BF16, "cn")
        kin = ar.alloc([128, 64], F32, "kin")
        kin2 = ar.alloc([128, 128], BF16, "kin2")
        WSC = (8.0 ** -0.5) * (64.0 ** -0.5)
        for blk in range(self.N // 512):
            n0 = blk * 512
            b2 = blk % 2
            xT = xTs[b2]
            self.pump(2)
            self.ld("sp", xT[:, :, :], XTold[:, n0:n0 + 512].rearrange("(c p) t -> p c t", p=128), [XTold], [xT])
            fm = [(g * 128, 128, pqs[b2], g, 1.0) for g in range(4)] + [(640 + g * 128, 128, pqs[b2], 4 + g, 1.0) for g in range(4)] + \
                 [(1224 + g * 128, 128, pbs[b2], g, 0.125) for g in range(2)] + [(1480 + g * 128, 128, pbs[b2], 2 + g, 1.0) for g in range(2)] + \
                 [(2248, 16, bgs[b2], None, 1.0)]
            for gi, (c0, M, dst, slot, sc) in enumerate(fm):
                pb = PB[gi % 2]
                for c in range(DC):
                    self.mm(pb[0:M, :], WIN[:, c, c0:c0 + M], xT[:, c, :], c == 0, c == DC - 1, [WIN, xT], [pb])
                o = dst[0:M, :] if slot is None else dst[:, slot, :]
                if gi % 2 == 0:
                    self.act(o, pb[0:M, :], AF.Copy, [pb], [dst], scale=sc)
                else:
                    self.ts("dve", o, pb[0:M, :], sc, None, ALU.mult, ALU.bypass, [pb], [dst])
            self.ld("sp", self.PQ[0:1024, n0:n0 + 512].rearrange("(g p) t -> p g t", p=128), pqs[b2][:, :, :], [pqs[b2]], [self.PQ])
            self.ld("sp", self.PBQK[:, n0:n0 + 512].rearrange("(g p) t -> p g t", p=128), pbs[b2][:, :, :], [pbs[b2]], [self.PBQK])
            self.ld("sp", self.BGTd[:, n0:n0 + 512], bgs[b2][:, :], [bgs[b2]], [self.BGTd])
            for u in range(4):
                xs = lambda c: xT[:, c, u * 128:(u + 1) * 128]
                p2 = PB[2]
                for c in range(DC):
                    self.mm(p2[:, 0:128], xs(c), WIN[:, c, 512:640], c == 0, c == DC - 1, [xT, WIN], [p2])
                for c in range(DC):
                    self.mm(p2[:, 128:200], xs(c), WIN[:, c, 1152:1224], c == 0, c == DC - 1, [xT, WIN], [p2])
                self.act(junk[:, :], p2[:, 0:128], AF.Square, [p2], [junk, st], accum_out=st[:, 0:1])
                self.act(st[:, 1:2], st[:, 0:1], AF.Sqrt, [st, epsr], [st], scale=1.0 / 128, bias=epsr[:, 0:1])
                kb.op("dve", lambda e: e.reciprocal(out=st[:, 2:3], in_=st[:, 1:2]), reads=[st], writes=[st])
                self.stt(cn[:, :], p2[:, 0:128], st[:, 2:3], kvg[:, :], ALU.mult, ALU.mult, [p2, st, kvg], [cn])
                p3 = PB[3]
                p3b = p3[:, :].bitcast(BF16)
                self.tp(p3b[:, 0:128], cn[:, :], self.identb[:, :], [cn, self.identb], [p3])
                kb.op("dve", lambda e: e.bn_stats(out=st[:, 4:10], in_=p2[:, 128:192]), reads=[p2], writes=[st])
                kb.op("dve", lambda e: e.bn_aggr(out=st[:, 10:12], in_=st[:, 4:10]), reads=[st], writes=[st])
                self.act(st[:, 12:13], st[:, 11:12], AF.Sqrt, [st, epsr], [st], scale=1.0, bias=epsr[:, 1:2])
                kb.op("dve", lambda e: e.reciprocal(out=st[:, 13:14], in_=st[:, 12:13]), reads=[st], writes=[st])
                self.ts("dve", st[:, 14:15], st[:, 10:11], -1.0, st[:, 13:14], ALU.mult, ALU.mult, [st], [st])
                self.act(kin[:, :], p2[:, 128:192], AF.Identity, [p2, st], [kin], scale=st[:, 13:14], bias=st[:, 14:15])
                self.tt("dve", kin[:, :], kin[:, :], klg[:, :], ALU.mult, [kin, klg], [kin])
                self.tt("dve", kin2[:, 0:64], kin[:, :], klb[:, :], ALU.add, [kin, klb], [kin2])
                self.tt("dve", kin2[:, 64:128], kin[:, :], klb[:, :], ALU.add, [kin, klb], [kin2])
                self.tp(p3b[:, 128:256], kin2[:, :], self.identb[:, :], [kin2, self.identb], [p3])
                self.cp("act", cns[b2][:, u * 128:(u + 1) * 128], p3b[:, 0:128], [p3], [cns[b2]])
                self.cp("act", kis[b2][:, u * 128:(u + 1) * 128], p3b[:, 128:256], [p3], [kis[b2]])
                self.ts("dve", wis[b2][:, u, :], p2[:, 192:200], WSC, None, ALU.mult, ALU.bypass, [p2], [wis[b2]])
                p4, p5 = PB[4], PB[5]
                for c in range(DC):
                    self.mm(p4[:, :], xs(c), WIN[:, c, 1736:2248], c == 0, c == DC - 1, [xT, WIN], [p4])
                for c in range(DC):
                    self.mm(p5[:, :], xs(c), WIN[:, c, 2264:2776], c == 0, c == DC - 1, [xT, WIN], [p5])
                self.cp("dve", bvs[b2][:, u, 0:512], p4[:, :], [p4], [bvs[b2]])
                self.act(bvs[b2][:, u, 512:1024], p5[:, :], AF.Silu, [p5], [bvs[b2]])
            self.ld("sp", self.CNTd[:, n0:n0 + 512], cns[b2][:, :], [cns[b2]], [self.CNTd])
            self.ld("sp", self.KITd[:, n0:n0 + 512], kis[b2][:, :], [kis[b2]], [self.KITd])
            self.ld("sp", self.WId[n0:n0 + 512, :].rearrange("(u p) e -> p u e", p=128), wis[b2][:, :, :], [wis[b2]], [self.WId])
            self.ld("sp", self.BVd[n0:n0 + 512, :].rearrange("(u p) f -> p u f", p=128), bvs[b2][:, :, :], [bvs[b2]], [self.BVd])

    def dsa(self, j, sq):
        kb, ar, PB = self.kb, self.ar, self.PB
        inp = self.inp
        S, NT = self.S, self.NT
        nb = sq * S
        ar.reset()
        QT = ar.alloc([128, 4, S], BF16, "QT")
        QIT = ar.alloc([128, 4, S], BF16, "QIT")
        CNT = ar.alloc([128, S], BF16, "CNT")
        KIT = ar.alloc([128, S], BF16, "KIT")
        KT2 = ar.alloc([128, S], BF16, "KT2")
        V1 = ar.alloc([128, NT, 65], BF16, "V1")
        WI = ar.alloc([128, NT, 8], F32, "WI")
        WUK2 = ar.alloc([128, 128], BF16, "WUK2")
        WUV = ar.alloc([128, 64], BF16, "WUV")
        self.ld("sp", QT[:, :, :], self.PQ[0:512, nb:nb + S].rearrange("(g p) t -> p g t", p=128), [self.PQ], [QT])
        self.ld("sp", QIT[:, :, :], self.PQ[512:1024, nb:nb + S].rearrange("(g p) t -> p g t", p=128), [self.PQ], [QIT])
        self.ld("sp", CNT[:, :], self.CNTd[:, nb:nb + S], [self.CNTd], [CNT])
        self.ld("sp", KIT[:, :], self.KITd[:, nb:nb + S], [self.KITd], [KIT])
        self.ld("sp", WI[:, :, :], self.WId[nb:nb + S, :].rearrange("(t p) e -> p t e", p=128), [self.WId], [WI])
        self.ld("pool", WUK2[:, 0:64], inp["ev_a_w_uk"][j], [], [WUK2])
        self.ld("pool", WUK2[:, 64:128], inp["ev_a_w_uk"][j], [], [WUK2])
        self.ld("pool", WUV[:, :], inp["ev_a_w_uv"][j], [], [WUV])
        kb.op("pool", lambda e: e.memset(V1[:, :, 64:65], 1.0), writes=[V1])
        for blk in range(S // 512):
            pb = PB[blk % 2]
            self.mm(pb[:, :], WUK2[:, :], CNT[:, blk * 512:(blk + 1) * 512], True, True, [WUK2, CNT], [pb])
            self.cp("act", KT2[:, blk * 512:(blk + 1) * 512], pb[:, :], [pb], [KT2])
        for t in range(NT):
            pb = PB[2 + t % 2]
            self.mm(pb[:, 0:64], CNT[:, t * 128:(t + 1) * 128], WUV[:, :], True, True, [CNT, WUV], [pb])
            self.cp("dve", V1[:, t, 0:64], pb[:, 0:64], [pb], [V1])
        SC = ar.alloc([128, S], F32, "SC")
        JK = ar.alloc([128, S], BF16, "JK")
        MK = ar.alloc([128, S], BF16, "MK")
        MKT = ar.alloc([128, NT, 128], BF16, "MKT")
        Rs = [ar.alloc([128, 512], F32, "R%d" % i) for i in range(2)]
        PTs = [ar.alloc([128, 512], BF16, "PT%d" % i) for i in range(4)]
        bs = ar.alloc([128, 16], F32, "bs")
        mixs = [ar.alloc([128, 512], BF16, "amix%d" % i) for i in range(2)]
        NIT = 12
        K = float(self.n_sel)
        rk = 0
        pk = 0
        for jt in range(NT):
            n = (jt + 1) * 128
            self.pump(1)
            for h in range(8):
                g, hf = h // 2, h % 2
                for k0 in range(0, n, 512):
                    w = min(512, n - k0)
                    pb = PB[rk % 2]
                    R = Rs[rk % 2]
                    rk += 1
                    self.mm(pb[:, 0:w], QIT[hf * 64:(hf + 1) * 64, g, jt * 128:(jt + 1) * 128], KIT[hf * 64:(hf + 1) * 64, k0:k0 + w], True, True, [QIT, KIT], [pb])
                    self.act(R[:, 0:w], pb[:, 0:w], AF.Relu, [pb], [R])
                    if h == 0:
                        self.ts("dve", SC[:, k0:k0 + w], R[:, 0:w], WI[:, jt, 0:1], None, ALU.mult, ALU.bypass, [R, WI], [SC])
                    else:
                        self.stt(SC[:, k0:k0 + w], R[:, 0:w], WI[:, jt, h:h + 1], SC[:, k0:k0 + w], ALU.mult, ALU.add, [R, WI, SC], [SC])
            thr = jt * 128 >= self.n_sel
            if thr:
                kb.op("dve", lambda e, n=n: e.tensor_reduce(out=bs[:, 0:1], in_=SC[:, 0:n], axis=AX.X, op=ALU.max), reads=[SC], writes=[bs])
                kb.op("dve", lambda e, n=n: e.tensor_reduce(out=bs[:, 1:2], in_=SC[:, 0:n], axis=AX.X, op=ALU.min), reads=[SC], writes=[bs])
                self.tt("dve", bs[:, 2:3], bs[:, 0:1], bs[:, 1:2], ALU.subtract, [bs], [bs])
            self.tt("dve", SC[:, jt * 128:n], SC[:, jt * 128:n], self.cbias[:, :], ALU.add, [SC, self.cbias], [SC])
            if thr:
                lo = bs[:, 1:2]
                for it in range(NIT):
                    f = 2.0 ** -(it + 1)
                    self.stt(bs[:, 3:4], bs[:, 2:3], f, lo, ALU.mult, ALU.add, [bs], [bs])
                    self.ts("dve", JK[:, 0:n], SC[:, 0:n], bs[:, 3:4], None, ALU.is_ge, ALU.add, [SC, bs], [JK, bs], accum=bs[:, 4:5])
                    self.ts("dve", bs[:, 5:6], bs[:, 4:5], K - 0.5, f, ALU.is_ge, ALU.mult, [bs], [bs])
                    self.stt(lo, bs[:, 5:6], bs[:, 2:3], lo, ALU.mult, ALU.add, [bs], [bs])
                self.ts("dve", MK[:, 0:n], SC[:, 0:n], lo, None, ALU.is_ge, ALU.bypass, [SC, bs], [MK])
            else:
                self.ts("dve", MK[:, 0:n], SC[:, 0:n], -1e29, None, ALU.is_ge, ALU.bypass, [SC], [MK])
            for i0 in range(0, jt + 1, 8):
                i1 = min(jt + 1, i0 + 8)
                p2 = PB[2]
                p2b = p2[:, :].bitcast(BF16)
                for i in range(i0, i1):
                    self.tp(p2b[:, (i - i0) * 128:(i - i0 + 1) * 128], MK[:, i * 128:(i + 1) * 128], self.identb[:, :], [MK, self.identb], [p2])
                self.cp("act", MKT[:, i0:i1, :], p2b[:, 0:(i1 - i0) * 128].rearrange("p (i t) -> p i t", t=128), [p2], [MKT])
            for b in range(2):
                self.mm(PB[6 + b][:, 0:260], self.zb[:, 0:128], self.zb[:, 0:260], True, False, [self.zb], [PB[6 + b]], skip=True)
            steps = [(i, hf) for i in range(jt + 1) for hf in range(2)]
            pts = {}

            def stA(k, jt=jt):
                nonlocal pk
                i, hf = steps[k]
                ps = PB[2 + pk % 4]
                PT = PTs[pk % 4]
                pk += 1
                pts[k] = PT
                self.mm(ps[:, :], KT2[hf * 64:(hf + 1) * 64, i * 128:(i + 1) * 128], QT[hf * 64:(hf + 1) * 64, :, jt * 128:(jt + 1) * 128], True, True, [KT2, QT], [ps])
                self.act(PT[:, :], ps[:, :], AF.Exp, [ps], [PT], scale=0.125)
                pv = PT[:, :].rearrange("p (g t) -> p g t", t=128)
                self.tt("dve", pv, pv, MKT[:, i, :].unsqueeze(1).to_broadcast([128, 4, 128]), ALU.mult, [PT, MKT], [PT])

            def stB(k, jt=jt):
                i, hf = steps[k]
                PT = pts.pop(k)
                for g in range(4):
                    h = 2 * g + hf
                    ob = PB[6 + h // 4]
                    self.mm(ob[:, (h % 4) * 65:(h % 4) * 65 + 65], PT[:, g * 128:(g + 1) * 128], V1[:, i, :], False, i == jt, [PT, V1], [ob], skip=True)
            LA_ = 2
            for k in range(min(LA_, len(steps))):
                stA(k)
            for k in range(len(steps)):
                if k + LA_ < len(steps):
                    stA(k + LA_)
                stB(k)
            mx = mixs[jt % 2]
            for b in range(2):
                ov = PB[6 + b][:, 0:260].rearrange("p (h d) -> p h d", d=65)
                kb.op("dve", lambda e, ov=ov, b=b: e.reciprocal(out=bs[:, 8 + b * 4:12 + b * 4].unsqueeze(2), in_=ov[:, :, 64:65]), reads=[PB[6 + b]], writes=[bs])
                self.tt("dve", mx[:, b * 256:(b + 1) * 256].rearrange("p (h d) -> p h d", d=64), ov[:, :, 0:64],
                        bs[:, 8 + b * 4:12 + b * 4].unsqueeze(2).to_broadcast([128, 4, 64]), ALU.mult, [PB[6 + b], bs], [mx])
            self.ld("sp", self.MIXD[nb + jt * 128:nb + (jt + 1) * 128, 0:512], mx[:, :], [mx], [self.MIXD])

    def gla(self, j, sq):
        kb, ar, PB = self.kb, self.ar, self.PB
        inp = self.inp
        S = self.S
        NCH = S // 64
        nb = sq * S
        ar.reset()
        BQs = [ar.alloc([64, S], F32, "BQ%d" % i) for i in range(2)]
        BKs = [ar.alloc([64, S], F32, "BK%d" % i) for i in range(2)]
        BG = ar.alloc([16, S], BF16, "BG")
        BV = ar.alloc([64, NCH, 1024], BF16, "BV")
        WG2 = ar.alloc([16, 256], BF16, "WG2")
        nbias = ar.alloc([64, 4], F32, "nbias")
        GNB = ar.alloc([64, 512], F32, "GNB")
        self.ld("sp", BG[:, :], self.BGTd[:, nb:nb + S], [self.BGTd], [BG])
        self.ld("sp", BV[:, :, :], self.BVd[nb:nb + S, :].rearrange("(c p) f -> p c f", p=64), [self.BVd], [BV])
        self.ld("pool", WG2[:, :], inp["ev_b_w_g2"][j], [], [WG2])
        for h in range(4):
            self.ld("sp", nbias[:, h:h + 1], inp["ev_b_b_g"][j, h * 64:(h + 1) * 64].rearrange("(p o) -> p o", o=1), [], [nbias])
        kb.op("dve", lambda e: e.tensor_scalar(out=nbias[:, :], in0=nbias[:, :], scalar1=-1.0, scalar2=None, op0=ALU.mult), reads=[nbias], writes=[nbias])
        self.ld("sp", GNB[:, :], inp["ev_b_norm_g"][j:j + 1, :].partition_broadcast(64), [], [GNB])
        LA = ar.alloc([64, 512], F32, "LA")
        BP = ar.alloc([64, 512], F32, "BP")
        EQ = ar.alloc([64, 512], F32, "EQ")
        EBL = ar.alloc([64, 4, NCH], F32, "EBL")
        QG = ar.alloc([64, 4, S], BF16, "QG")
        KG = ar.alloc([64, 4, S], BF16, "KG")
        KTM = ar.alloc([64, NCH, 256], BF16, "KTM")
        epsr = ar.alloc([64, 1], F32, "epsr")
        kb.op("pool", lambda e: e.memset(epsr[:, :], RMS_EPS), writes=[epsr])
        rst = self.rst[0:64, :]
        for h in range(4):
            BQ, BK = BQs[h % 2], BKs[h % 2]
            self.ld("sp", BQ[:, :], self.PBQK[h * 64:(h + 1) * 64, nb:nb + S], [self.PBQK], [BQ])
            self.ld("sp", BK[:, :], self.PBQK[256 + h * 64:256 + (h + 1) * 64, nb:nb + S], [self.PBQK], [BK])
            for blk in range(S // 512):
                sl = slice(blk * 512, (blk + 1) * 512)
                pb = PB[blk % 2]
                self.mm(pb[0:64, :], WG2[0:16, h * 64:(h + 1) * 64], BG[0:16, sl], True, True, [WG2, BG], [pb])
                self.act(LA[:, :], pb[0:64, :], AF.Exp, [pb, nbias], [LA], scale=-1.0, bias=nbias[:, h:h + 1])
                self.act(LA[:, :], LA[:, :], AF.Ln, [LA], [LA], bias=1.0)
                kb.op("dve", lambda e: e.tensor_tensor_scan(out=BP[:, :], data0=rst, data1=LA[:, :], initial=0.0, op0=ALU.mult, op1=ALU.add),
                      reads=[self.rst, LA], writes=[BP])
                self.act(EQ[:, :], BP[:, :], AF.Exp, [BP], [EQ], scale=-1.0 / 16)
                self.tt("dve", QG[:, h, sl], BQ[:, sl], EQ[:, :], ALU.mult, [BQ, EQ], [QG])
                self.cp("dve", EBL[:, h, blk * 8:(blk + 1) * 8], EQ[:, :].rearrange("p (c j) -> p c j", j=64)[:, :, 63], [EQ], [EBL])
                self.act(EQ[:, :], BP[:, :], AF.Exp, [BP], [EQ], scale=1.0 / 16)
                self.tt("dve", KG[:, h, sl], BK[:, sl], EQ[:, :], ALU.mult, [BK, EQ], [KG])
        for c in range(NCH):
            pb = PB[2 + c % 2]
            pbb = pb[:, :].bitcast(BF16)
            for h in range(4):
                self.tp(pbb[0:64, h * 64:(h + 1) * 64], KG[:, h, c * 64:(c + 1) * 64], self.identb[0:64, 0:64], [KG, self.identb], [pb])
            self.cp("act", KTM[:, c, :], pbb[0:64, 0:256], [pb], [KTM])
            self.tt("pool", BV[:, c, 512:1024], BV[:, c, 512:1024], GNB[:, :], ALU.mult, [BV, GNB], [BV])
        ST32 = ar.alloc([64, 4, 128], F32, "ST32")
        STB = ar.alloc([64, 4, 128], BF16, "STB")
        TMP = ar.alloc([64, 4, 128], F32, "TMP")
        SCMs = [ar.alloc([64, 256], BF16, "SCM%d" % i) for i in range(2)]
        ss = ar.alloc([64, 16], F32, "ss")
        junk = ar.alloc([64, 128], F32, "gjunk")
        mixs = [ar.alloc([64, 512], BF16, "bmix%d" % i) for i in range(2)]
        kb.op("pool", lambda e: e.memset(ST32[:, :, :], 0.0), writes=[ST32])
        kb.op("pool", lambda e: e.memset(STB[:, :, :], 0.0), writes=[STB])
        trig = self.trigb[0:64, :]
        for c in range(NCH):
            cs = slice(c * 64, (c + 1) * 64)
            psc = PB[c % 2]
            for h in range(4):
                self.mm(psc[0:64, h * 64:(h + 1) * 64], KG[:, h, cs], QG[:, h, cs], True, True, [KG, QG], [psc])
            SCM = SCMs[c % 2]
            self.tt("dve", SCM[:, :].rearrange("p (h i) -> p h i", i=64), psc[0:64, 0:256].rearrange("p (h i) -> p h i", i=64),
                    trig.unsqueeze(1).to_broadcast([64, 4, 64]), ALU.mult, [psc, self.trigb], [SCM])
            og = PB[2 + c % 2]
            for h in range(4):
                self.mm(og[0:64, h * 128:(h + 1) * 128], SCM[:, h * 64:(h + 1) * 64], BV[:, c, h * 128:(h + 1) * 128], True, c == 0, [SCM, BV], [og])
                if c > 0:
                    self.mm(og[0:64, h * 128:(h + 1) * 128], QG[:, h, cs], STB[:, h, :], False, True, [QG, STB], [og])
            if c < NCH - 1:
                pst = PB[4 + c % 2]
                for h in range(4):
                    self.mm(pst[0:64, h * 128:(h + 1) * 128], KTM[:, c, h * 64:(h + 1) * 64], BV[:, c, h * 128:(h + 1) * 128], True, True, [KTM, BV], [pst])
                for h in range(4):
                    self.act(TMP[:, h, :], pst[0:64, h * 128:(h + 1) * 128], AF.Identity, [pst, EBL], [TMP], scale=EBL[:, h, c:c + 1])
                    self.stt(ST32[:, h, :], ST32[:, h, :], EBL[:, h, c:c + 1], TMP[:, h, :], ALU.mult, ALU.add, [ST32, EBL, TMP], [ST32])
                self.cp("pool", STB[:, :, :], ST32[:, :, :], [ST32], [STB])
            mx = mixs[c % 2]
            for h in range(4):
                self.act(junk[:, :], og[0:64, h * 128:(h + 1) * 128], AF.Square, [og], [junk, ss], accum_out=ss[:, h:h + 1])
            self.act(ss[:, 4:8], ss[:, 0:4], AF.Sqrt, [ss, epsr], [ss], scale=1.0 / 128, bias=epsr[:, 0:1])
            kb.op("dve", lambda e: e.reciprocal(out=ss[:, 8:12], in_=ss[:, 4:8]), reads=[ss], writes=[ss])
            for h in range(4):
                self.stt(mx[:, h * 128:(h + 1) * 128], og[0:64, h * 128:(h + 1) * 128], ss[:, 8 + h:9 + h], BV[:, c, 512 + h * 128:512 + (h + 1) * 128],
                         ALU.mult, ALU.mult, [og, ss, BV], [mx])
            self.ld("sp", self.MIXD[nb + c * 64:nb + (c + 1) * 64, 512:1024], mx[:, :], [mx], [self.MIXD])

    def odd_proj(self, j, XTold):
        kb, ar, PB = self.kb, self.ar, self.PB
        self.even_scratch()
        ar.reset()
        W = ar.alloc([128, DC, 3072], BF16, "WQKV")
        self.load_w_cast(W, self.inp["od_w_qkv"][j], 3072, [])
        xTs = [ar.alloc([128, DC, 512], BF16, "xT%d" % i) for i in range(2)]
        pqs = [ar.alloc([128, 16, 512], BF16, "pqs%d" % i) for i in range(2)]
        vs = [ar.alloc([128, 4, 1024], BF16, "vs%d" % i) for i in range(2)]
        for blk in range(self.N // 512):
            n0 = blk * 512
            b2 = blk % 2
            xT = xTs[b2]
            self.ld("sp", xT[:, :, :], XTold[:, n0:n0 + 512].rearrange("(c p) t -> p c t", p=128), [XTold], [xT])
            for g in range(16):
                pb = PB[g % 2]
                for c in range(DC):
                    self.mm(pb[:, :], W[:, c, g * 128:(g + 1) * 128], xT[:, c, :], c == 0, c == DC - 1, [W, xT], [pb])
                self.cp("act" if g % 2 == 0 else "dve", pqs[b2][:, g, :], pb[:, :], [pb], [pqs[b2]])
            self.ld("sp", self.PQ[:, n0:n0 + 512].rearrange("(g p) t -> p g t", p=128), pqs[b2][:, :, :], [pqs[b2]], [self.PQ])
            for u in range(4):
                for h in range(2):
                    pb = PB[2 + (2 * u + h) % 4]
                    for c in range(DC):
                        self.mm(pb[:, :], xT[:, c, u * 128:(u + 1) * 128], W[:, c, 2048 + h * 512:2048 + (h + 1) * 512], c == 0, c == DC - 1, [xT, W], [pb])
                    self.cp("act" if h == 0 else "dve", vs[b2][:, u, h * 512:(h + 1) * 512], pb[:, :], [pb], [vs[b2]])
            self.ld("sp", self.BVd[n0:n0 + 512, :].rearrange("(u p) f -> p u f", p=128), vs[b2][:, :, :], [vs[b2]], [self.BVd])

    def diffattn(self, j, sq, lambda_init):
        kb, ar, PB = self.kb, self.ar, self.PB
        inp = self.inp
        S, NT, NB = self.S, self.NT, self.NB
        nb = sq * S
        ar.reset()
        QT = ar.alloc([128, 8, S], BF16, "QT")
        KT = ar.alloc([128, 8, S], BF16, "KT")
        V1 = ar.alloc([128, NT, 8, 129], BF16, "V1")
        self.ld("sp", QT[:, :, :], self.PQ[0:1024, nb:nb + S].rearrange("(g p) t -> p g t", p=128), [self.PQ], [QT])
        self.ld("sp", KT[:, :, :], self.PQ[1024:2048, nb:nb + S].rearrange("(g p) t -> p g t", p=128), [self.PQ], [KT])
        kb.op("pool", lambda e: e.memset(V1[:, :, :, 128:129], 1.0), writes=[V1])
        for t in range(NT):
            self.ld("sp", V1[:, t, :, 0:128], self.BVd[nb + t * 128:nb + (t + 1) * 128, :].rearrange("p (h e) -> p h e", e=128), [self.BVd], [V1])
        LM = ar.alloc([128, 4, 64], F32, "LM")
        lm = ar.alloc([128, 16], F32, "lm")
        SG = ar.alloc([128, 128], F32, "SG")
        epsr = ar.alloc([128, 1], F32, "epsr")
        kb.op("pool", lambda e: e.memset(epsr[:, :], RMS_EPS), writes=[epsr])
        self.ld("sp", LM[:, :, :].rearrange("p a b -> p (a b)"), inp["od_lam"][j:j + 1].rearrange("o a b -> o (a b)").partition_broadcast(128), [], [LM])
        self.bcast_load(SG, inp["od_subln_g"][j:j + 1, :], [])
        kb.op("dve", lambda e: e.tensor_scalar(out=SG[:, :], in0=SG[:, :], scalar1=1.0 - lambda_init, scalar2=None, op0=ALU.mult), reads=[SG], writes=[SG])
        junk = ar.alloc([128, 128], F32, "junk")
        for q in range(2):
            kb.op("dve", lambda e, q=q: e.tensor_tensor(out=junk[:, 0:64], in0=LM[:, 2 * q, :], in1=LM[:, 2 * q + 1, :], op=ALU.mult), reads=[LM], writes=[junk])
            kb.op("dve", lambda e, q=q: e.tensor_reduce(out=lm[:, q:q + 1], in_=junk[:, 0:64], axis=AX.X, op=ALU.add), reads=[junk], writes=[lm])
        self.act(lm[:, 2:4], lm[:, 0:2], AF.Exp, [lm], [lm])
        self.tt("dve", lm[:, 4:5], lm[:, 3:4], lm[:, 2:3], ALU.subtract, [lm], [lm])
        self.ts("dve", lm[:, 5:6], lm[:, 4:5], -lambda_init, None, ALU.add, ALU.bypass, [lm], [lm])
        PTs = [ar.alloc([128, 512], BF16, "PT%d" % i) for i in range(4)]
        dt_ = ar.alloc([128, 128], F32, "dt")
        st = ar.alloc([128, 16], F32, "st")
        mos = [ar.alloc([128, 4, 128], BF16, "mo%d" % i) for i in range(2)]
        pk = 0
        it = 0
        for h in range(8):
            for tb in range(NB):
                self.pump(1)

                def bank(m, u):
                    return PB[4 + 2 * m + u // 2], (u % 2) * 129
                for b in range(4):
                    self.mm(PB[4 + b][:, 0:258], self.zb[:, 0:128], self.zb[:, 0:258], True, False, [self.zb], [PB[4 + b]], skip=True)
                steps = [(m, i) for m in range(2) for i in range(4 * tb + 4)]
                pts = {}

                def stA(k, h=h, tb=tb):
                    nonlocal pk
                    m, i = steps[k]
                    ms = slice(m * 64, (m + 1) * 64)
                    u0 = max(0, i - 4 * tb)
                    w = 512 - u0 * 128
                    ps = PB[pk % 4]
                    PT = PTs[pk % 4]
                    pk += 1
                    pts[k] = PT
                    self.mm(ps[:, 0:w], KT[ms, h, i * 128:(i + 1) * 128], QT[ms, h, tb * 512 + u0 * 128:(tb + 1) * 512], True, True, [KT, QT], [ps])
                    self.act(PT[:, 0:w], ps[:, 0:w], AF.Exp, [ps], [PT], scale=0.125)
                    if i >= 4 * tb:
                        self.tt("pool", PT[:, 0:128], PT[:, 0:128], self.trib[:, :], ALU.mult, [PT, self.trib], [PT])

                def stB(k, h=h, tb=tb):
                    m, i = steps[k]
                    u0 = max(0, i - 4 * tb)
                    PT = pts.pop(k)
                    for u in range(u0, 4):
                        bk, off = bank(m, u)
                        self.mm(bk[:, off:off + 129], PT[:, (u - u0) * 128:(u - u0 + 1) * 128], V1[:, i, h, :], False, i == 4 * tb + u, [PT, V1], [bk], skip=True)
                LA_ = 2
                for k in range(min(LA_, len(steps))):
                    stA(k)
                for k in range(len(steps)):
                    if k + LA_ < len(steps):
                        stA(k + LA_)
                    stB(k)
                mo = mos[it % 2]
                it += 1
                for u in range(4):
                    b0, o0 = bank(0, u)
                    b1, o1 = bank(1, u)
                    kb.op("dve", lambda e, b0=b0, o0=o0: e.reciprocal(out=st[:, 0:1], in_=b0[:, o0 + 128:o0 + 129]), reads=[b0], writes=[st])
                    kb.op("dve", lambda e, b1=b1, o1=o1: e.reciprocal(out=st[:, 1:2], in_=b1[:, o1 + 128:o1 + 129]), reads=[b1], writes=[st])
                    self.tt("dve", st[:, 2:3], st[:, 1:2], lm[:, 5:6], ALU.mult, [st, lm], [st])
                    self.ts("dve", dt_[:, :], b0[:, o0:o0 + 128], st[:, 0:1], None, ALU.mult, ALU.bypass, [b0, st], [dt_])
                    self.stt(dt_[:, :], b1[:, o1:o1 + 128], st[:, 2:3], dt_[:, :], ALU.mult, ALU.add, [b1, st, dt_], [dt_])
                    self.act(junk[:, :], dt_[:, :], AF.Square, [dt_], [junk, st], accum_out=st[:, 3:4])
                    self.act(st[:, 4:5], st[:, 3:4], AF.Sqrt, [st, epsr], [st], scale=1.0 / 128, bias=epsr[:, 0:1])
                    kb.op("dve", lambda e: e.reciprocal(out=st[:, 5:6], in_=st[:, 4:5]), reads=[st], writes=[st])
                    self.stt(mo[:, u, :], dt_[:, :], st[:, 5:6], SG[:, :], ALU.mult, ALU.mult, [dt_, st, SG], [mo])
                self.ld("sp", self.MIXD[nb + tb * 512:nb + (tb + 1) * 512, h * 128:(h + 1) * 128].rearrange("(u p) e -> p u e", p=128), mo[:, :, :], [mo], [self.MIXD])

    def build(self):
        inp = self.inp
        NL = self.NL
        nsub = 2 * NL
        xs = [inp["x"]] + [self.XA if k % 2 == 0 else self.XB for k in range(nsub - 1)] + [self.out]
        ph = self.dbg
        on = lambda p: ph is None or p in ph
        self.queue_conversions()
        self.pump(0, upto=(0, 0))
        if on("init"):
            self.init_xt(inp["x"], self.XT[0])
        k = 0
        for li in range(NL):
            j = li // 2
            if li % 2 == 0:
                if on("eproj"):
                    self.even_proj(j, self.XT[k % 2])
                for sq in range(self.NSEQ):
                    if on("dsa"):
                        self.dsa(j, sq)
                    if on("gla"):
                        self.gla(j, sq)
                if on("oproj"):
                    self.outproj_phase(inp["ev_w_out"][j], xs[k], xs[k + 1], self.XT[(k + 1) % 2], inp["ev_ln1_g"][j:j + 1, :], inp["ev_ln1_b"][j:j + 1, :])
                k += 1
                if on("ffn"):
                    self.ffn_phase(j, xs[k], xs[k + 1], self.XT[k % 2], self.XT[(k + 1) % 2], inp["ev_ln2_g"][j:j + 1, :], inp["ev_ln2_b"][j:j + 1, :])
                k += 1
            else:
                lambda_init = 0.8 - 0.6 * math.exp(-0.3 * li)
                if on("qkv"):
                    self.odd_proj(j, self.XT[k % 2])
                for sq in range(self.NSEQ):
                    if on("dattn"):
                        self.diffattn(j, sq, lambda_init)
                if on("oproj2"):
                    self.outproj_phase(inp["od_w_out"][j], xs[k], xs[k + 1], self.XT[(k + 1) % 2], inp["od_ln1_g"][j:j + 1, :], inp["od_ln1_b"][j:j + 1, :],
                                       router=(inp["od_router_w"][j], inp["od_router_b"][j:j + 1, :]))
                k += 1
                if on("moe"):
                    self.route(xs[k])
                    self.moe_sparse(j, xs[k], xs[k + 1], self.XT[(k + 1) % 2], inp["od_ln2_g"][j:j + 1, :], inp["od_ln2_b"][j:j + 1, :])
                k += 1
        self.kb.barrier()
        self.kb.emit()
        return self.nc


def make_consts():
    c = np.zeros((128, 1024), np.float32)
    p = np.arange(128)
    c[:, 0:128] = np.eye(128, dtype=np.float32)
    c[:, 128:256] = np.where(p[None, :] <= p[:, None], 0.0, -1e30)
    c[:, 256:384] = (p[:, None] <= p[None, :]).astype(np.float32)
    c[:, 384:896] = (np.arange(512)[None, :] % 64 != 0).astype(np.float32)
    c[:, 896:960] = ((p[:, None] % 64) <= np.arange(64)[None, :]).astype(np.float32)
    c[:, 960:968] = 512.0 * np.arange(8)[None, :]
    c[:, 968:992] = 512.0 * np.arange(24)[None, :]
    c[:, 992:1003] = np.arange(11)[None, :]
    c[:, 1003] = 11.0 * p
    return c


_CACHE = {}


def run_prog(inputs, S, NSEQ, NL, ncores, trace=False):
    key = (S, NSEQ, NL)
    if key not in _CACHE:
        p_ = Prog(S, NSEQ, NL)
        _CACHE[key] = (p_.build(), set(p_.inp.keys()))
    nc, names = _CACHE[key]
    x = np.ascontiguousarray(inputs["x"], dtype=np.float32)
    shared = {"cst": make_consts()}
    for name, v in inputs.items():
        if name == "x" or name.startswith("od_lam_") or name not in names:
            continue
        v = np.ascontiguousarray(v, dtype=np.float32)
        if name == "ev_b_norm_g":
            v = v.reshape(2, 512)
        shared[name] = v
    shared["od_lam"] = np.ascontiguousarray(np.stack([inputs["od_lam_q1"], inputs["od_lam_k1"], inputs["od_lam_q2"], inputs["od_lam_k2"]], axis=1), dtype=np.float32)
    in_maps = []
    for c in range(ncores):
        m = dict(shared)
        m["x"] = np.ascontiguousarray(x[c * NSEQ:(c + 1) * NSEQ].reshape(NSEQ * S, D))
        in_maps.append(m)
    res = run_bass_kernel_spmd(nc, in_maps, core_ids=list(range(ncores)), trace=trace)
    out = np.concatenate([r["out"].reshape(NSEQ, S, D) for r in res.results], axis=0)
    return out, res


def kernel(**inputs):
    out, _ = run_prog(inputs, 2048, 2, 4, 8)
    return out.astype(np.float32)
```

```python
import math
import numpy as np
import ml_dtypes
from contextlib import ExitStack
import concourse.bass as bass
import concourse.mybir as mybir
from concourse.bass_utils import run_bass_kernel_spmd

F32 = mybir.dt.float32
BF16 = mybir.dt.bfloat16
U8 = mybir.dt.uint8
ALU = mybir.AluOpType
AF = mybir.ActivationFunctionType
AX = mybir.AxisListType

D = 1024
DC = 8
DEPTH = 4
ALPHA = (2.0 * DEPTH) ** 0.25
LN_EPS = 1e-5
RMS_EPS = 1e-6
DFF = 2816
FC = 22
NEXP = 8
EVEN_IN = 2776


class Res:
    __slots__ = ("ws", "rs", "name")

    def __init__(self, name=""):
        self.ws = {}
        self.rs = {}
        self.name = name


class T:
    def __init__(self, ap, name="", res=None):
        self.ap = ap
        self.name = name
        self.res = res if res is not None else Res(name)

    def __getitem__(self, idx):
        return self.ap[idx]


def _res(x):
    return x.res if isinstance(x, T) else x


class KB:
    ENG = ("pe", "act", "dve", "pool", "sp")
    NDMA = 8

    def __init__(self, nc):
        self.nc = nc
        self.stack = ExitStack()
        self.prog = {e: [] for e in self.ENG}
        self.cnt = {e: 0 for e in self.ENG}
        self.seen = {e: {} for e in self.ENG}
        self.sems = {e: self.stack.enter_context(nc.semaphore("s_" + e)) for e in self.ENG}
        self.dsem, self.dcnt, self.dnext = {}, {}, {}
        for q in ("sp", "pool", "act", "bulk"):
            self.dsem[q] = [self.stack.enter_context(nc.semaphore("d_%s%d" % (q, i))) for i in range(self.NDMA)]
            self.dcnt[q] = [0] * self.NDMA
            self.dnext[q] = 0
        self.nalloc = 0
        self.nins = 0

    def sb(self, shape, dtype, name=None):
        self.nalloc += 1
        name = name or "t%d" % self.nalloc
        h = self.stack.enter_context(self.nc.sbuf_tensor(name, list(shape), dtype))
        return T(h[tuple(slice(None) for _ in shape)], name)

    def ps(self, shape, dtype, name=None):
        self.nalloc += 1
        name = name or "p%d" % self.nalloc
        h = self.stack.enter_context(self.nc.psum_tensor(name, list(shape), dtype))
        return T(h[tuple(slice(None) for _ in shape)], name)

    def dram(self, name, shape, dtype, kind="Internal"):
        h = self.nc.dram_tensor(name, list(shape), dtype, kind=kind)
        return T(h[tuple(slice(None) for _ in shape)], name)

    def _waits(self, eng, reads, writes):
        need = {}
        seen = self.seen[eng]

        def add(d):
            for key, (val, src) in d.items():
                if src == "pe" and eng == "pe":
                    continue
                if seen.get(key, 0) >= val:
                    continue
                if need.get(key, 0) < val:
                    need[key] = val

        for r in reads:
            add(_res(r).ws)
        for w in writes:
            w = _res(w)
            add(w.ws)
            add(w.rs)
        out = []
        for key, val in need.items():
            seen[key] = val
            out.append((key, val))
        return out

    def _commit(self, ev, reads, writes):
        key, val, src = ev
        for r in reads:
            _res(r).rs[key] = (val, src)
        for w in writes:
            w = _res(w)
            w.ws[key] = (val, src)
            w.rs = {}

    def _semof(self, key):
        if isinstance(key, str):
            return self.sems[key]
        return self.dsem[key[0]][key[1]]

    def op(self, eng, fn, reads=(), writes=()):
        waits = self._waits(eng, reads, writes)
        self.cnt[eng] += 1
        ev = (eng, self.cnt[eng], eng)
        self.prog[eng].append((waits, fn, (eng, 1)))
        self._commit(ev, reads, writes)
        self.nins += 1
        return ev

    def dma(self, q, fn, reads=(), writes=(), ring=None):
        ring = ring or q
        i = self.dnext[ring]
        self.dnext[ring] = (i + 1) % self.NDMA
        key = (ring, i)
        waits = self._waits(q, reads, writes)
        prev = self.dcnt[ring][i]
        if prev and self.seen[q].get(key, 0) < prev:
            self.seen[q][key] = prev
            waits.append((key, prev))
        self.dcnt[ring][i] = prev + 16
        ev = (key, prev + 16, "dma")
        self.prog[q].append((waits, fn, (key, 16)))
        self._commit(ev, reads, writes)
        self.nins += 1
        return ev

    def barrier(self, full=False):
        for e in self.ENG:
            waits = []
            for f in self.ENG:
                if f != e and self.cnt[f] and self.seen[e].get(f, 0) < self.cnt[f]:
                    self.seen[e][f] = self.cnt[f]
                    waits.append((f, self.cnt[f]))
            if self.cnt[e] and e != "pe" and self.seen[e].get(e, 0) < self.cnt[e]:
                self.seen[e][e] = self.cnt[e]
                waits.append((e, self.cnt[e]))
            for q in self.dsem:
                if q == "bulk" and not full:
                    continue
                for i in range(self.NDMA):
                    v = self.dcnt[q][i]
                    if v and self.seen[e].get((q, i), 0) < v:
                        self.seen[e][(q, i)] = v
                        waits.append(((q, i), v))
            if waits:
                self.prog[e].append((waits, None, None))

    def emit(self):
        nc = self.nc
        with nc.Block() as block:
            def run(e, name):
                for waits, fn, inc in self.prog[name]:
                    for key, val in waits:
                        e.wait_ge(self._semof(key), val)
                    if fn is not None:
                        fn(e).then_inc(self._semof(inc[0]), inc[1])

            @block.tensor
            def _(e):
                run(e, "pe")

            @block.scalar
            def _(e):
                run(e, "act")

            @block.vector
            def _(e):
                run(e, "dve")

            @block.gpsimd
            def _(e):
                run(e, "pool")

            @block.sync
            def _(e):
                run(e, "sp")
        self.stack.close()


class Arena:
    def __init__(self, kb, nbytes):
        self.kb = kb
        self.t = kb.sb([128, nbytes], U8, name="arena")
        self.n = nbytes
        self.off = 0

    def reset(self):
        self.kb.barrier()
        self.off = 0

    def alloc(self, shape, dtype, name=""):
        nb = 4 if dtype == F32 else 2
        n = int(np.prod(shape[1:])) * nb
        n_al = (n + 63) // 64 * 64
        assert self.off + n_al <= self.n, "arena overflow %s %d+%d>%d" % (name, self.off, n_al, self.n)
        ap = self.t.ap[:, self.off:self.off + n].bitcast(dtype)
        self.off += n_al
        if len(shape) == 3:
            ap = ap.rearrange("p (a b) -> p a b", b=shape[2])
        elif len(shape) == 4:
            ap = ap.rearrange("p (a b c) -> p a b c", b=shape[2], c=shape[3])
        if shape[0] < 128:
            ap = ap[0:shape[0]]
        return T(ap, name)


class Prog:
    def __init__(self, S, NSEQ, NL, dbg=None):
        self.S, self.NSEQ, self.NL = S, NSEQ, NL
        self.N = S * NSEQ
        self.NT = S // 128
        self.NB = S // 512
        self.n_sel = min(256, S // 4)
        self.dbg = dbg
        nc = self.nc = bass.Bass("TRN2", target_bir_lowering=False)
        kb = self.kb = KB(nc)
        N = self.N
        self.inp = {}

        def ext(name, shape, dt=F32):
            self.inp[name] = kb.dram(name, shape, dt, kind="ExternalInput")
            return self.inp[name]

        ext("x", [N, D])
        ext("cst", [128, 1024])
        ext("ev_w_in", [2, D, EVEN_IN]); ext("ev_a_kv_norm_g", [2, 128]); ext("ev_a_w_uk", [2, 128, 64]); ext("ev_a_w_uv", [2, 128, 64])
        ext("ev_a_kidx_ln_g", [2, 64]); ext("ev_a_kidx_ln_b", [2, 64]); ext("ev_b_w_g2", [2, 16, 256]); ext("ev_b_b_g", [2, 256])
        ext("ev_b_norm_g", [2, 512]); ext("ev_w_out", [2, D, D]); ext("ev_ln1_g", [2, D]); ext("ev_ln1_b", [2, D])
        ext("ev_ffn_w_gate", [2, D, DFF]); ext("ev_ffn_w_up", [2, D, DFF]); ext("ev_ffn_w_down", [2, DFF, D])
        ext("ev_ln2_g", [2, D]); ext("ev_ln2_b", [2, D])
        ext("od_w_qkv", [2, D, 3072]); ext("od_lam", [2, 4, 64]); ext("od_subln_g", [2, 128]); ext("od_w_out", [2, D, D])
        ext("od_ln1_g", [2, D]); ext("od_ln1_b", [2, D]); ext("od_router_w", [2, D, 8]); ext("od_router_b", [2, 8])
        if NL > 1:
            ext("od_moe_w_gate", [2, NEXP, D, DFF]); ext("od_moe_w_up", [2, NEXP, D, DFF]); ext("od_moe_w_down", [2, NEXP, DFF, D])
        ext("od_ln2_g", [2, D]); ext("od_ln2_b", [2, D])
        self.out = kb.dram("out", [N, D], F32, kind="ExternalOutput")
        self.XA = kb.dram("XA", [N, D], F32)
        self.XB = kb.dram("XB", [N, D], F32)
        self.XT = [kb.dram("XT0", [D, N], BF16), kb.dram("XT1", [D, N], BF16)]
        self.MIXD = kb.dram("MIXD", [N, D], BF16)
        NG = self.NG = (2 * N) // 512 + NEXP
        def table(name):
            full = [kb.dram("%s%d" % (name, j), [(NEXP + 1) * 1408, 2048], BF16) for j in range(2)]
            parts = [[T(full[j].ap[sl * 1408:(sl + 1) * 1408, :], "%s%d_%d" % (name, j, sl)) for sl in range(NEXP + 1)] for j in range(2)]
            return full, parts
        self.TgF, self.Tg = table("Tg")
        self.TuF, self.Tu = table("Tu")
        self.TdF, self.Td = table("Td")
        self.XS = kb.dram("XS", [NG * 512, D], BF16)
        self.YS = kb.dram("YS", [NG * 512, D], F32)
        self.PB = [kb.ps([128, 512], F32, name="bank%d" % i) for i in range(8)]
        self.cstf = kb.sb([128, 1024], F32, name="cstf")
        self.identb = kb.sb([128, 128], BF16, name="identb")
        self.trib = kb.sb([128, 128], BF16, name="trib")
        self.trigb = kb.sb([128, 64], BF16, name="trigb")
        self.zb = kb.sb([128, 512], BF16, name="zb")
        NTT = self.NTT = NSEQ * self.NT
        self.comb = kb.sb([128, NTT, 8], F32, name="comb")
        self.sel12 = kb.sb([128, NTT, 16], F32, name="sel12")
        self.gates = kb.sb([128, NTT, 2], F32, name="gates")
        self.pidx = kb.sb([128, NTT, 2], mybir.dt.int32, name="pidx")
        self.idxq = kb.sb([128, NG, 11], mybir.dt.int32, name="idxq")
        self.onesb = kb.sb([128, 128], BF16, name="onesb")
        self.onesf = kb.sb([128, 8], F32, name="onesf")
        self.ustb = kb.sb([128, 128], BF16, name="ustb")
        self.ar = Arena(kb, 192 * 1024)
        self.identf = T(self.cstf[:, 0:128], "identf", self.cstf.res)
        self.cbias = T(self.cstf[:, 128:256], "cbias", self.cstf.res)
        self.rst = T(self.cstf[:, 384:896], "rst", self.cstf.res)
        kb.dma("sp", lambda e: e.dma_start(out=self.cstf[:, :], in_=self.inp["cst"][:, :]), reads=[self.inp["cst"]], writes=[self.cstf])
        kb.op("dve", lambda e: e.tensor_copy(out=self.identb[:, :], in_=self.cstf[:, 0:128]), reads=[self.cstf], writes=[self.identb])
        kb.op("dve", lambda e: e.tensor_copy(out=self.trib[:, :], in_=self.cstf[:, 256:384]), reads=[self.cstf], writes=[self.trib])
        kb.op("dve", lambda e: e.tensor_copy(out=self.trigb[:, :], in_=self.cstf[:, 896:960]), reads=[self.cstf], writes=[self.trigb])
        kb.op("pool", lambda e: e.memset(self.zb[:, :], 0.0), writes=[self.zb])
        kb.op("pool", lambda e: e.memset(self.onesb[:, :], 1.0), writes=[self.onesb])
        kb.op("pool", lambda e: e.memset(self.onesf[:, :], 1.0), writes=[self.onesf])
        kb.op("dve", lambda e: e.tensor_tensor(out=self.ustb[:, :], in0=self.cstf[:, 256:384], in1=self.cstf[:, 0:128], op=ALU.subtract),
              reads=[self.cstf], writes=[self.ustb])
        self.conv_q = []

    def mm(self, out, lhsT, rhs, start, stop, reads, writes, skip=False):
        self.kb.op("pe", lambda e: e.matmul(out, lhsT=lhsT, rhs=rhs, start=start, stop=stop, skip_group_check=skip), reads=reads, writes=writes)

    def tp(self, out, in_, ident, reads, writes):
        self.kb.op("pe", lambda e: e.transpose(out, in_, ident), reads=reads, writes=writes)

    def act(self, out, in_, func, reads, writes, **kw):
        self.kb.op("act", lambda e: e.activation(out=out, in_=in_, func=func, **kw), reads=reads, writes=writes)

    def tt(self, eng, out, in0, in1, op, reads, writes):
        self.kb.op(eng, lambda e: e.tensor_tensor(out=out, in0=in0, in1=in1, op=op), reads=reads, writes=writes)

    def ts(self, eng, out, in0, s1, s2, op0, op1, reads, writes, accum=None):
        if accum is None:
            self.kb.op(eng, lambda e: e.tensor_scalar(out=out, in0=in0, scalar1=s1, scalar2=s2, op0=op0, op1=op1), reads=reads, writes=writes)
        else:
            self.kb.op(eng, lambda e: e.tensor_scalar(out=out, in0=in0, scalar1=s1, scalar2=s2, op0=op0, op1=op1, accum_out=accum), reads=reads, writes=writes)

    def stt(self, out, in0, scalar, in1, op0, op1, reads, writes):
        self.kb.op("dve", lambda e: e.scalar_tensor_tensor(out=out, in0=in0, scalar=scalar, in1=in1, op0=op0, op1=op1), reads=reads, writes=writes)

    def cp(self, eng, out, in_, reads, writes):
        if eng == "act":
            self.kb.op("act", lambda e: e.copy(out=out, in_=in_), reads=reads, writes=writes)
        else:
            self.kb.op(eng, lambda e: e.tensor_copy(out=out, in_=in_), reads=reads, writes=writes)

    def ld(self, q, out, in_, reads, writes):
        return self.kb.dma(q, lambda e: e.dma_start(out=out, in_=in_), reads=reads, writes=writes)

    def bcast_load(self, dst, src_row, reads):
        self.ld("sp", dst[:, :], src_row.partition_broadcast(128), reads, [dst])

    def load_w_cast(self, dst, src2d, ncols, reads):
        step = 1024
        for c0 in range(0, ncols, step):
            c1 = min(ncols, c0 + step)
            self.ld("pool", dst[:, :, c0:c1], src2d[:, c0:c1].rearrange("(c p) n -> p c n", p=128), reads, [dst])

    def queue_conversions(self):
        inp = self.inp
        for j in range(2):
            srcs = []
            if 2 * j < self.NL:
                srcs.append((NEXP, inp["ev_ffn_w_gate"][j], inp["ev_ffn_w_up"][j], inp["ev_ffn_w_down"][j]))
            if 2 * j + 1 < self.NL:
                for e in range(NEXP):
                    srcs.append((e, inp["od_moe_w_gate"][j, e], inp["od_moe_w_up"][j, e], inp["od_moe_w_down"][j, e]))
            for (slot, g, u, d) in srcs:
                for (tab, src) in ((self.Tg, g), (self.Tu, u)):
                    for c0 in (0, 4):
                        self.conv_q.append((j, slot, tab[j][slot],
                                            tab[j][slot][:, :].rearrange("(p q) (c f) -> p q c f", q=11, f=256)[:, :, c0:c0 + 4, :],
                                            src.rearrange("(c p) (q f) -> p q c f", p=128, f=256)[:, :, c0:c0 + 4, :]))
                self.conv_q.append((j, slot, self.Td[j][slot],
                                    self.Td[j][slot][:, :].rearrange("(p q) (a n) -> p q a n", q=11, n=1024),
                                    d.rearrange("(q a p) n -> p q a n", a=2, p=128)))

    def pump(self, n=1, upto=None):
        while self.conv_q and (n > 0 or (upto is not None and (self.conv_q[0][0], self.conv_q[0][1] != NEXP) <= upto)):
            j, slot, t, dst, src = self.conv_q.pop(0)
            self.kb.dma("pool", lambda e, dst=dst, src=src: e.dma_start(out=dst, in_=src), reads=[], writes=[t], ring="bulk")
            n -= 1

    def ln_tile(self, hsrc, hres, Xold, Xnew, XTnew, n0, gbc, bbc, bufs, router=None):
        kb = self.kb
        par = (n0 // 128) % 2
        bufs = dict(bufs)
        for nm in ("xo", "z", "st", "xtb", "xtl"):
            if nm in bufs and isinstance(bufs[nm], list):
                bufs[nm] = bufs[nm][par]
        xo, z, st, xtb = bufs["xo"], bufs["z"], bufs["st"], bufs["xtb"]
        self.ld("sp", xo[:, :], Xold[n0:n0 + 128, :], [Xold], [xo])
        for h in range(2):
            self.stt(z[:, h * 512:(h + 1) * 512], xo[:, h * 512:(h + 1) * 512], ALPHA, hsrc[h], ALU.mult, ALU.add, [xo] + hres, [z])
        for h in range(2):
            kb.op("dve", lambda e, h=h: e.bn_stats(out=st[:, h * 6:(h + 1) * 6], in_=z[:, h * 512:(h + 1) * 512]), reads=[z], writes=[st])
        kb.op("dve", lambda e: e.bn_aggr(out=st[:, 12:14], in_=st[:, 0:12]), reads=[st], writes=[st])
        self.act(st[:, 14:15], st[:, 13:14], AF.Sqrt, [st], [st], bias=bufs["eps"][:, 0:1], scale=1.0)
        kb.op("dve", lambda e: e.reciprocal(out=st[:, 15:16], in_=st[:, 14:15]), reads=[st], writes=[st])
        self.ts("dve", st[:, 16:17], st[:, 12:13], -1.0, st[:, 15:16], ALU.mult, ALU.mult, [st], [st])
        self.act(xo[:, :], z[:, :], AF.Identity, [z, st], [xo], scale=st[:, 15:16], bias=st[:, 16:17])
        self.tt("dve", xo[:, :], xo[:, :], gbc[:, :], ALU.mult, [xo, gbc], [xo])
        self.tt("pool", xo[:, :], xo[:, :], bbc[:, :], ALU.add, [xo, bbc], [xo])
        self.ld("sp", Xnew[n0:n0 + 128, :], xo[:, :], [xo], [Xnew])
        if XTnew is not None:
            pb = [self.PB[6], self.PB[7]]
            for c in range(DC):
                self.tp(pb[c // 4][:, (c % 4) * 128:(c % 4 + 1) * 128], xo[:, c * 128:(c + 1) * 128], self.identf[:, :], [xo, self.identf], [pb[c // 4]])
            for h in range(2):
                self.cp("act", xtb[:, h * 4:(h + 1) * 4, :], pb[h][:, :].rearrange("p (c t) -> p c t", t=128), [pb[h]], [xtb])
            self.ld("sp", XTnew[:, n0:n0 + 128].rearrange("(c p) t -> p c t", p=128), xtb[:, :, :], [xtb], [XTnew])
            if router is not None:
                self.router_tile(pb, router, n0, bufs)

    def router_tile(self, pb, router, n0, bufs):
        kb = self.kb
        wrh, wrl, brb = router
        xtl, st, xtb = bufs["xtl"], bufs["st"], bufs["xtb"]
        for h in range(2):
            self.tt("dve", xtl[:, h * 4:(h + 1) * 4, :], pb[h][:, :].rearrange("p (c t) -> p c t", t=128), xtb[:, h * 4:(h + 1) * 4, :],
                    ALU.subtract, [pb[h], xtb], [xtl])
        lg = self.PB[5]
        k = 0
        for c in range(DC):
            for (xa, wa) in ((xtb, wrh), (xtb, wrl), (xtl, wrh)):
                self.mm(lg[:, 0:8], xa[:, c, :], wa[:, c, :], k == 0, k == 3 * DC - 1, [xa, wa], [lg])
                k += 1
        tix = n0 // 128
        L = st[:, 24:32]
        self.tt("dve", L, lg[:, 0:8], brb[:, :], ALU.add, [lg, brb], [st])
        kb.op("dve", lambda e: e.max(out=st[:, 32:40], in_=L), reads=[st], writes=[st])
        self.tt("dve", st[:, 40:41], st[:, 33:34], st[:, 32:33], ALU.subtract, [st], [st])
        self.act(st[:, 41:42], st[:, 40:41], AF.Exp, [st], [st])
        self.ts("dve", st[:, 42:43], st[:, 41:42], 1.0, None, ALU.add, ALU.bypass, [st], [st])
        kb.op("dve", lambda e: e.reciprocal(out=st[:, 43:44], in_=st[:, 42:43]), reads=[st], writes=[st])
        self.tt("dve", st[:, 44:45], st[:, 41:42], st[:, 43:44], ALU.mult, [st], [st])
        self.ts("dve", self.sel12[:, tix, 0:8], L, st[:, 32:33], None, ALU.is_equal, ALU.bypass, [st], [self.sel12])
        self.ts("dve", self.sel12[:, tix, 8:16], L, st[:, 33:34], None, ALU.is_equal, ALU.bypass, [st], [self.sel12])
        self.cp("dve", self.gates[:, tix, :], st[:, 43:45], [st], [self.gates])

    def ln_bufs(self):
        ar = self.ar
        b = {"xo": [ar.alloc([128, D], F32, "xo%d" % i) for i in range(2)], "z": [ar.alloc([128, D], F32, "z%d" % i) for i in range(2)],
             "st": [ar.alloc([128, 64], F32, "st%d" % i) for i in range(2)],
             "xtb": [ar.alloc([128, DC, 128], BF16, "xtb%d" % i) for i in range(2)], "eps": ar.alloc([128, 1], F32, "eps"),
             "gbc": ar.alloc([128, D], F32, "gbc"), "bbc": ar.alloc([128, D], F32, "bbc")}
        self.kb.op("pool", lambda e: e.memset(b["eps"][:, :], LN_EPS), writes=[b["eps"]])
        return b

    def ffn_setup(self):
        ar = self.ar
        f = {"xT": [ar.alloc([128, DC, 512], BF16, "xT%d" % i) for i in range(2)],
             "hT": ar.alloc([128, FC, 512], BF16, "hT"),
             "ring": [ar.alloc([128, 2048], BF16, "ring%d" % i) for i in range(12)],
             "sg": [ar.alloc([128, 512], BF16, "sg%d" % i) for i in range(2)],
             "ri": 0}
        return f

    def ffn_block(self, f, xT, load):
        self.ffn_p1(f, xT, load)
        self.ffn_p2(f, load)

    def ffn_p1(self, f, xT, load):
        PB = self.PB
        hT = f["hT"]
        k = 0
        for q in range(11):
            gs = f["ring"][f["ri"] % 12]; f["ri"] += 1
            us = f["ring"][f["ri"] % 12]; f["ri"] += 1
            load("g", q, gs)
            load("u", q, us)
            gv = gs[:, :].rearrange("p (c n) -> p c n", n=256)
            uv = us[:, :].rearrange("p (c n) -> p c n", n=256)
            for fl in range(2):
                fi = 2 * q + fl
                pg, pu = PB[(k % 2) * 2], PB[(k % 2) * 2 + 1]
                for c in range(DC):
                    self.mm(pg[:, :], gv[:, c, fl * 128:(fl + 1) * 128], xT[:, c, :], c == 0, c == DC - 1, [gs, xT], [pg])
                for c in range(DC):
                    self.mm(pu[:, :], uv[:, c, fl * 128:(fl + 1) * 128], xT[:, c, :], c == 0, c == DC - 1, [us, xT], [pu])
                sg = f["sg"][k % 2]
                self.act(sg[:, :], pg[:, :], AF.Silu, [pg], [sg])
                self.tt("dve", hT[:, fi, :], sg[:, :], pu[:, :], ALU.mult, [sg, pu], [hT])
                k += 1

    def ffn_p2(self, f, load):
        PB = self.PB
        hT = f["hT"]
        for q in range(11):
            ds = f["ring"][f["ri"] % 12]; f["ri"] += 1
            load("d", q, ds)
            dv = ds[:, :].rearrange("p (a n) -> p a n", n=D)
            for a_ in range(2):
                fi = 2 * q + a_
                for u in range(4):
                    for h in range(2):
                        self.mm(PB[2 * u + h][:, :], hT[:, fi, u * 128:(u + 1) * 128], dv[:, a_, h * 512:(h + 1) * 512],
                                fi == 0, fi == FC - 1, [hT, ds], [PB[2 * u + h]])

    def static_loader(self, j, slot):
        tabs = {"g": self.Tg[j][slot], "u": self.Tu[j][slot], "d": self.Td[j][slot]}

        def load(kind, q, dst):
            t = tabs[kind]
            self.ld("sp", dst[:, :], t[:, :].rearrange("(p q) n -> p q n", q=11)[:, q, :], [t], [dst])
        return load

    def ffn_phase(self, j, Xold, Xnew, XTold, XTnew, lng, lnb):
        kb, ar = self.kb, self.ar
        self.pump(0, upto=(j, 0))
        ar.reset()
        f = self.ffn_setup()
        lb = self.ln_bufs()
        self.bcast_load(lb["gbc"], lng, [])
        self.bcast_load(lb["bbc"], lnb, [])
        accs = [ar.alloc([128, 4, D], F32, "acc%d" % i) for i in range(2)]
        load = self.static_loader(j, NEXP)
        nblk = self.N // 512

        def lns(blk):
            acc = accs[blk % 2]
            for u in range(4):
                hs = [acc[:, u, 0:512], acc[:, u, 512:1024]]
                self.ln_tile(hs, [acc], Xold, Xnew, XTnew, blk * 512 + u * 128, lb["gbc"], lb["bbc"], lb)
        for blk in range(nblk):
            n0 = blk * 512
            xT = f["xT"][blk % 2]
            acc = accs[blk % 2]
            self.ld("sp", xT[:, :, :], XTold[:, n0:n0 + 512].rearrange("(c p) t -> p c t", p=128), [XTold], [xT])
            self.ffn_p1(f, xT, load)
            if blk > 0:
                lns(blk - 1)
            self.ffn_p2(f, load)
            self.pump(3)
            for u in range(4):
                for h in range(2):
                    self.cp("act" if h == 0 else "dve", acc[:, u, h * 512:(h + 1) * 512], self.PB[2 * u + h][:, :], [self.PB[2 * u + h]], [acc])
        lns(nblk - 1)

    def route(self, Xcur):
        kb, ar, PB = self.kb, self.ar, self.PB
        NTT, NG = self.NTT, self.NG
        I32 = mybir.dt.int32
        ar.reset()
        SEL = ar.alloc([128, NTT, 8], BF16, "SEL")
        RANK = ar.alloc([128, NTT, 8], F32, "RANK")
        carry = ar.alloc([128, 8], F32, "carry")
        w = ar.alloc([128, 256], F32, "rw")
        big = ar.alloc([128, NG, 11], F32, "rbig")
        t3 = ar.alloc([128, NTT, 8], F32, "rt3")
        pf = ar.alloc([128, NTT, 2], F32, "rpf")
        self.tt("dve", SEL[:, :, :], self.sel12[:, :, 0:8], self.sel12[:, :, 8:16], ALU.add, [self.sel12], [SEL])
        kb.op("pool", lambda e: e.memset(carry[:, :], 0.0), writes=[carry])
        for i in range(NTT):
            p = PB[i % 2]
            self.mm(p[:, 0:8], self.ustb[:, :], SEL[:, i, :], True, True, [self.ustb, SEL], [p])
            self.mm(p[:, 8:16], self.onesb[:, :], SEL[:, i, :], True, True, [self.onesb, SEL], [p])
            self.tt("dve", RANK[:, i, :], p[:, 0:8], carry[:, :], ALU.add, [p, carry], [RANK])
            self.tt("dve", carry[:, :], p[:, 8:16], carry[:, :], ALU.add, [p, carry], [carry])
        cst = self.cstf
        THR, GT, QC, P11 = cst[:, 960:968], cst[:, 968:968 + NG], cst[:, 992:1003], cst[:, 1003:1004]
        ge, pn, incl, off = w[:, 0:8], w[:, 8:16], w[:, 16:24], w[:, 24:32]
        cmp_ = w[:, 64:128].rearrange("p (e k) -> p e k", k=8)
        self.tt("dve", cmp_, carry[:, :].unsqueeze(2).to_broadcast([128, 8, 8]), THR.unsqueeze(1).to_broadcast([128, 8, 8]), ALU.is_gt, [carry, cst], [w])
        kb.op("dve", lambda e: e.tensor_reduce(out=ge, in_=cmp_, axis=AX.X, op=ALU.add), reads=[w], writes=[w])
        self.ts("dve", pn, ge, 512.0, None, ALU.mult, ALU.bypass, [w], [w])
        kb.op("dve", lambda e: e.tensor_tensor_scan(out=incl, data0=self.onesf[:, 0:8], data1=pn, initial=0.0, op0=ALU.mult, op1=ALU.add), reads=[w, self.onesf], writes=[w])
        self.tt("dve", off, incl, pn, ALU.subtract, [w], [w])
        self.tt("dve", t3[:, :, :], RANK[:, :, :], off.unsqueeze(1).to_broadcast([128, NTT, 8]), ALU.add, [RANK, w], [t3])
        for k in range(2):
            self.tt("dve", RANK[:, :, :], t3[:, :, :], self.sel12[:, :, k * 8:(k + 1) * 8], ALU.mult, [t3, self.sel12], [RANK])
            kb.op("dve", lambda e, k=k: e.tensor_reduce(out=pf[:, :, k], in_=RANK[:, :, :], axis=AX.X, op=ALU.add), reads=[RANK], writes=[pf])
        self.cp("dve", self.pidx[:, :, :], pf[:, :, :], [pf], [self.pidx])
        cmp2 = big[:, :, 0:8]
        self.tt("dve", cmp2, GT.unsqueeze(2).to_broadcast([128, NG, 8]), incl.unsqueeze(1).to_broadcast([128, NG, 8]), ALU.is_ge, [cst, w], [big])
        eg = w[:, 32:32 + NG]
        kb.op("dve", lambda e: e.tensor_reduce(out=eg, in_=cmp2, axis=AX.X, op=ALU.add), reads=[big], writes=[w])
        self.ts("dve", eg, eg, 7.0, 1408.0, ALU.min, ALU.mult, [w], [w])
        self.ts("dve", eg, eg, P11, None, ALU.add, ALU.bypass, [w, cst], [w])
        self.tt("dve", big[:, :, :], eg.unsqueeze(2).to_broadcast([128, NG, 11]), QC.unsqueeze(1).to_broadcast([128, NG, 11]), ALU.add, [w, cst], [big])
        self.cp("dve", self.idxq[:, :, :], big[:, :, :], [big], [self.idxq])
        xos = [ar.alloc([128, D], F32, "sxo%d" % i) for i in range(2)]
        xbs = [ar.alloc([128, D], BF16, "sxb%d" % i) for i in range(2)]
        for i in range(NTT):
            xo, xb = xos[i % 2], xbs[i % 2]
            self.ld("sp", xo[:, :], Xcur[i * 128:(i + 1) * 128, :], [Xcur], [xo])
            self.cp("act", xb[:, :], xo[:, :], [xo], [xb])
            for k in range(2):
                self.kb.dma("pool", lambda e, xb=xb, i=i, k=k: e.indirect_dma_start(
                    out=self.XS[:, :], out_offset=bass.IndirectOffsetOnAxis(self.pidx[:, i, k:k + 1], 0), in_=xb[:, :], in_offset=None),
                    reads=[xb, self.pidx], writes=[self.XS])

    def moe_sparse(self, j, Xold, Xnew, XTnew, lng, lnb):
        kb, ar, PB = self.kb, self.ar, self.PB
        NG = self.NG
        self.pump(0, upto=(j, 1))
        ar.reset()
        f = self.ffn_setup()
        xss = [ar.alloc([128, 4, D], BF16, "xs%d" % i) for i in range(2)]
        ys = ar.alloc([128, 4, D], F32, "ys")
        tabs = {"g": self.Tg[j], "u": self.Tu[j], "d": self.Td[j]}
        fulls = {"g": self.TgF[j], "u": self.TuF[j], "d": self.TdF[j]}
        tres = [t for k in tabs for t in tabs[k][0:NEXP]]
        for g in range(NG):
            xs = xss[g % 2]
            xT = f["xT"][g % 2]
            self.ld("sp", xs[:, :, :], self.XS[g * 512:(g + 1) * 512, :].rearrange("(u p) d -> p u d", p=128), [self.XS], [xs])
            for u in range(4):
                pt = PB[4 + u]
                ptb = pt[:, :].bitcast(BF16)
                for c in range(DC):
                    self.tp(ptb[:, c * 128:(c + 1) * 128], xs[:, u, c * 128:(c + 1) * 128], self.identb[:, :], [xs, self.identb], [pt])
                self.cp("act" if u % 2 == 0 else "dve", xT[:, :, u * 128:(u + 1) * 128], ptb.rearrange("p (c t) -> p c t", t=128), [pt], [xT])

            def load(kind, q, dst, g=g):
                src = fulls[kind].ap[:, :]
                self.kb.dma("pool", lambda e, src=src, dst=dst, q=q: e.indirect_dma_start(
                    out=dst[:, :], out_offset=None, in_=src, in_offset=bass.IndirectOffsetOnAxis(self.idxq[:, g, q:q + 1], 0)),
                    reads=tres + [self.idxq], writes=[dst])
            self.ffn_block(f, xT, load)
            for u in range(4):
                for h in range(2):
                    self.cp("act" if h == 0 else "dve", ys[:, u, h * 512:(h + 1) * 512], PB[2 * u + h][:, :], [PB[2 * u + h]], [ys])
            self.ld("sp", self.YS[g * 512:(g + 1) * 512, :].rearrange("(u p) d -> p u d", p=128), ys[:, :, :], [ys], [self.YS])
        ar.reset()
        lb = self.ln_bufs()
        self.bcast_load(lb["gbc"], lng, [])
        self.bcast_load(lb["bbc"], lnb, [])
        y1s = [ar.alloc([128, D], F32, "y1_%d" % i) for i in range(2)]
        y2s = [ar.alloc([128, D], F32, "y2_%d" % i) for i in range(2)]
        for i in range(self.NTT):
            y1, y2 = y1s[i % 2], y2s[i % 2]
            for k, y in ((0, y1), (1, y2)):
                self.kb.dma("pool", lambda e, y=y, i=i, k=k: e.indirect_dma_start(
                    out=y[:, :], out_offset=None, in_=self.YS[:, :], in_offset=bass.IndirectOffsetOnAxis(self.pidx[:, i, k:k + 1], 0)),
                    reads=[self.YS, self.pidx], writes=[y])
            self.ts("dve", y1[:, :], y1[:, :], self.gates[:, i, 0:1], None, ALU.mult, ALU.bypass, [y1, self.gates], [y1])
            self.stt(y1[:, :], y2[:, :], self.gates[:, i, 1:2], y1[:, :], ALU.mult, ALU.add, [y2, self.gates, y1], [y1])
            self.ln_tile([y1[:, 0:512], y1[:, 512:1024]], [y1], Xold, Xnew, XTnew, i * 128, lb["gbc"], lb["bbc"], lb)

    def outproj_phase(self, w_out, Xold, Xnew, XTnew, lng, lnb, router=None):
        kb, ar = self.kb, self.ar
        ar.reset()
        lb = self.ln_bufs()
        self.bcast_load(lb["gbc"], lng, [])
        self.bcast_load(lb["bbc"], lnb, [])
        W = ar.alloc([128, DC, D], BF16, "wout")
        self.load_w_cast(W, w_out, D, [])
        rt = None
        if router is not None:
            wr = ar.alloc([128, DC, 8], F32, "wr")
            wrh = ar.alloc([128, DC, 8], BF16, "wrh")
            wrl = ar.alloc([128, DC, 8], BF16, "wrl")
            brb = ar.alloc([128, 8], F32, "brb")
            lb["xtl"] = [ar.alloc([128, DC, 128], BF16, "xtl%d" % i) for i in range(2)]
            self.ld("sp", wr[:, :, :], router[0].rearrange("(c p) n -> p c n", p=128), [], [wr])
            self.cp("dve", wrh[:, :, :], wr[:, :, :], [wr], [wrh])
            self.tt("dve", wrl[:, :, :], wr[:, :, :], wrh[:, :, :], ALU.subtract, [wr, wrh], [wrl])
            self.bcast_load(brb, router[1], [])
            rt = (wrh, wrl, brb)
        mixs = [ar.alloc([128, D], BF16, "mix%d" % i) for i in range(2)]
        mixT = [ar.alloc([128, DC, 128], BF16, "mixT%d" % i) for i in range(2)]
        PB = self.PB
        ntile = self.N // 128

        def front(t):
            n0 = t * 128
            mx, mt = mixs[t % 2], mixT[t % 2]
            self.ld("sp", mx[:, :], self.MIXD[n0:n0 + 128, :], [self.MIXD], [mx])
            pt = PB[4 + (t % 2)]
            ptb = pt[:, :].bitcast(BF16)
            for c in range(DC):
                self.tp(ptb[:, c * 128:(c + 1) * 128], mx[:, c * 128:(c + 1) * 128], self.identb[:, :], [mx, self.identb], [pt])
            self.cp("act", mt[:, :, :], ptb.rearrange("p (c t) -> p c t", t=128), [pt], [mt])
            hb = [PB[(t % 2) * 2], PB[(t % 2) * 2 + 1]]
            for h in range(2):
                for c in range(DC):
                    self.mm(hb[h][:, :], mt[:, c, :], W[:, c, h * 512:(h + 1) * 512], c == 0, c == DC - 1, [mt, W], [hb[h]])
        front(0)
        for t in range(ntile):
            if t + 1 < ntile:
                front(t + 1)
            hb = [PB[(t % 2) * 2], PB[(t % 2) * 2 + 1]]
            self.ln_tile([hb[0][:, :], hb[1][:, :]], hb, Xold, Xnew, XTnew, t * 128, lb["gbc"], lb["bbc"], lb, router=rt)

    def init_xt(self, Xin, XTnew):
        ar = self.ar
        ar.reset()
        xos = [ar.alloc([128, D], F32, "ixo%d" % i) for i in range(2)]
        xtbs = [ar.alloc([128, DC, 128], BF16, "ixt%d" % i) for i in range(2)]
        for t in range(self.N // 128):
            n0 = t * 128
            xo, xtb = xos[t % 2], xtbs[t % 2]
            self.ld("sp", xo[:, :], Xin[n0:n0 + 128, :], [Xin], [xo])
            pb = [self.PB[(t % 2) * 2], self.PB[(t % 2) * 2 + 1]]
            for c in range(DC):
                self.tp(pb[c // 4][:, (c % 4) * 128:(c % 4 + 1) * 128], xo[:, c * 128:(c + 1) * 128], self.identf[:, :], [xo, self.identf], [pb[c // 4]])
            for h in range(2):
                self.cp("act" if h == 0 else "dve", xtb[:, h * 4:(h + 1) * 4, :], pb[h][:, :].rearrange("p (c t) -> p c t", t=128), [pb[h]], [xtb])
            self.ld("sp", XTnew[:, n0:n0 + 128].rearrange("(c p) t -> p c t", p=128), xtb[:, :, :], [xtb], [XTnew])

    def even_scratch(self):
        kb, N = self.kb, self.N
        if hasattr(self, "PQ"):
            return
        self.PQ = kb.dram("PQ", [2048, N], BF16)
        self.PBQK = kb.dram("PBQK", [512, N], F32)
        self.CNTd = kb.dram("CNTd", [128, N], BF16)
        self.KITd = kb.dram("KITd", [128, N], BF16)
        self.WId = kb.dram("WId", [N, 8], F32)
        self.BGTd = kb.dram("BGTd", [16, N], BF16)
        self.BVd = kb.dram("BVd", [N, 1024], BF16)

    def even_proj(self, j, XTold):
        kb, ar, PB = self.kb, self.ar, self.PB
        inp = self.inp
        self.even_scratch()
        ar.reset()
        WIN = ar.alloc([128, DC, EVEN_IN], BF16, "WIN")
        self.load_w_cast(WIN, inp["ev_w_in"][j], EVEN_IN, [])
        kvg = ar.alloc([128, 128], F32, "kvg"); self.bcast_load(kvg, inp["ev_a_kv_norm_g"][j:j + 1, :], [])
        klg = ar.alloc([128, 64], F32, "klg"); self.bcast_load(klg, inp["ev_a_kidx_ln_g"][j:j + 1, :], [])
        klb = ar.alloc([128, 64], F32, "klb"); self.bcast_load(klb, inp["ev_a_kidx_ln_b"][j:j + 1, :], [])
        epsr = ar.alloc([128, 2], F32, "epsr")
        kb.op("pool", lambda e: e.memset(epsr[:, 0:1], RMS_EPS), writes=[epsr])
        kb.op("pool", lambda e: e.memset(epsr[:, 1:2], LN_EPS), writes=[epsr])
        xTs = [ar.alloc([128, DC, 512], BF16, "xT%d" % i) for i in range(2)]
        pqs = [ar.alloc([128, 8, 512], BF16, "pqs%d" % i) for i in range(2)]
        pbs = [ar.alloc([128, 4, 512], F32, "pbs%d" % i) for i in range(2)]
        bgs = [ar.alloc([16, 512], BF16, "bgs%d" % i) for i in range(2)]
        cns = [ar.alloc([128, 512], BF16, "cns%d" % i) for i in range(2)]
        kis = [ar.alloc([128, 512], BF16, "kis%d" % i) for i in range(2)]
        wis = [ar.alloc([128, 4, 8], F32, "wis%d" % i) for i in range(2)]
        bvs = [ar.alloc([128, 4, 1024], BF16, "bvs%d" % i) for i in range(2)]
        st = ar.alloc([128, 32], F32, "st")
        junk = ar.alloc([128, 128], F32, "junk")
        cn = ar.alloc([128, 128], BF16, "cn")
        kin = ar.alloc([128, 64], F32, "kin")
        kin2 = ar.alloc([128, 128], BF16, "kin2")
        WSC = (8.0 ** -0.5) * (64.0 ** -0.5)
        for blk in range(self.N // 512):
            n0 = blk * 512
            b2 = blk % 2
            xT = xTs[b2]
            self.pump(2)
            self.ld("sp", xT[:, :, :], XTold[:, n0:n0 + 512].rearrange("(c p) t -> p c t", p=128), [XTold], [xT])
            fm = [(g * 128, 128, pqs[b2], g, 1.0) for g in range(4)] + [(640 + g * 128, 128, pqs[b2], 4 + g, 1.0) for g in range(4)] + \
                 [(1224 + g * 128, 128, pbs[b2], g, 0.125) for g in range(2)] + [(1480 + g * 128, 128, pbs[b2], 2 + g, 1.0) for g in range(2)] + \
                 [(2248, 16, bgs[b2], None, 1.0)]
            for gi, (c0, M, dst, slot, sc) in enumerate(fm):
                pb = PB[gi % 2]
                for c in range(DC):
                    self.mm(pb[0:M, :], WIN[:, c, c0:c0 + M], xT[:, c, :], c == 0, c == DC - 1, [WIN, xT], [pb])
                o = dst[0:M, :] if slot is None else dst[:, slot, :]
                if gi % 2 == 0:
                    self.act(o, pb[0:M, :], AF.Copy, [pb], [dst], scale=sc)
                else:
                    self.ts("dve", o, pb[0:M, :], sc, None, ALU.mult, ALU.bypass, [pb], [dst])
            self.ld("sp", self.PQ[0:1024, n0:n0 + 512].rearrange("(g p) t -> p g t", p=128), pqs[b2][:, :, :], [pqs[b2]], [self.PQ])
            self.ld("sp", self.PBQK[:, n0:n0 + 512].rearrange("(g p) t -> p g t", p=128), pbs[b2][:, :, :], [pbs[b2]], [self.PBQK])
            self.ld("sp", self.BGTd[:, n0:n0 + 512], bgs[b2][:, :], [bgs[b2]], [self.BGTd])
            for u in range(4):
                xs = lambda c: xT[:, c, u * 128:(u + 1) * 128]
                p2 = PB[2]
                for c in range(DC):
                    self.mm(p2[:, 0:128], xs(c), WIN[:, c, 512:640], c == 0, c == DC - 1, [xT, WIN], [p2])
                for c in range(DC):
                    self.mm(p2[:, 128:200], xs(c), WIN[:, c, 1152:1224], c == 0, c == DC - 1, [xT, WIN], [p2])
                self.act(junk[:, :], p2[:, 0:128], AF.Square, [p2], [junk, st], accum_out=st[:, 0:1])
                self.act(st[:, 1:2], st[:, 0:1], AF.Sqrt, [st, epsr], [st], scale=1.0 / 128, bias=epsr[:, 0:1])
                kb.op("dve", lambda e: e.reciprocal(out=st[:, 2:3], in_=st[:, 1:2]), reads=[st], writes=[st])
                self.stt(cn[:, :], p2[:, 0:128], st[:, 2:3], kvg[:, :], ALU.mult, ALU.mult, [p2, st, kvg], [cn])
                p3 = PB[3]
                p3b = p3[:, :].bitcast(BF16)
                self.tp(p3b[:, 0:128], cn[:, :], self.identb[:, :], [cn, self.identb], [p3])
                kb.op("dve", lambda e: e.bn_stats(out=st[:, 4:10], in_=p2[:, 128:192]), reads=[p2], writes=[st])
                kb.op("dve", lambda e: e.bn_aggr(out=st[:, 10:12], in_=st[:, 4:10]), reads=[st], writes=[st])
                self.act(st[:, 12:13], st[:, 11:12], AF.Sqrt, [st, epsr], [st], scale=1.0, bias=epsr[:, 1:2])
                kb.op("dve", lambda e: e.reciprocal(out=st[:, 13:14], in_=st[:, 12:13]), reads=[st], writes=[st])
                self.ts("dve", st[:, 14:15], st[:, 10:11], -1.0, st[:, 13:14], ALU.mult, ALU.mult, [st], [st])
                self.act(kin[:, :], p2[:, 128:192], AF.Identity, [p2, st], [kin], scale=st[:, 13:14], bias=st[:, 14:15])
                self.tt("dve", kin[:, :], kin[:, :], klg[:, :], ALU.mult, [kin, klg], [kin])
                self.tt("dve", kin2[:, 0:64], kin[:, :], klb[:, :], ALU.add, [kin, klb], [kin2])
                self.tt("dve", kin2[:, 64:128], kin[:, :], klb[:, :], ALU.add, [kin, klb], [kin2])
                self.tp(p3b[:, 128:256], kin2[:, :], self.identb[:, :], [kin2, self.identb], [p3])
                self.cp("act", cns[b2][:, u * 128:(u + 1) * 128], p3b[:, 0:128], [p3], [cns[b2]])
                self.cp("act", kis[b2][:, u * 128:(u + 1) * 128], p3b[:, 128:256], [p3], [kis[b2]])
                self.ts("dve", wis[b2][:, u, :], p2[:, 192:200], WSC, None, ALU.mult, ALU.bypass, [p2], [wis[b2]])
                p4, p5 = PB[4], PB[5]
                for c in range(DC):
                    self.mm(p4[:, :], xs(c), WIN[:, c, 1736:2248], c == 0, c == DC - 1, [xT, WIN], [p4])
                for c in range(DC):
                    self.mm(p5[:, :], xs(c), WIN[:, c, 2264:2776], c == 0, c == DC - 1, [xT, WIN], [p5])
                self.cp("dve", bvs[b2][:, u, 0:512], p4[:, :], [p4], [bvs[b2]])
                self.act(bvs[b2][:, u, 512:1024], p5[:, :], AF.Silu, [p5], [bvs[b2]])
            self.ld("sp", self.CNTd[:, n0:n0 + 512], cns[b2][:, :], [cns[b2]], [self.CNTd])
            self.ld("sp", self.KITd[:, n0:n0 + 512], kis[b2][:, :], [kis[b2]], [self.KITd])
            self.ld("sp", self.WId[n0:n0 + 512, :].rearrange("(u p) e -> p u e", p=128), wis[b2][:, :, :], [wis[b2]], [self.WId])
            self.ld("sp", self.BVd[n0:n0 + 512, :].rearrange("(u p) f -> p u f", p=128), bvs[b2][:, :, :], [bvs[b2]], [self.BVd])

    def dsa(self, j, sq):
        kb, ar, PB = self.kb, self.ar, self.PB
        inp = self.inp
        S, NT = self.S, self.NT
        nb = sq * S
        ar.reset()
        QT = ar.alloc([128, 4, S], BF16, "QT")
        QIT = ar.alloc([128, 4, S], BF16, "QIT")
        CNT = ar.alloc([128, S], BF16, "CNT")
        KIT = ar.alloc([128, S], BF16, "KIT")
        KT2 = ar.alloc([128, S], BF16, "KT2")
        V1 = ar.alloc([128, NT, 65], BF16, "V1")
        WI = ar.alloc([128, NT, 8], F32, "WI")
        WUK2 = ar.alloc([128, 128], BF16, "WUK2")
        WUV = ar.alloc([128, 64], BF16, "WUV")
        self.ld("sp", QT[:, :, :], self.PQ[0:512, nb:nb + S].rearrange("(g p) t -> p g t", p=128), [self.PQ], [QT])
        self.ld("sp", QIT[:, :, :], self.PQ[512:1024, nb:nb + S].rearrange("(g p) t -> p g t", p=128), [self.PQ], [QIT])
        self.ld("sp", CNT[:, :], self.CNTd[:, nb:nb + S], [self.CNTd], [CNT])
        self.ld("sp", KIT[:, :], self.KITd[:, nb:nb + S], [self.KITd], [KIT])
        self.ld("sp", WI[:, :, :], self.WId[nb:nb + S, :].rearrange("(t p) e -> p t e", p=128), [self.WId], [WI])
        self.ld("pool", WUK2[:, 0:64], inp["ev_a_w_uk"][j], [], [WUK2])
        self.ld("pool", WUK2[:, 64:128], inp["ev_a_w_uk"][j], [], [WUK2])
        self.ld("pool", WUV[:, :], inp["ev_a_w_uv"][j], [], [WUV])
        kb.op("pool", lambda e: e.memset(V1[:, :, 64:65], 1.0), writes=[V1])
        for blk in range(S // 512):
            pb = PB[blk % 2]
            self.mm(pb[:, :], WUK2[:, :], CNT[:, blk * 512:(blk + 1) * 512], True, True, [WUK2, CNT], [pb])
            self.cp("act", KT2[:, blk * 512:(blk + 1) * 512], pb[:, :], [pb], [KT2])
        for t in range(NT):
            pb = PB[2 + t % 2]
            self.mm(pb[:, 0:64], CNT[:, t * 128:(t + 1) * 128], WUV[:, :], True, True, [CNT, WUV], [pb])
            self.cp("dve", V1[:, t, 0:64], pb[:, 0:64], [pb], [V1])
        SC = ar.alloc([128, S], F32, "SC")
        JK = ar.alloc([128, S], BF16, "JK")
        MK = ar.alloc([128, S], BF16, "MK")
        MKT = ar.alloc([128, NT, 128], BF16, "MKT")
        Rs = [ar.alloc([128, 512], F32, "R%d" % i) for i in range(2)]
        PTs = [ar.alloc([128, 512], BF16, "PT%d" % i) for i in range(4)]
        bs = ar.alloc([128, 16], F32, "bs")
        mixs = [ar.alloc([128, 512], BF16, "amix%d" % i) for i in range(2)]
        NIT = 12
        K = float(self.n_sel)
        rk = 0
        pk = 0
        for jt in range(NT):
            n = (jt + 1) * 128
            self.pump(1)
            for h in range(8):
                g, hf = h // 2, h % 2
                for k0 in range(0, n, 512):
                    w = min(512, n - k0)
                    pb = PB[rk % 2]
                    R = Rs[rk % 2]
                    rk += 1
                    self.mm(pb[:, 0:w], QIT[hf * 64:(hf + 1) * 64, g, jt * 128:(jt + 1) * 128], KIT[hf * 64:(hf + 1) * 64, k0:k0 + w], True, True, [QIT, KIT], [pb])
                    self.act(R[:, 0:w], pb[:, 0:w], AF.Relu, [pb], [R])
                    if h == 0:
                        self.ts("dve", SC[:, k0:k0 + w], R[:, 0:w], WI[:, jt, 0:1], None, ALU.mult, ALU.bypass, [R, WI], [SC])
                    else:
                        self.stt(SC[:, k0:k0 + w], R[:, 0:w], WI[:, jt, h:h + 1], SC[:, k0:k0 + w], ALU.mult, ALU.add, [R, WI, SC], [SC])
            thr = jt * 128 >= self.n_sel
            if thr:
                kb.op("dve", lambda e, n=n: e.tensor_reduce(out=bs[:, 0:1], in_=SC[:, 0:n], axis=AX.X, op=ALU.max), reads=[SC], writes=[bs])
                kb.op("dve", lambda e, n=n: e.tensor_reduce(out=bs[:, 1:2], in_=SC[:, 0:n], axis=AX.X, op=ALU.min), reads=[SC], writes=[bs])
                self.tt("dve", bs[:, 2:3], bs[:, 0:1], bs[:, 1:2], ALU.subtract, [bs], [bs])
            self.tt("dve", SC[:, jt * 128:n], SC[:, jt * 128:n], self.cbias[:, :], ALU.add, [SC, self.cbias], [SC])
            if thr:
                lo = bs[:, 1:2]
                for it in range(NIT):
                    f = 2.0 ** -(it + 1)
                    self.stt(bs[:, 3:4], bs[:, 2:3], f, lo, ALU.mult, ALU.add, [bs], [bs])
                    self.ts("dve", JK[:, 0:n], SC[:, 0:n], bs[:, 3:4], None, ALU.is_ge, ALU.add, [SC, bs], [JK, bs], accum=bs[:, 4:5])
                    self.ts("dve", bs[:, 5:6], bs[:, 4:5], K - 0.5, f, ALU.is_ge, ALU.mult, [bs], [bs])
                    self.stt(lo, bs[:, 5:6], bs[:, 2:3], lo, ALU.mult, ALU.add, [bs], [bs])
                self.ts("dve", MK[:, 0:n], SC[:, 0:n], lo, None, ALU.is_ge, ALU.bypass, [SC, bs], [MK])
            else:
                self.ts("dve", MK[:, 0:n], SC[:, 0:n], -1e29, None, ALU.is_ge, ALU.bypass, [SC], [MK])
            for i0 in range(0, jt + 1, 8):
                i1 = min(jt + 1, i0 + 8)
                p2 = PB[2]
                p2b = p2[:, :].bitcast(BF16)
                for i in range(i0, i1):
                    self.tp(p2b[:, (i - i0) * 128:(i - i0 + 1) * 128], MK[:, i * 128:(i + 1) * 128], self.identb[:, :], [MK, self.identb], [p2])
                self.cp("act", MKT[:, i0:i1, :], p2b[:, 0:(i1 - i0) * 128].rearrange("p (i t) -> p i t", t=128), [p2], [MKT])
            for b in range(2):
                self.mm(PB[6 + b][:, 0:260], self.zb[:, 0:128], self.zb[:, 0:260], True, False, [self.zb], [PB[6 + b]], skip=True)
            steps = [(i, hf) for i in range(jt + 1) for hf in range(2)]
            pts = {}

            def stA(k, jt=jt):
                nonlocal pk
                i, hf = steps[k]
                ps = PB[2 + pk % 4]
                PT = PTs[pk % 4]
                pk += 1
                pts[k] = PT
                self.mm(ps[:, :], KT2[hf * 64:(hf + 1) * 64, i * 128:(i + 1) * 128], QT[hf * 64:(hf + 1) * 64, :, jt * 128:(jt + 1) * 128], True, True, [KT2, QT], [ps])
                self.act(PT[:, :], ps[:, :], AF.Exp, [ps], [PT], scale=0.125)
                pv = PT[:, :].rearrange("p (g t) -> p g t", t=128)
                self.tt("dve", pv, pv, MKT[:, i, :].unsqueeze(1).to_broadcast([128, 4, 128]), ALU.mult, [PT, MKT], [PT])

            def stB(k, jt=jt):
                i, hf = steps[k]
                PT = pts.pop(k)
                for g in range(4):
                    h = 2 * g + hf
                    ob = PB[6 + h // 4]
                    self.mm(ob[:, (h % 4) * 65:(h % 4) * 65 + 65], PT[:, g * 128:(g + 1) * 128], V1[:, i, :], False, i == jt, [PT, V1], [ob], skip=True)
            LA_ = 2
            for k in range(min(LA_, len(steps))):
                stA(k)
            for k in range(len(steps)):
                if k + LA_ < len(steps):
                    stA(k + LA_)
                stB(k)
            mx = mixs[jt % 2]
            for b in range(2):
                ov = PB[6 + b][:, 0:260].rearrange("p (h d) -> p h d", d=65)
                kb.op("dve", lambda e, ov=ov, b=b: e.reciprocal(out=bs[:, 8 + b * 4:12 + b * 4].unsqueeze(2), in_=ov[:, :, 64:65]), reads=[PB[6 + b]], writes=[bs])
                self.tt("dve", mx[:, b * 256:(b + 1) * 256].rearrange("p (h d) -> p h d", d=64), ov[:, :, 0:64],
                        bs[:, 8 + b * 4:12 + b * 4].unsqueeze(2).to_broadcast([128, 4, 64]), ALU.mult, [PB[6 + b], bs], [mx])
            self.ld("sp", self.MIXD[nb + jt * 128:nb + (jt + 1) * 128, 0:512], mx[:, :], [mx], [self.MIXD])

    def gla(self, j, sq):
        kb, ar, PB = self.kb, self.ar, self.PB
        inp = self.inp
        S = self.S
        NCH = S // 64
        nb = sq * S
        ar.reset()
        BQs = [ar.alloc([64, S], F32, "BQ%d" % i) for i in range(2)]
        BKs = [ar.alloc([64, S], F32, "BK%d" % i) for i in range(2)]
        BG = ar.alloc([16, S], BF16, "BG")
        BV = ar.alloc([64, NCH, 1024], BF16, "BV")
        WG2 = ar.alloc([16, 256], BF16, "WG2")
        nbias = ar.alloc([64, 4], F32, "nbias")
        GNB = ar.alloc([64, 512], F32, "GNB")
        self.ld("sp", BG[:, :], self.BGTd[:, nb:nb + S], [self.BGTd], [BG])
        self.ld("sp", BV[:, :, :], self.BVd[nb:nb + S, :].rearrange("(c p) f -> p c f", p=64), [self.BVd], [BV])
        self.ld("pool", WG2[:, :], inp["ev_b_w_g2"][j], [], [WG2])
        for h in range(4):
            self.ld("sp", nbias[:, h:h + 1], inp["ev_b_b_g"][j, h * 64:(h + 1) * 64].rearrange("(p o) -> p o", o=1), [], [nbias])
        kb.op("dve", lambda e: e.tensor_scalar(out=nbias[:, :], in0=nbias[:, :], scalar1=-1.0, scalar2=None, op0=ALU.mult), reads=[nbias], writes=[nbias])
        self.ld("sp", GNB[:, :], inp["ev_b_norm_g"][j:j + 1, :].partition_broadcast(64), [], [GNB])
        LA = ar.alloc([64, 512], F32, "LA")
        BP = ar.alloc([64, 512], F32, "BP")
        EQ = ar.alloc([64, 512], F32, "EQ")
        EBL = ar.alloc([64, 4, NCH], F32, "EBL")
        QG = ar.alloc([64, 4, S], BF16, "QG")
        KG = ar.alloc([64, 4, S], BF16, "KG")
        KTM = ar.alloc([64, NCH, 256], BF16, "KTM")
        epsr = ar.alloc([64, 1], F32, "epsr")
        kb.op("pool", lambda e: e.memset(epsr[:, :], RMS_EPS), writes=[epsr])
        rst = self.rst[0:64, :]
        for h in range(4):
            BQ, BK = BQs[h % 2], BKs[h % 2]
            self.ld("sp", BQ[:, :], self.PBQK[h * 64:(h + 1) * 64, nb:nb + S], [self.PBQK], [BQ])
            self.ld("sp", BK[:, :], self.PBQK[256 + h * 64:256 + (h + 1) * 64, nb:nb + S], [self.PBQK], [BK])
            for blk in range(S // 512):
                sl = slice(blk * 512, (blk + 1) * 512)
                pb = PB[blk % 2]
                self.mm(pb[0:64, :], WG2[0:16, h * 64:(h + 1) * 64], BG[0:16, sl], True, True, [WG2, BG], [pb])
                self.act(LA[:, :], pb[0:64, :], AF.Exp, [pb, nbias], [LA], scale=-1.0, bias=nbias[:, h:h + 1])
                self.act(LA[:, :], LA[:, :], AF.Ln, [LA], [LA], bias=1.0)
                kb.op("dve", lambda e: e.tensor_tensor_scan(out=BP[:, :], data0=rst, data1=LA[:, :], initial=0.0, op0=ALU.mult, op1=ALU.add),
                      reads=[self.rst, LA], writes=[BP])
                self.act(EQ[:, :], BP[:, :], AF.Exp, [BP], [EQ], scale=-1.0 / 16)
                self.tt("dve", QG[:, h, sl], BQ[:, sl], EQ[:, :], ALU.mult, [BQ, EQ], [QG])
                self.cp("dve", EBL[:, h, blk * 8:(blk + 1) * 8], EQ[:, :].rearrange("p (c j) -> p c j", j=64)[:, :, 63], [EQ], [EBL])
                self.act(EQ[:, :], BP[:, :], AF.Exp, [BP], [EQ], scale=1.0 / 16)
                self.tt("dve", KG[:, h, sl], BK[:, sl], EQ[:, :], ALU.mult, [BK, EQ], [KG])
        for c in range(NCH):
            pb = PB[2 + c % 2]
            pbb = pb[:, :].bitcast(BF16)
            for h in range(4):
                self.tp(pbb[0:64, h * 64:(h + 1) * 64], KG[:, h, c * 64:(c + 1) * 64], self.identb[0:64, 0:64], [KG, self.identb], [pb])
            self.cp("act", KTM[:, c, :], pbb[0:64, 0:256], [pb], [KTM])
            self.tt("pool", BV[:, c, 512:1024], BV[:, c, 512:1024], GNB[:, :], ALU.mult, [BV, GNB], [BV])
        ST32 = ar.alloc([64, 4, 128], F32, "ST32")
        STB = ar.alloc([64, 4, 128], BF16, "STB")
        TMP = ar.alloc([64, 4, 128], F32, "TMP")
        SCMs = [ar.alloc([64, 256], BF16, "SCM%d" % i) for i in range(2)]
        ss = ar.alloc([64, 16], F32, "ss")
        junk = ar.alloc([64, 128], F32, "gjunk")
        mixs = [ar.alloc([64, 512], BF16, "bmix%d" % i) for i in range(2)]
        kb.op("pool", lambda e: e.memset(ST32[:, :, :], 0.0), writes=[ST32])
        kb.op("pool", lambda e: e.memset(STB[:, :, :], 0.0), writes=[STB])
        trig = self.trigb[0:64, :]
        for c in range(NCH):
            cs = slice(c * 64, (c + 1) * 64)
            psc = PB[c % 2]
            for h in range(4):
                self.mm(psc[0:64, h * 64:(h + 1) * 64], KG[:, h, cs], QG[:, h, cs], True, True, [KG, QG], [psc])
            SCM = SCMs[c % 2]
            self.tt("dve", SCM[:, :].rearrange("p (h i) -> p h i", i=64), psc[0:64, 0:256].rearrange("p (h i) -> p h i", i=64),
                    trig.unsqueeze(1).to_broadcast([64, 4, 64]), ALU.mult, [psc, self.trigb], [SCM])
            og = PB[2 + c % 2]
            for h in range(4):
                self.mm(og[0:64, h * 128:(h + 1) * 128], SCM[:, h * 64:(h + 1) * 64], BV[:, c, h * 128:(h + 1) * 128], True, c == 0, [SCM, BV], [og])
                if c > 0:
                    self.mm(og[0:64, h * 128:(h + 1) * 128], QG[:, h, cs], STB[:, h, :], False, True, [QG, STB], [og])
            if c < NCH - 1:
                pst = PB[4 + c % 2]
                for h in range(4):
                    self.mm(pst[0:64, h * 128:(h + 1) * 128], KTM[:, c, h * 64:(h + 1) * 64], BV[:, c, h * 128:(h + 1) * 128], True, True, [KTM, BV], [pst])
                for h in range(4):
                    self.act(TMP[:, h, :], pst[0:64, h * 128:(h + 1) * 128], AF.Identity, [pst, EBL], [TMP], scale=EBL[:, h, c:c + 1])
                    self.stt(ST32[:, h, :], ST32[:, h, :], EBL[:, h, c:c + 1], TMP[:, h, :], ALU.mult, ALU.add, [ST32, EBL, TMP], [ST32])
                self.cp("pool", STB[:, :, :], ST32[:, :, :], [ST32], [STB])
            mx = mixs[c % 2]
            for h in range(4):
                self.act(junk[:, :], og[0:64, h * 128:(h + 1) * 128], AF.Square, [og], [junk, ss], accum_out=ss[:, h:h + 1])
            self.act(ss[:, 4:8], ss[:, 0:4], AF.Sqrt, [ss, epsr], [ss], scale=1.0 / 128, bias=epsr[:, 0:1])
            kb.op("dve", lambda e: e.reciprocal(out=ss[:, 8:12], in_=ss[:, 4:8]), reads=[ss], writes=[ss])
            for h in range(4):
                self.stt(mx[:, h * 128:(h + 1) * 128], og[0:64, h * 128:(h + 1) * 128], ss[:, 8 + h:9 + h], BV[:, c, 512 + h * 128:512 + (h + 1) * 128],
                         ALU.mult, ALU.mult, [og, ss, BV], [mx])
            self.ld("sp", self.MIXD[nb + c * 64:nb + (c + 1) * 64, 512:1024], mx[:, :], [mx], [self.MIXD])

    def odd_proj(self, j, XTold):
        kb, ar, PB = self.kb, self.ar, self.PB
        self.even_scratch()
        ar.reset()
        W = ar.alloc([128, DC, 3072], BF16, "WQKV")
        self.load_w_cast(W, self.inp["od_w_qkv"][j], 3072, [])
        xTs = [ar.alloc([128, DC, 512], BF16, "xT%d" % i) for i in range(2)]
        pqs = [ar.alloc([128, 16, 512], BF16, "pqs%d" % i) for i in range(2)]
        vs = [ar.alloc([128, 4, 1024], BF16, "vs%d" % i) for i in range(2)]
        for blk in range(self.N // 512):
            n0 = blk * 512
            b2 = blk % 2
            xT = xTs[b2]
            self.ld("sp", xT[:, :, :], XTold[:, n0:n0 + 512].rearrange("(c p) t -> p c t", p=128), [XTold], [xT])
            for g in range(16):
                pb = PB[g % 2]
                for c in range(DC):
                    self.mm(pb[:, :], W[:, c, g * 128:(g + 1) * 128], xT[:, c, :], c == 0, c == DC - 1, [W, xT], [pb])
                self.cp("act" if g % 2 == 0 else "dve", pqs[b2][:, g, :], pb[:, :], [pb], [pqs[b2]])
            self.ld("sp", self.PQ[:, n0:n0 + 512].rearrange("(g p) t -> p g t", p=128), pqs[b2][:, :, :], [pqs[b2]], [self.PQ])
            for u in range(4):
                for h in range(2):
                    pb = PB[2 + (2 * u + h) % 4]
                    for c in range(DC):
                        self.mm(pb[:, :], xT[:, c, u * 128:(u + 1) * 128], W[:, c, 2048 + h * 512:2048 + (h + 1) * 512], c == 0, c == DC - 1, [xT, W], [pb])
                    self.cp("act" if h == 0 else "dve", vs[b2][:, u, h * 512:(h + 1) * 512], pb[:, :], [pb], [vs[b2]])
            self.ld("sp", self.BVd[n0:n0 + 512, :].rearrange("(u p) f -> p u f", p=128), vs[b2][:, :, :], [vs[b2]], [self.BVd])

    def diffattn(self, j, sq, lambda_init):
        kb, ar, PB = self.kb, self.ar, self.PB
        inp = self.inp
        S, NT, NB = self.S, self.NT, self.NB
        nb = sq * S
        ar.reset()
        QT = ar.alloc([128, 8, S], BF16, "QT")
        KT = ar.alloc([128, 8, S], BF16, "KT")
        V1 = ar.alloc([128, NT, 8, 129], BF16, "V1")
        self.ld("sp", QT[:, :, :], self.PQ[0:1024, nb:nb + S].rearrange("(g p) t -> p g t", p=128), [self.PQ], [QT])
        self.ld("sp", KT[:, :, :], self.PQ[1024:2048, nb:nb + S].rearrange("(g p) t -> p g t", p=128), [self.PQ], [KT])
        kb.op("pool", lambda e: e.memset(V1[:, :, :, 128:129], 1.0), writes=[V1])
        for t in range(NT):
            self.ld("sp", V1[:, t, :, 0:128], self.BVd[nb + t * 128:nb + (t + 1) * 128, :].rearrange("p (h e) -> p h e", e=128), [self.BVd], [V1])
        LM = ar.alloc([128, 4, 64], F32, "LM")
        lm = ar.alloc([128, 16], F32, "lm")
        SG = ar.alloc([128, 128], F32, "SG")
        epsr = ar.alloc([128, 1], F32, "epsr")
        kb.op("pool", lambda e: e.memset(epsr[:, :], RMS_EPS), writes=[epsr])
        self.ld("sp", LM[:, :, :].rearrange("p a b -> p (a b)"), inp["od_lam"][j:j + 1].rearrange("o a b -> o (a b)").partition_broadcast(128), [], [LM])
        self.bcast_load(SG, inp["od_subln_g"][j:j + 1, :], [])
        kb.op("dve", lambda e: e.tensor_scalar(out=SG[:, :], in0=SG[:, :], scalar1=1.0 - lambda_init, scalar2=None, op0=ALU.mult), reads=[SG], writes=[SG])
        junk = ar.alloc([128, 128], F32, "junk")
        for q in range(2):
            kb.op("dve", lambda e, q=q: e.tensor_tensor(out=junk[:, 0:64], in0=LM[:, 2 * q, :], in1=LM[:, 2 * q + 1, :], op=ALU.mult), reads=[LM], writes=[junk])
            kb.op("dve", lambda e, q=q: e.tensor_reduce(out=lm[:, q:q + 1], in_=junk[:, 0:64], axis=AX.X, op=ALU.add), reads=[junk], writes=[lm])
        self.act(lm[:, 2:4], lm[:, 0:2], AF.Exp, [lm], [lm])
        self.tt("dve", lm[:, 4:5], lm[:, 3:4], lm[:, 2:3], ALU.subtract, [lm], [lm])
        self.ts("dve", lm[:, 5:6], lm[:, 4:5], -lambda_init, None, ALU.add, ALU.bypass, [lm], [lm])
        PTs = [ar.alloc([128, 512], BF16, "PT%d" % i) for i in range(4)]
        dt_ = ar.alloc([128, 128], F32, "dt")
        st = ar.alloc([128, 16], F32, "st")
        mos = [ar.alloc([128, 4, 128], BF16, "mo%d" % i) for i in range(2)]
        osbs = [ar.alloc([128, 4, 258], F32, "osb%d" % i) for i in range(2)]
        pk = 0
        it = 0
        for h in range(8):
            for tb in range(NB):
                self.pump(1)

                def bank(m, u):
                    return PB[4 + 2 * m + u // 2], (u % 2) * 129
                for b in range(4):
                    self.mm(PB[4 + b][:, 0:258], self.zb[:, 0:128], self.zb[:, 0:258], True, False, [self.zb], [PB[4 + b]], skip=True)
                steps = [(m, i) for m in range(2) for i in range(4 * tb + 4)]
                pts = {}

                def stA(k, h=h, tb=tb):
                    nonlocal pk
                    m, i = steps[k]
                    ms = slice(m * 64, (m + 1) * 64)
                    u0 = max(0, i - 4 * tb)
                    w = 512 - u0 * 128
                    ps = PB[pk % 4]
                    PT = PTs[pk % 4]
                    pk += 1
                    pts[k] = PT
                    self.mm(ps[:, 0:w], KT[ms, h, i * 128:(i + 1) * 128], QT[ms, h, tb * 512 + u0 * 128:(tb + 1) * 512], True, True, [KT, QT], [ps])
                    self.act(PT[:, 0:w], ps[:, 0:w], AF.Exp, [ps], [PT], scale=0.125)
                    if i >= 4 * tb:
                        self.tt("pool", PT[:, 0:128], PT[:, 0:128], self.trib[:, :], ALU.mult, [PT, self.trib], [PT])

                def stB(k, h=h, tb=tb):
                    m, i = steps[k]
                    u0 = max(0, i - 4 * tb)
                    PT = pts.pop(k)
                    for u in range(u0, 4):
                        bk, off = bank(m, u)
                        self.mm(bk[:, off:off + 129], PT[:, (u - u0) * 128:(u - u0 + 1) * 128], V1[:, i, h, :], False, i == 4 * tb + u, [PT, V1], [bk], skip=True)
                LA_ = 2
                for k in range(min(LA_, len(steps))):
                    stA(k)
                for k in range(len(steps)):
                    if k + LA_ < len(steps):
                        stA(k + LA_)
                    stB(k)
                mo = mos[it % 2]
                osb = osbs[it % 2]
                it += 1
                for b in range(4):
                    self.cp("act" if b % 2 == 0 else "dve", osb[:, b, :], PB[4 + b][:, 0:258], [PB[4 + b]], [osb])
                for u in range(4):
                    o0 = (u % 2) * 129
                    s0 = osb[:, u // 2, :]
                    s1 = osb[:, 2 + u // 2, :]
                    kb.op("dve", lambda e, s0=s0, o0=o0: e.reciprocal(out=st[:, 0:1], in_=s0[:, o0 + 128:o0 + 129]), reads=[osb], writes=[st])
                    kb.op("dve", lambda e, s1=s1, o0=o0: e.reciprocal(out=st[:, 1:2], in_=s1[:, o0 + 128:o0 + 129]), reads=[osb], writes=[st])
                    self.tt("dve", st[:, 2:3], st[:, 1:2], lm[:, 5:6], ALU.mult, [st, lm], [st])
                    self.ts("dve", dt_[:, :], s0[:, o0:o0 + 128], st[:, 0:1], None, ALU.mult, ALU.bypass, [osb, st], [dt_])
                    self.stt(dt_[:, :], s1[:, o0:o0 + 128], st[:, 2:3], dt_[:, :], ALU.mult, ALU.add, [osb, st, dt_], [dt_])
                    self.act(junk[:, :], dt_[:, :], AF.Square, [dt_], [junk, st], accum_out=st[:, 3:4])
                    self.act(st[:, 4:5], st[:, 3:4], AF.Sqrt, [st, epsr], [st], scale=1.0 / 128, bias=epsr[:, 0:1])
                    kb.op("dve", lambda e: e.reciprocal(out=st[:, 5:6], in_=st[:, 4:5]), reads=[st], writes=[st])
                    self.stt(mo[:, u, :], dt_[:, :], st[:, 5:6], SG[:, :], ALU.mult, ALU.mult, [dt_, st, SG], [mo])
                self.ld("sp", self.MIXD[nb + tb * 512:nb + (tb + 1) * 512, h * 128:(h + 1) * 128].rearrange("(u p) e -> p u e", p=128), mo[:, :, :], [mo], [self.MIXD])

    def build(self):
        inp = self.inp
        NL = self.NL
        nsub = 2 * NL
        xs = [inp["x"]] + [self.XA if k % 2 == 0 else self.XB for k in range(nsub - 1)] + [self.out]
        ph = self.dbg
        on = lambda p: ph is None or p in ph
        self.queue_conversions()
        self.pump(0, upto=(0, 0))
        if on("init"):
            self.init_xt(inp["x"], self.XT[0])
        k = 0
        for li in range(NL):
            j = li // 2
            if li % 2 == 0:
                if on("eproj"):
                    self.even_proj(j, self.XT[k % 2])
                for sq in range(self.NSEQ):
                    if on("dsa"):
                        self.dsa(j, sq)
                    if on("gla"):
                        self.gla(j, sq)
                if on("oproj"):
                    self.outproj_phase(inp["ev_w_out"][j], xs[k], xs[k + 1], self.XT[(k + 1) % 2], inp["ev_ln1_g"][j:j + 1, :], inp["ev_ln1_b"][j:j + 1, :])
                k += 1
                if on("ffn"):
                    self.ffn_phase(j, xs[k], xs[k + 1], self.XT[k % 2], self.XT[(k + 1) % 2], inp["ev_ln2_g"][j:j + 1, :], inp["ev_ln2_b"][j:j + 1, :])
                k += 1
            else:
                lambda_init = 0.8 - 0.6 * math.exp(-0.3 * li)
                if on("qkv"):
                    self.odd_proj(j, self.XT[k % 2])
                for sq in range(self.NSEQ):
                    if on("dattn"):
                        self.diffattn(j, sq, lambda_init)
                if on("oproj2"):
                    self.outproj_phase(inp["od_w_out"][j], xs[k], xs[k + 1], self.XT[(k + 1) % 2], inp["od_ln1_g"][j:j + 1, :], inp["od_ln1_b"][j:j + 1, :],
                                       router=(inp["od_router_w"][j], inp["od_router_b"][j:j + 1, :]))
                k += 1
                if on("moe"):
                    self.route(xs[k])
                    self.moe_sparse(j, xs[k], xs[k + 1], self.XT[(k + 1) % 2], inp["od_ln2_g"][j:j + 1, :], inp["od_ln2_b"][j:j + 1, :])
                k += 1
        self.kb.barrier()
        self.kb.emit()
        return self.nc


def make_consts():
    c = np.zeros((128, 1024), np.float32)
    p = np.arange(128)
    c[:, 0:128] = np.eye(128, dtype=np.float32)
    c[:, 128:256] = np.where(p[None, :] <= p[:, None], 0.0, -1e30)
    c[:, 256:384] = (p[:, None] <= p[None, :]).astype(np.float32)
    c[:, 384:896] = (np.arange(512)[None, :] % 64 != 0).astype(np.float32)
    c[:, 896:960] = ((p[:, None] % 64) <= np.arange(64)[None, :]).astype(np.float32)
    c[:, 960:968] = 512.0 * np.arange(8)[None, :]
    c[:, 968:992] = 512.0 * np.arange(24)[None, :]
    c[:, 992:1003] = np.arange(11)[None, :]
    c[:, 1003] = 11.0 * p
    return c


_CACHE = {}


def run_prog(inputs, S, NSEQ, NL, ncores, trace=False):
    key = (S, NSEQ, NL)
    if key not in _CACHE:
        p_ = Prog(S, NSEQ, NL)
        _CACHE[key] = (p_.build(), set(p_.inp.keys()))
    nc, names = _CACHE[key]
    x = np.ascontiguousarray(inputs["x"], dtype=np.float32)
    shared = {"cst": make_consts()}
    for name, v in inputs.items():
        if name == "x" or name.startswith("od_lam_") or name not in names:
            continue
        v = np.ascontiguousarray(v, dtype=np.float32)
        if name == "ev_b_norm_g":
            v = v.reshape(2, 512)
        shared[name] = v
    shared["od_lam"] = np.ascontiguousarray(np.stack([inputs["od_lam_q1"], inputs["od_lam_k1"], inputs["od_lam_q2"], inputs["od_lam_k2"]], axis=1), dtype=np.float32)
    in_maps = []
    for c in range(ncores):
        m = dict(shared)
        m["x"] = np.ascontiguousarray(x[c * NSEQ:(c + 1) * NSEQ].reshape(NSEQ * S, D))
        in_maps.append(m)
    res = run_bass_kernel_spmd(nc, in_maps, core_ids=list(range(ncores)), trace=trace)
    out = np.concatenate([r["out"].reshape(NSEQ, S, D) for r in res.results], axis=0)
    return out, res


def kernel(**inputs):
    out, _ = run_prog(inputs, 2048, 2, 4, 8)
    return out.astype(np.float32)
```
